# Optimizing a Trainium2 kernel written in Bass

```python
import math
import numpy as np
import jax
import jax.numpy as jnp
from jax import lax

D_MODEL = 1024
BATCH = 4
SEQ = 8192
DEPTH = 4

GRID_W = 64
CTX_LEN = 256

A_HEADS = 4
A_HD = 64
A_WIDTH = A_HEADS * 2 * A_HD
B_HEADS = 8
B_HD = 64
B_WIDTH = B_HEADS * B_HD
B_DECAY_RANK = 64
B_ICL_RANK = 64
B_GATE_RANK = 128
C_WIDTH = 512
N_BRANCH = 3
IN_SIZES = (A_WIDTH, A_WIDTH, A_WIDTH,
            B_WIDTH, B_WIDTH, B_WIDTH,
            B_DECAY_RANK, B_ICL_RANK, B_GATE_RANK,
            C_WIDTH, C_WIDTH, C_WIDTH,
            N_BRANCH * D_MODEL)
N_IN = 3 * A_WIDTH + 3 * B_WIDTH + B_DECAY_RANK + B_ICL_RANK + B_GATE_RANK + 3 * C_WIDTH + N_BRANCH * D_MODEL
N_EXPERTS = 16
EXPERT_FF = 1024
CAPACITY_FACTOR = 2

ROPE_THETA = 10000.0
Q_BLOCK = 128
NORM_EPS = 1e-6
GN_EPS = 64e-5

kernel_name = "hybrid_diffusion_trunk"


def _split(t, sizes):
    cuts = [int(v) for v in np.cumsum(sizes)[:-1]]
    return jnp.split(t, cuts, axis=-1)


def _rmsnorm(x, g):
    xf = x.astype(jnp.float32)
    y = xf * lax.rsqrt(jnp.mean(xf * xf, axis=-1, keepdims=True) + NORM_EPS)
    return (y * g.astype(jnp.float32)).astype(x.dtype)


def _adaln_params(cond, w, b):
    return jnp.split(jax.nn.silu(cond) @ w + b, 6, axis=-1)


def _dwconv3(x, w):
    xp = jnp.pad(x, ((0, 0), (1, 1), (0, 0)))
    return w[0] * xp[:, :-2] + w[1] * xp[:, 1:-1] + w[2] * xp[:, 2:]


def _axial_rope_tables(rows):
    half = A_HD // 2
    inv = jnp.power(ROPE_THETA, -jnp.arange(0, half, 2, dtype=jnp.float32) / half)
    r = jnp.repeat(jnp.arange(rows, dtype=jnp.float32), GRID_W)
    col = jnp.tile(jnp.arange(GRID_W, dtype=jnp.float32), rows)
    ang = jnp.concatenate([r[:, None] * inv, col[:, None] * inv], axis=-1)
    return jnp.cos(ang), jnp.sin(ang)


def _rope(x, cos, sin):
    half = A_HD // 2
    xf = x.astype(jnp.float32)
    x1, x2 = xf[..., :half], xf[..., half:]
    c = cos[None, :, None, None, :]
    s = sin[None, :, None, None, :]
    return jnp.concatenate([x1 * c - x2 * s, x2 * c + x1 * s], axis=-1).astype(x.dtype)


def _diff_softmax_attn(q, k, v, lam):
    s = jnp.einsum('bqhmd,bkhmd->bhmqk', q, k, preferred_element_type=jnp.float32) * (A_HD ** -0.5)
    p = jax.nn.softmax(s, axis=-1)
    w = p[:, :, 0] - lam * p[:, :, 1]
    return jnp.einsum('bhqk,bkhe->bqhe', w.astype(v.dtype), v)


def _branch_diff_attn(pc, pl, cos, sin, q_g, k_g, lam_vec, subln_g, lam_init, need_ctx):
    def q_heads(p):
        b, t, _ = p[0].shape
        return _rmsnorm(p[0].reshape(b, t, A_HEADS, 2, A_HD), q_g)

    def kv_heads(p):
        b, t, _ = p[1].shape
        k = _rmsnorm(p[1].reshape(b, t, A_HEADS, 2, A_HD), k_g)
        v = p[2].reshape(b, t, A_HEADS, 2 * A_HD)
        return k, v

    kc, vc = kv_heads(pc)
    kl, vl = kv_heads(pl)
    ql = _rope(q_heads(pl), cos, sin)
    kl = _rope(kl, cos, sin)
    lv = lam_vec.astype(jnp.float32)
    lam = jnp.exp(jnp.sum(lv[0] * lv[1])) - jnp.exp(jnp.sum(lv[2] * lv[3])) + lam_init

    k_all = jnp.concatenate([kc, kl], axis=1)
    v_all = jnp.concatenate([vc, vl], axis=1)
    b, t = ql.shape[:2]
    nblk = t // Q_BLOCK
    qb = jnp.moveaxis(ql.reshape(b, nblk, Q_BLOCK, A_HEADS, 2, A_HD), 1, 0)
    ob = lax.map(lambda qq: _diff_softmax_attn(qq, k_all, v_all, lam), qb)
    ol = jnp.moveaxis(ob, 0, 1).reshape(b, t, A_HEADS, 2 * A_HD)

    def post(o):
        return (_rmsnorm(o, subln_g) * (1.0 - lam_init)).reshape(o.shape[0], o.shape[1], A_WIDTH)

    yc = post(_diff_softmax_attn(q_heads(pc), kc, vc, lam)) if need_ctx else None
    return yc, post(ol)


def _bheads(t):
    return t.astype(jnp.float32).reshape(t.shape[0], t.shape[1], B_HEADS, B_HD)


def _rwkv_prep(p, conv_w, k_k):
    r, k, v = jnp.split(_dwconv3(jnp.concatenate(p[:3], axis=-1), conv_w), 3, axis=-1)
    kk = _bheads(k * k_k)
    kk = kk / jnp.maximum(jnp.sqrt(jnp.sum(kk * kk, axis=-1, keepdims=True)), 1e-12)
    return r, k, v, kk


def _rwkv_direction(k, wl, al, w0, w_up, a0, a_up, k_a):
    w = -jax.nn.softplus(-(w0 + jnp.tanh(wl) @ w_up)) - 0.5
    decay = jnp.exp(-jnp.exp(w.astype(jnp.float32)))
    a = jax.nn.sigmoid(a0 + al @ a_up)
    kd = k * (1.0 + (a - 1.0) * k_a)
    return _bheads(decay), _bheads(a), _bheads(kd)


def _rwkv_scan(s0, decay, kk, a, k, v, r, reverse):
    emit = r is not None
    xs = (decay, kk, a, k, v) + ((r,) if emit else ())
    xs = tuple(jnp.moveaxis(t, 1, 0) for t in xs)

    def step(s, inp):
        wt, kkt, at, kt, vt = inp[:5]
        sa = jnp.einsum('bhvk,bhk->bhv', s, kkt)
        s = s * wt[:, :, None, :] - sa[..., None] * (kkt * at)[:, :, None, :] + vt[..., None] * kt[:, :, None, :]
        y = jnp.einsum('bhvk,bhk->bhv', s, inp[5]) if emit else None
        return s, y

    s_fin, ys = lax.scan(step, s0, xs, reverse=reverse)
    return s_fin, (jnp.moveaxis(ys, 0, 1) if emit else None)


def _rwkv_readout(o, r, kd, v, r_k, ln_g, ln_b):
    mu = jnp.mean(o, axis=-1, keepdims=True)
    dlt = o - mu
    on = dlt * lax.rsqrt(jnp.mean(dlt * dlt, axis=-1, keepdims=True) + GN_EPS)
    bonus = jnp.sum(r * kd * r_k.astype(jnp.float32), axis=-1, keepdims=True) * v
    y = on * ln_g.astype(jnp.float32).reshape(B_HEADS, B_HD) + ln_b.astype(jnp.float32).reshape(B_HEADS, B_HD) + bonus
    return y.reshape(o.shape[0], o.shape[1], B_WIDTH)


def _branch_rwkv(pc, pl, conv_w, w0, w_up, a0, a_up, g_up, k_k, k_a, r_k, ln_g, ln_b, need_ctx):
    rc, kc, vc, kkc = _rwkv_prep(pc, conv_w, k_k)
    rl, kl, vl, kkl = _rwkv_prep(pl, conv_w, k_k)
    vc_h = _bheads(vc)
    rl_h, vl_h = _bheads(rl), _bheads(vl)
    rc_h = _bheads(rc) if need_ctx else None
    s0 = jnp.zeros((pl[0].shape[0], B_HEADS, B_HD, B_HD), jnp.float32)
    y_l = 0.0
    y_c = 0.0
    for d in range(2):
        rev = d == 1
        dec_c, a_c, kd_c = _rwkv_direction(kc, pc[3], pc[4], w0[d], w_up[d], a0[d], a_up[d], k_a)
        s_c, o_c = _rwkv_scan(s0, dec_c, kkc, a_c, kd_c, vc_h, rc_h, rev)
        dec_l, a_l, kd_l = _rwkv_direction(kl, pl[3], pl[4], w0[d], w_up[d], a0[d], a_up[d], k_a)
        _, o_l = _rwkv_scan(s_c, dec_l, kkl, a_l, kd_l, vl_h, rl_h, rev)
        y_l = y_l + _rwkv_readout(o_l, rl_h, kd_l, vl_h, r_k, ln_g, ln_b)
        if need_ctx:
            y_c = y_c + _rwkv_readout(o_c, rc_h, kd_c, vc_h, r_k, ln_g, ln_b)
    out_l = (y_l * (jax.nn.sigmoid(pl[5]) @ g_up)).astype(pl[0].dtype)
    out_c = (y_c * (jax.nn.sigmoid(pc[5]) @ g_up)).astype(pc[0].dtype) if need_ctx else None
    return out_c, out_l


def _branch_conv(p, w):
    bg, cg, xv = p
    return bg * _dwconv3(cg * xv, w)


def _merge(ys, gates, w_branch, w_out):
    gs = jnp.split(jax.nn.sigmoid(gates), N_BRANCH, axis=-1)
    m = gs[0] * (ys[0] @ w_branch[0]) + gs[1] * (ys[1] @ w_branch[1]) + gs[2] * (ys[2] @ w_branch[2])
    return m @ w_out


def _mixer(hc, hl, lp, lam_init, cos, sin, need_ctx):
    pc = _split(hc @ lp['w_in'], IN_SIZES)
    pl = _split(hl @ lp['w_in'], IN_SIZES)
    ac, al = _branch_diff_attn(pc[0:3], pl[0:3], cos, sin, lp['q_norm_g'], lp['k_norm_g'],
                               lp['diff_lambda'], lp['diff_subln_g'], lam_init, need_ctx)
    bc, bl = _branch_rwkv(pc[3:9], pl[3:9], lp['rwkv_conv_w'], lp['rwkv_w0'], lp['rwkv_w_up'],
                          lp['rwkv_a0'], lp['rwkv_a_up'], lp['rwkv_g_up'], lp['rwkv_k_k'],
                          lp['rwkv_k_a'], lp['rwkv_r_k'], lp['rwkv_ln_g'], lp['rwkv_ln_b'], need_ctx)
    cl = _branch_conv(pl[9:12], lp['conv_w'])
    yl = _merge((al, bl, cl), pl[12], lp['w_branch'], lp['w_out'])
    yc = None
    if need_ctx:
        cc = _branch_conv(pc[9:12], lp['conv_w'])
        yc = _merge((ac, bc, cc), pc[12], lp['w_branch'], lp['w_out'])
    return yc, yl


def _expert_choice(h, router_w, w1, w3, w2):
    b, n, dm = h.shape
    cap = CAPACITY_FACTOR * n // N_EXPERTS
    aff = jax.nn.softmax((h @ router_w).astype(jnp.float32), axis=-1)
    gate, idx = lax.top_k(jnp.swapaxes(aff, 1, 2), cap)
    xin = jax.vmap(lambda hb, ib: hb[ib])(h, idx)
    hid = jax.nn.silu(jnp.einsum('becd,edf->becf', xin, w1)) * jnp.einsum('becd,edf->becf', xin, w3)
    out = jnp.einsum('becf,efd->becd', hid, w2) * gate[..., None].astype(h.dtype)
    return jax.vmap(lambda ob, ib: jnp.zeros((n, dm), ob.dtype).at[ib.reshape(-1)].add(ob.reshape(-1, dm)))(out, idx)


def _layer(xc, xl, c, c_ctx, lp, lam_init, cos, sin, last):
    need_ctx = not last
    sh1l, sc1l, g1l, sh2l, sc2l, g2l = [m[:, None, :] for m in _adaln_params(c, lp['ada_w'], lp['ada_b'])]
    sh1c, sc1c, g1c, sh2c, sc2c, g2c = _adaln_params(c_ctx, lp['ada_w'], lp['ada_b'])
    hl = _rmsnorm(xl, lp['norm1_g']) * (1.0 + sc1l) + sh1l
    hc = _rmsnorm(xc, lp['norm1_g']) * (1.0 + sc1c) + sh1c
    yc, yl = _mixer(hc, hl, lp, lam_init, cos, sin, need_ctx)
    xl = xl + g1l * yl
    hl = _rmsnorm(xl, lp['norm2_g']) * (1.0 + sc2l) + sh2l
    xl = xl + g2l * _expert_choice(hl, lp['router_w'], lp['exp_w1'], lp['exp_w3'], lp['exp_w2'])
    if need_ctx:
        xc = xc + g1c * yc
        hc = _rmsnorm(xc, lp['norm2_g']) * (1.0 + sc2c) + sh2c
        xc = xc + g2c * _expert_choice(hc, lp['router_w'], lp['exp_w1'], lp['exp_w3'], lp['exp_w2'])
    return xc, xl


def setup_inputs(seed: int = 0) -> dict:
    key = jax.random.key(seed)
    ks = list(jax.random.split(key, 40))
    f32 = jnp.float32

    def nrm(shape, scale):
        return jax.random.normal(ks.pop(), shape, f32) * scale

    D = D_MODEL
    return {
        "x": nrm((BATCH, SEQ, D), 1.0),
        "c": nrm((BATCH, D), 1.0),
        "ctx": nrm((BATCH, CTX_LEN, D), 1.0),
        "c_ctx": nrm((D,), 1.0),
        "norm1_g": 1.0 + nrm((DEPTH, D), 0.05),
        "norm2_g": 1.0 + nrm((DEPTH, D), 0.05),
        "ada_w": nrm((DEPTH, D, 6 * D), 0.3 * D ** -0.5),
        "ada_b": nrm((DEPTH, 6 * D), 0.01),
        "w_in": nrm((DEPTH, D, N_IN), D ** -0.5),
        "q_norm_g": 1.0 + nrm((DEPTH, A_HD), 0.05),
        "k_norm_g": 1.0 + nrm((DEPTH, A_HD), 0.05),
        "diff_lambda": nrm((DEPTH, 4, A_HD), 0.1),
        "diff_subln_g": 1.0 + nrm((DEPTH, 2 * A_HD), 0.05),
        "rwkv_conv_w": jnp.array([0.2, 0.6, 0.2], f32)[None, :, None] + nrm((DEPTH, 3, 3 * B_WIDTH), 0.05),
        "rwkv_w0": jnp.linspace(-6.0, -1.0, B_WIDTH, dtype=f32)[None, None, :] + nrm((DEPTH, 2, B_WIDTH), 0.3),
        "rwkv_w_up": nrm((DEPTH, 2, B_DECAY_RANK, B_WIDTH), 0.1 * B_DECAY_RANK ** -0.5),
        "rwkv_a0": nrm((DEPTH, 2, B_WIDTH), 0.1),
        "rwkv_a_up": nrm((DEPTH, 2, B_ICL_RANK, B_WIDTH), 0.3 * B_ICL_RANK ** -0.5),
        "rwkv_g_up": nrm((DEPTH, B_GATE_RANK, B_WIDTH), B_GATE_RANK ** -0.5),
        "rwkv_k_k": 0.85 + nrm((DEPTH, B_WIDTH), 0.05),
        "rwkv_k_a": 1.0 + nrm((DEPTH, B_WIDTH), 0.05),
        "rwkv_r_k": nrm((DEPTH, B_HEADS, B_HD), 0.1),
        "rwkv_ln_g": 1.0 + nrm((DEPTH, B_WIDTH), 0.05),
        "rwkv_ln_b": nrm((DEPTH, B_WIDTH), 0.01),
        "conv_w": nrm((DEPTH, 3, C_WIDTH), 3 ** -0.5),
        "w_branch": nrm((DEPTH, N_BRANCH, A_WIDTH, D), A_WIDTH ** -0.5),
        "w_out": nrm((DEPTH, D, D), D ** -0.5),
        "router_w": nrm((DEPTH, D, N_EXPERTS), D ** -0.5),
        "exp_w1": nrm((DEPTH, N_EXPERTS, D, EXPERT_FF), D ** -0.5),
        "exp_w3": nrm((DEPTH, N_EXPERTS, D, EXPERT_FF), D ** -0.5),
        "exp_w2": nrm((DEPTH, N_EXPERTS, EXPERT_FF, D), EXPERT_FF ** -0.5),
    }


def reference(x, c, ctx, c_ctx, norm1_g, norm2_g, ada_w, ada_b, w_in, q_norm_g, k_norm_g,
              diff_lambda, diff_subln_g, rwkv_conv_w, rwkv_w0, rwkv_w_up, rwkv_a0, rwkv_a_up,
              rwkv_g_up, rwkv_k_k, rwkv_k_a, rwkv_r_k, rwkv_ln_g, rwkv_ln_b, conv_w, w_branch,
              w_out, router_w, exp_w1, exp_w3, exp_w2):
    n_lat = x.shape[1]
    rows = n_lat // GRID_W
    cos, sin = _axial_rope_tables(rows)
    xc, xl = ctx, x
    for i in range(DEPTH):
        lp = {
            'norm1_g': norm1_g[i], 'norm2_g': norm2_g[i], 'ada_w': ada_w[i], 'ada_b': ada_b[i],
            'w_in': w_in[i], 'q_norm_g': q_norm_g[i], 'k_norm_g': k_norm_g[i],
            'diff_lambda': diff_lambda[i], 'diff_subln_g': diff_subln_g[i],
            'rwkv_conv_w': rwkv_conv_w[i], 'rwkv_w0': rwkv_w0[i], 'rwkv_w_up': rwkv_w_up[i],
            'rwkv_a0': rwkv_a0[i], 'rwkv_a_up': rwkv_a_up[i], 'rwkv_g_up': rwkv_g_up[i],
            'rwkv_k_k': rwkv_k_k[i], 'rwkv_k_a': rwkv_k_a[i], 'rwkv_r_k': rwkv_r_k[i],
            'rwkv_ln_g': rwkv_ln_g[i], 'rwkv_ln_b': rwkv_ln_b[i], 'conv_w': conv_w[i],
            'w_branch': w_branch[i], 'w_out': w_out[i], 'router_w': router_w[i],
            'exp_w1': exp_w1[i], 'exp_w3': exp_w3[i], 'exp_w2': exp_w2[i],
        }
        lam_init = 0.8 - 0.6 * math.exp(-0.3 * i)
        xc, xl = _layer(xc, xl, c, c_ctx, lp, lam_init, cos, sin, i == DEPTH - 1)
    return xl
```

```python
import contextlib
import math
import numpy as np
import ml_dtypes
import concourse.bass as bass
import concourse.mybir as mybir
from concourse.bass_utils import run_bass_kernel_spmd

F32 = mybir.dt.float32
BF16 = mybir.dt.bfloat16
AF = mybir.ActivationFunctionType
ALU = mybir.AluOpType

D = 1024
CTX = 256
N_IN = 7936
NE = 16
EPS = 1e-6
GN_EPS = 64e-5
NDQ = 10
PHASES = ["attn", "conv", "rwkv", "merge", "moe"]
RWS = 3


class Res:
    __slots__ = ("w", "r", "name", "excl")

    def __init__(self, name="", excl=False):
        self.w = {}
        self.r = {}
        self.name = name
        self.excl = excl


class Em:
    ENG = ("pe", "act", "dve", "pool", "sp")

    def __init__(self, nc, stack):
        self.nc = nc
        self.stack = stack
        self.eng = {"pe": nc.tensor, "act": nc.scalar, "dve": nc.vector,
                    "pool": nc.gpsimd, "sp": nc.sync}
        self.sem = {}
        self.cnt = {}
        for k in ("pe", "act", "dve", "pool"):
            self.sem[k] = stack.enter_context(nc.semaphore("s_" + k))
            self.cnt[k] = 0
        self.dslot = {}
        for q in ("sp", "act", "pool"):
            self.dslot[q] = 0
            for i in range(NDQ):
                key = "d_%s_%d" % (q, i)
                self.sem[key] = stack.enter_context(nc.semaphore("sd_%s_%d" % (q, i)))
                self.cnt[key] = 0
        self.seen = {e: {} for e in self.ENG}
        self.n_inst = 0
        self.n_wait = 0
        self.uid = 0

    def sb(self, shape, dt, name=None):
        self.uid += 1
        return self.stack.enter_context(
            self.nc.sbuf_tensor(name or ("t%d" % self.uid), list(shape), dt))

    def ps(self, shape, dt=F32, name=None):
        self.uid += 1
        return self.stack.enter_context(
            self.nc.psum_tensor(name or ("p%d" % self.uid), list(shape), dt))

    def _wait(self, e, key, c):
        if c <= 0:
            return
        s = self.seen[e]
        if s.get(key, 0) >= c:
            return
        self.eng[e].wait_ge(self.sem[key], c)
        s[key] = c
        self.n_wait += 1

    def _deps(self, e, reads, writes, own_key):
        skip_raw = own_key if own_key == "pe" else None
        for r in reads:
            for k, c in r.w.items():
                if k == skip_raw:
                    continue
                self._wait(e, k, c)
        for w in writes:
            for k, c in w.w.items():
                if k == skip_raw:
                    continue
                self._wait(e, k, c)
            for k, c in w.r.items():
                if k == own_key:
                    continue
                self._wait(e, k, c)

    def _commit(self, key, c, reads, writes):
        for r in reads:
            if r.r.get(key, 0) < c:
                r.r[key] = c
        for w in writes:
            w.w = {key: c}
            w.r = {}

    def op(self, e, fn, reads=(), writes=()):
        if any(r.excl for r in reads):
            writes = list(writes) + [r for r in reads if r.excl]
            reads = [r for r in reads if not r.excl]
        self._deps(e, reads, writes, e)
        ins = fn()
        self.cnt[e] += 1
        c = self.cnt[e]
        ins.then_inc(self.sem[e], 1)
        self._commit(e, c, reads, writes)
        self.n_inst += 1
        return ins

    def dma(self, q, out, in_, reads=(), writes=(), **kw):
        slot = self.dslot[q]
        self.dslot[q] = (slot + 1) % NDQ
        key = "d_%s_%d" % (q, slot)
        self._wait(q, key, self.cnt[key])
        self._deps(q, reads, writes, None)
        ins = self.eng[q].dma_start(out=out, in_=in_, **kw)
        self.cnt[key] += 16
        c = self.cnt[key]
        ins.then_inc(self.sem[key], 16)
        self._commit(key, c, reads, writes)
        self.n_inst += 1
        return ins

    def barrier(self):
        for e in self.ENG:
            for k, c in self.cnt.items():
                if k == e:
                    continue
                self._wait(e, k, c)

    def finish(self):
        for k, c in self.cnt.items():
            self._wait("sp", k, c)


class Rot:
    def __init__(self, em, n, shape, dt, psum=False):
        self.items = []
        for _ in range(n):
            t = em.ps(shape, dt) if psum else em.sb(shape, dt)
            self.items.append((t, Res()))
        self.i = 0

    def next(self):
        it = self.items[self.i]
        self.i = (self.i + 1) % len(self.items)
        return it


def host_consts(t_lat):
    c = {}
    c["ident"] = np.eye(128, dtype=np.float32)
    c["ones"] = np.ones((128, 128), np.float32)
    bd = np.zeros((128, 128), np.float32)
    bd[:64, :64] = 1.0
    bd[64:, 64:] = 1.0
    c["bd64"] = bd
    rm = np.zeros((128, 128), np.float32)
    for p in range(128):
        d = p % 64
        if d < 32:
            rm[p + 32, p] = -1.0
        else:
            rm[p - 32, p] = 1.0
    c["rotm"] = rm
    sel = np.zeros((16, 16, 128), np.float32)
    for e in range(16):
        sel[e, e, :] = 1.0
    c["sel"] = sel.reshape(16, 2048)
    ii = np.arange(128)
    ms_f = (ii[:, None] < ii[None, :]).astype(np.float32)
    mi_f = (ii[:, None] <= ii[None, :]).astype(np.float32)
    c["mskf"] = np.concatenate([ms_f, mi_f, ms_f, mi_f], axis=1)
    c["mskb"] = np.concatenate([ms_f.T, mi_f.T, ms_f.T, mi_f.T], axis=1)
    rst = np.ones((64, 512), np.float32)
    rst[:, ::128] = 0.0
    c["rst"] = rst
    half = 32
    inv = np.power(10000.0, -np.arange(0, half, 2, dtype=np.float32) / half).astype(np.float32)
    rows = t_lat // 64
    r = np.repeat(np.arange(rows, dtype=np.float32), 64)
    col = np.tile(np.arange(64, dtype=np.float32), rows)
    ang = np.concatenate([r[:, None] * inv, col[:, None] * inv], axis=-1).astype(np.float32)
    cos = np.cos(ang).astype(np.float32).T
    sin = np.sin(ang).astype(np.float32).T
    c["cosT"] = np.ascontiguousarray(np.tile(cos, (4, 1)))
    c["sinT"] = np.ascontiguousarray(np.tile(sin, (4, 1)))
    return c


class K:
    def __init__(self, t_lat, depth, lam_inits, dbg=None):
        self.TL = t_lat
        self.T = CTX + t_lat
        self.depth = depth
        self.lam_inits = lam_inits
        self.dbg = dbg or []
        T = self.T
        nc = self.nc = bass.Bass("TRN2", target_bir_lowering=False)
        self.inp = {}

        def din(name, shape, dt=F32):
            self.inp[name] = nc.dram_tensor(name, list(shape), dt, kind="ExternalInput").ap()
            return self.inp[name]

        L = depth
        din("x", [t_lat, D]); din("c", [D]); din("ctx", [CTX, D]); din("c_ctx", [D])
        din("norm1_g", [L, D]); din("norm2_g", [L, D]); din("ada_w", [L, D, 6 * D]); din("ada_b", [L, 6 * D])
        din("w_in", [L, D, N_IN]); din("q_norm_g", [L, 64]); din("k_norm_g", [L, 64])
        din("diff_lambda", [L, 4, 64]); din("diff_subln_g", [L, 128])
        din("rwkv_conv_w", [L, 3, 1536]); din("rwkv_w0", [L, 2, 512]); din("rwkv_w_up", [L, 2, 64, 512])
        din("rwkv_a0", [L, 2, 512]); din("rwkv_a_up", [L, 2, 64, 512]); din("rwkv_g_up", [L, 128, 512])
        din("rwkv_k_k", [L, 512]); din("rwkv_k_a", [L, 512]); din("rwkv_r_k", [L, 512])
        din("rwkv_ln_g", [L, 512]); din("rwkv_ln_b", [L, 512]); din("conv_w", [L, 3, 512])
        din("w_branch", [L, 3, 512, D]); din("w_out", [L, D, D]); din("router_w", [L, D, NE])
        if "moe" in PHASES:
            din("exp_w1", [L, NE, D, D]); din("exp_w3", [L, NE, D, D]); din("exp_w2", [L, NE, D, D])
        for k, v in host_consts(t_lat).items():
            din("cst_" + k, v.shape)
        self.out = nc.dram_tensor("out", [t_lat, D], F32, kind="ExternalOutput").ap()
        self.dbg_out = {}

        def scratch(name, shape, dt=F32):
            kind = "ExternalOutput" if name in self.dbg else "Internal"
            ap = nc.dram_tensor(name, list(shape), dt, kind=kind).ap()
            if name in self.dbg:
                self.dbg_out[name] = ap
            return ap

        self.xT = scratch("xT", [D, T]); self.r_xT = Res("xT")
        self.hT = scratch("hT", [D, T], BF16); self.r_hT = Res("hT")
        self.projT = scratch("projT", [N_IN, T]); self.r_projT = Res("projT")
        self.vaTM = scratch("vaTM", [T, 512], BF16); self.r_vaTM = Res("vaTM")
        self.yT = scratch("yT", [3 * 512, T], BF16); self.r_yT = Res("yT")
        self.affT = scratch("affT", [NE, T]); self.r_aff = Res("aff")
        self.coefT = scratch("coefT", [NE, T]); self.r_coef = Res("coef")

        with contextlib.ExitStack() as st:
            em = self.em = Em(nc, st)
            st.enter_context(nc.Block())
            self.consts(st)
            self.phase_transpose_in()
            for l in range(depth):
                self.layer(l)
            self.phase_transpose_out()
            em.barrier()
            em.finish()

    def chunks(self):
        res = [(0, CTX, 1)]
        t = CTX
        while t < self.T:
            n = min(512, self.T - t)
            res.append((t, n, 0))
            t += n
        return res

    def consts(self, st):
        em, nc = self.em, self.nc
        self.rc = Res("consts")
        self.ident = em.sb([128, 128], F32)
        self.ones = em.sb([128, 128], F32)
        self.bd64 = em.sb([128, 128], F32)
        self.rotm = em.sb([128, 128], F32)
        self.ones_bf = em.sb([128, 128], BF16)
        for t, nm in ((self.ident, "ident"), (self.ones, "ones"), (self.bd64, "bd64"), (self.rotm, "rotm")):
            em.dma("sp", t[:], self.inp["cst_" + nm][:, :], writes=[self.rc])
        em.op("dve", lambda: nc.vector.tensor_copy(out=self.ones_bf[:], in_=self.ones[:]),
              reads=[self.rc], writes=[self.rc])
        self.ccv = [EPS, GN_EPS, 1e-24, 0.0, 1.0]
        self.cc = em.sb([128, len(self.ccv)], F32)
        for i, v in enumerate(self.ccv):
            em.op("pool", lambda: nc.gpsimd.memset(self.cc[:, i:i + 1], float(v)), writes=[self.rc])

    def phase_transpose_in(self):
        em, nc = self.em, self.nc
        with contextlib.ExitStack() as st:
            em.stack, old = st, em.stack
            xin = Rot(em, 2, [128, D], F32)
            pst = Rot(em, 2, [128, 512], F32, psum=True)
            stg = Rot(em, 2, [128, 8, 128], F32)
            xTv = self.xT.rearrange("(kt p) t -> p kt t", p=128)
            for j in range(self.T // 128):
                t0 = j * 128
                src = self.inp["ctx"][t0:t0 + 128, :] if t0 < CTX else self.inp["x"][t0 - CTX:t0 - CTX + 128, :]
                xt, rx = xin.next()
                em.dma("sp", xt[:], src, writes=[rx])
                sg, rs = stg.next()
                for half in range(2):
                    pt, rp = pst.next()
                    for q in range(4):
                        kt = half * 4 + q
                        em.op("pe", lambda: nc.tensor.transpose(out=pt[:, q * 128:(q + 1) * 128],
                                                                in_=xt[:, kt * 128:(kt + 1) * 128],
                                                                identity=self.ident[:]),
                              reads=[rx, self.rc], writes=[rp])
                    e = "dve" if half == 0 else "act"
                    dst = sg[:, half * 4:(half + 1) * 4, :]
                    srcp = pt[:].rearrange("p (q t) -> p q t", q=4)
                    if e == "dve":
                        em.op("dve", lambda: nc.vector.tensor_copy(out=dst, in_=srcp), reads=[rp], writes=[rs])
                    else:
                        em.op("act", lambda: nc.scalar.copy(out=dst, in_=srcp), reads=[rp], writes=[rs])
                em.dma("act", xTv[:, :, t0:t0 + 128], sg[:], reads=[rs], writes=[self.r_xT])
            em.barrier()
            em.stack = old

    def phase_transpose_out(self):
        em, nc = self.em, self.nc
        with contextlib.ExitStack() as st:
            em.stack, old = st, em.stack
            xin = Rot(em, 2, [128, 8, 128], F32)
            pst = Rot(em, 2, [128, 512], F32, psum=True)
            stg = Rot(em, 2, [128, D], F32)
            xTv = self.xT.rearrange("(kt p) t -> p kt t", p=128)
            self.r_out = Res("out")
            for j in range(CTX // 128, self.T // 128):
                t0 = j * 128
                xt, rx = xin.next()
                em.dma("sp", xt[:], xTv[:, :, t0:t0 + 128], reads=[self.r_xT], writes=[rx])
                sg, rs = stg.next()
                for half in range(2):
                    pt, rp = pst.next()
                    for q in range(4):
                        kt = half * 4 + q
                        em.op("pe", lambda: nc.tensor.transpose(out=pt[:, q * 128:(q + 1) * 128],
                                                                in_=xt[:, kt, :], identity=self.ident[:]),
                              reads=[rx, self.rc], writes=[rp])
                    dst = sg[:, half * 512:(half + 1) * 512]
                    if half == 0:
                        em.op("dve", lambda: nc.vector.tensor_copy(out=dst, in_=pt[:]), reads=[rp], writes=[rs])
                    else:
                        em.op("act", lambda: nc.scalar.copy(out=dst, in_=pt[:]), reads=[rp], writes=[rs])
                em.dma("act", self.out[t0 - CTX:t0 - CTX + 128, :], sg[:], reads=[rs], writes=[self.r_out])
            em.barrier()
            em.stack = old

    def layer(self, l):
        self.phase_ada(l)
        self.phase_norm(l, which=1)
        self.phase_inproj(l)
        if "attn" in PHASES:
            self.phase_attn(l)
        if "conv" in PHASES:
            self.phase_conv(l)
        if "rwkv" in PHASES:
            self.phase_rwkv(l)
        else:
            em, nc = self.em, self.nc
            with contextlib.ExitStack() as st:
                em.stack, old = st, em.stack
                z = em.sb([128, 512], BF16); rz = Res()
                em.op("pool", lambda: nc.gpsimd.memset(z[:], 0.0), writes=[rz])
                for j in range(4):
                    for (t0, n, cond) in self.chunks():
                        em.dma("act", self.yT[512 + j * 128:512 + (j + 1) * 128, t0:t0 + n], z[:, :n], reads=[rz], writes=[self.r_yT])
                em.barrier()
                em.stack = old
        if "merge" in PHASES:
            self.phase_merge(l)
        if "moe" in PHASES:
            self.phase_norm(l, which=2)
            self.phase_moe(l)

    def phase_attn(self, l):
        em, nc = self.em, self.nc
        T = self.T
        NT = T // 128
        with contextlib.ExitStack() as st:
            em.stack, old = st, em.stack
            rs_ = Res("attn_setup")
            qg = em.sb([128, 1], F32); kg = em.sb([128, 1], F32); sgc = em.sb([128, 1], F32)
            for hh in range(2):
                em.dma("sp", qg[hh * 64:(hh + 1) * 64, :], self.inp["q_norm_g"][l].rearrange("(p o) -> p o", o=1), writes=[rs_])
                em.dma("sp", kg[hh * 64:(hh + 1) * 64, :], self.inp["k_norm_g"][l].rearrange("(p o) -> p o", o=1), writes=[rs_])
            em.dma("sp", sgc[:], self.inp["diff_subln_g"][l].rearrange("(p o) -> p o", o=1), writes=[rs_])
            lam_init = self.lam_inits[l]
            em.op("dve", lambda: nc.vector.tensor_scalar(out=sgc[:], in0=sgc[:], scalar1=float(1.0 - lam_init),
                                                         scalar2=None, op0=ALU.mult), reads=[rs_], writes=[rs_])
            lv = em.sb([64, 4], F32)
            em.dma("sp", lv[:], self.inp["diff_lambda"][l].rearrange("f d -> d f"), writes=[rs_],
                   allow_slow_non_contiguous=True)
            pr = em.sb([64, 2], F32)
            lvv = lv[:].rearrange("p (a b) -> p a b", b=2)
            em.op("dve", lambda: nc.vector.tensor_tensor(out=pr[:], in0=lvv[:, :, 0], in1=lvv[:, :, 1], op=ALU.mult),
                  reads=[rs_], writes=[rs_])
            pmisc = em.ps([128, 512], F32); rpm = Res()
            em.op("pe", lambda: nc.tensor.matmul(pmisc[:, 0:2], lhsT=self.ones[0:64, :], rhs=pr[:], start=True, stop=True),
                  reads=[rs_, self.rc], writes=[rpm])
            el = em.sb([128, 2], F32)
            em.op("act", lambda: nc.scalar.activation(out=el[:], in_=pmisc[:, 0:2], func=AF.Exp), reads=[rpm], writes=[rs_])
            neglam = em.sb([128, 1], F32)
            em.op("dve", lambda: nc.vector.scalar_tensor_tensor(out=neglam[:], in0=el[:, 1:2], scalar=float(-lam_init),
                                                                in1=el[:, 0:1], op0=ALU.add, op1=ALU.subtract),
                  reads=[rs_], writes=[rs_])

            KT = em.sb([128, T], BF16); rK = Res()
            QT = em.sb([128, T], BF16); rQ = Res()
            Vh = em.sb([128, NT, 128], BF16); rV = Res()
            src = Rot(em, 2, [128, 512], F32)
            sqr = Rot(em, 2, [128, 512], F32)
            rsd = Rot(em, 2, [128, 512], F32)
            knr = Rot(em, 2, [128, 512], F32)
            cosr = Rot(em, 2, [128, 512], F32)
            sinr = Rot(em, 2, [128, 512], F32)
            t1r = Rot(em, 2, [128, 512], F32)
            t2r = Rot(em, 2, [128, 512], F32)
            pS = Rot(em, 3, [128, 512], F32, psum=True)
            pO = Rot(em, 2, [128, 512], F32, psum=True)
            pD = Rot(em, 2, [128, 512], F32, psum=True)
            Pr = Rot(em, 3, [128, 512], BF16)
            omr = Rot(em, 2, [128, 512], F32)
            yst = Rot(em, 2, [128, 512], BF16)

            def qk_prep(row0, gcol, dst, rdst):
                for (t0, n, cond) in self.chunks():
                    s, r_s = src.next()
                    em.dma("sp", s[:, :n], self.projT[row0:row0 + 128, t0:t0 + n], reads=[self.r_projT], writes=[r_s])
                    q2, r_q2 = sqr.next()
                    em.op("act", lambda: nc.scalar.activation(out=q2[:, :n], in_=s[:, :n], func=AF.Square),
                          reads=[r_s], writes=[r_q2])
                    em.op("pe", lambda: nc.tensor.matmul(pmisc[:, :n], lhsT=self.bd64[:], rhs=q2[:, :n], start=True, stop=True),
                          reads=[r_q2, self.rc], writes=[rpm])
                    rd, r_rd = rsd.next()
                    em.op("act", lambda: nc.scalar.activation(out=rd[:, :n], in_=pmisc[:, :n], func=AF.Sqrt,
                                                              scale=1.0 / 64, bias=self.eps_col(EPS)),
                          reads=[rpm, self.rc], writes=[r_rd])
                    em.op("dve", lambda: nc.vector.reciprocal(out=rd[:, :n], in_=rd[:, :n]), reads=[r_rd], writes=[r_rd])
                    kn, r_kn = knr.next()
                    em.op("dve", lambda: nc.vector.scalar_tensor_tensor(out=kn[:, :n], in0=s[:, :n], scalar=gcol[:],
                                                                        in1=rd[:, :n], op0=ALU.mult, op1=ALU.mult),
                          reads=[r_s, r_rd, rs_], writes=[r_kn])
                    if cond == 1:
                        em.op("act", lambda: nc.scalar.copy(out=dst[:, t0:t0 + n], in_=kn[:, :n]), reads=[r_kn], writes=[rdst])
                    else:
                        em.op("pe", lambda: nc.tensor.matmul(pmisc[:, :n], lhsT=self.rotm[:], rhs=kn[:, :n], start=True, stop=True),
                              reads=[r_kn, self.rc], writes=[rpm])
                        cs_, r_c = cosr.next(); sn_, r_sn = sinr.next()
                        em.dma("sp", cs_[:, :n], self.inp["cst_cosT"][:, t0 - CTX:t0 - CTX + n], writes=[r_c])
                        em.dma("sp", sn_[:, :n], self.inp["cst_sinT"][:, t0 - CTX:t0 - CTX + n], writes=[r_sn])
                        t1, r_t1 = t1r.next(); t2, r_t2 = t2r.next()
                        em.op("pool", lambda: nc.gpsimd.tensor_tensor(out=t1[:, :n], in0=kn[:, :n], in1=cs_[:, :n], op=ALU.mult),
                              reads=[r_kn, r_c], writes=[r_t1])
                        em.op("dve", lambda: nc.vector.tensor_tensor(out=t2[:, :n], in0=pmisc[:, :n], in1=sn_[:, :n], op=ALU.mult),
                              reads=[rpm, r_sn], writes=[r_t2])
                        em.op("pool", lambda: nc.gpsimd.tensor_tensor(out=dst[:, t0:t0 + n], in0=t1[:, :n], in1=t2[:, :n], op=ALU.add),
                              reads=[r_t1, r_t2], writes=[rdst])

            for h in range(4):
                qk_prep(512 + h * 128, kg, KT, rK)
                qk_prep(h * 128, qg, QT, rQ)
                em.dma("sp", Vh[:], self.vaTM[:, h * 128:(h + 1) * 128].rearrange("(j p) e -> p j e", p=128),
                       reads=[self.r_vaTM], writes=[rV])
                for (t0, n, cond) in self.chunks():
                    kts = range(0, CTX // 128) if cond == 1 else range(0, NT)
                    nk = len(kts)
                    oms = []
                    for m in range(2):
                        po, r_po = pO.next(); pd, r_pd = pD.next()
                        for i, kt in enumerate(kts):
                            ps_, r_ps = pS.next()
                            em.op("pe", lambda: nc.tensor.matmul(ps_[:, :n], lhsT=KT[m * 64:(m + 1) * 64, kt * 128:(kt + 1) * 128],
                                                                 rhs=QT[m * 64:(m + 1) * 64, t0:t0 + n], start=True, stop=True),
                                  reads=[rK, rQ], writes=[r_ps])
                            P, r_P = Pr.next()
                            em.op("act", lambda: nc.scalar.activation(out=P[:, :n], in_=ps_[:, :n], func=AF.Exp, scale=0.125),
                                  reads=[r_ps], writes=[r_P])
                            em.op("pe", lambda: nc.tensor.matmul(po[:, :n], lhsT=Vh[:, kt, :], rhs=P[:, :n],
                                                                 start=(i == 0), stop=(i == nk - 1)),
                                  reads=[rV, r_P], writes=[r_po])
                            em.op("pe", lambda: nc.tensor.matmul(pd[:, :n], lhsT=self.ones_bf[:], rhs=P[:, :n],
                                                                 start=(i == 0), stop=(i == nk - 1)),
                                  reads=[self.rc, r_P], writes=[r_pd])
                        rd, r_rd = rsd.next()
                        em.op("dve", lambda: nc.vector.reciprocal(out=rd[:, :n], in_=pd[:, :n]), reads=[r_pd], writes=[r_rd])
                        om, r_om = omr.next()
                        em.op("dve", lambda: nc.vector.tensor_tensor(out=om[:, :n], in0=po[:, :n], in1=rd[:, :n], op=ALU.mult),
                              reads=[r_po, r_rd], writes=[r_om])
                        oms.append((om, r_om))
                    (o0, r0), (o1, r1) = oms
                    em.op("dve", lambda: nc.vector.scalar_tensor_tensor(out=o0[:, :n], in0=o1[:, :n], scalar=neglam[:],
                                                                        in1=o0[:, :n], op0=ALU.mult, op1=ALU.add),
                          reads=[r1, r0, rs_], writes=[r0])
                    q2, r_q2 = sqr.next()
                    em.op("act", lambda: nc.scalar.activation(out=q2[:, :n], in_=o0[:, :n], func=AF.Square),
                          reads=[r0], writes=[r_q2])
                    em.op("pe", lambda: nc.tensor.matmul(pmisc[:, :n], lhsT=self.ones[:], rhs=q2[:, :n], start=True, stop=True),
                          reads=[r_q2, self.rc], writes=[rpm])
                    rd, r_rd = rsd.next()
                    em.op("act", lambda: nc.scalar.activation(out=rd[:, :n], in_=pmisc[:, :n], func=AF.Sqrt,
                                                              scale=1.0 / 128, bias=self.eps_col(EPS)),
                          reads=[rpm, self.rc], writes=[r_rd])
                    em.op("dve", lambda: nc.vector.reciprocal(out=rd[:, :n], in_=rd[:, :n]), reads=[r_rd], writes=[r_rd])
                    ys, r_ys = yst.next()
                    em.op("dve", lambda: nc.vector.scalar_tensor_tensor(out=ys[:, :n], in0=o0[:, :n], scalar=sgc[:],
                                                                        in1=rd[:, :n], op0=ALU.mult, op1=ALU.mult),
                          reads=[r0, r_rd, rs_], writes=[r_ys])
                    em.dma("act", self.yT[h * 128:(h + 1) * 128, t0:t0 + n], ys[:, :n], reads=[r_ys], writes=[self.r_yT])
            em.barrier()
            em.stack = old

    def seq_bounds(self, cond):
        return (0, CTX) if cond == 1 else (CTX, self.T)

    def phase_conv(self, l):
        em, nc = self.em, self.nc
        with contextlib.ExitStack() as st:
            em.stack, old = st, em.stack
            cw = em.sb([128, 4, 3], F32); rcw = Res()
            for k in range(3):
                em.dma("sp", cw[:, :, k], self.inp["conv_w"][l, k].rearrange("(j p) -> p j", p=128), writes=[rcw],
                       allow_slow_non_contiguous=True)
            cgr = Rot(em, 2, [128, 514], F32); xvr = Rot(em, 2, [128, 514], F32); bgr = Rot(em, 2, [128, 512], F32)
            ur = Rot(em, 2, [128, 514], F32); ar = Rot(em, 2, [128, 512], F32); yst = Rot(em, 2, [128, 512], BF16)
            for j in range(4):
                for (t0, n, cond) in self.chunks():
                    lo, hi = self.seq_bounds(cond)
                    a0 = max(lo, t0 - 1); a1 = min(hi, t0 + n + 1)
                    off = a0 - (t0 - 1)
                    cg, r_cg = cgr.next(); xv, r_xv = xvr.next(); bg, r_bg = bgr.next()
                    em.op("pool", lambda: nc.gpsimd.memset(cg[:], 0.0), writes=[r_cg])
                    em.dma("sp", cg[:, off:off + (a1 - a0)], self.projT[3840 + j * 128:3840 + (j + 1) * 128, a0:a1],
                           reads=[self.r_projT], writes=[r_cg])
                    em.dma("sp", xv[:, off:off + (a1 - a0)], self.projT[4352 + j * 128:4352 + (j + 1) * 128, a0:a1],
                           reads=[self.r_projT], writes=[r_xv])
                    em.dma("sp", bg[:, :n], self.projT[3328 + j * 128:3328 + (j + 1) * 128, t0:t0 + n],
                           reads=[self.r_projT], writes=[r_bg])
                    u, r_u = ur.next()
                    em.op("pool", lambda: nc.gpsimd.tensor_tensor(out=u[:, off:off + (a1 - a0)], in0=cg[:, off:off + (a1 - a0)],
                                                                  in1=xv[:, off:off + (a1 - a0)], op=ALU.mult),
                          reads=[r_cg, r_xv], writes=[r_u])
                    if off > 0:
                        em.op("pool", lambda: nc.gpsimd.memset(u[:, 0:1], 0.0), writes=[r_u])
                    if off + (a1 - a0) < n + 2:
                        em.op("pool", lambda: nc.gpsimd.memset(u[:, n + 1:n + 2], 0.0), writes=[r_u])
                    a, r_a = ar.next()
                    em.op("dve", lambda: nc.vector.tensor_scalar(out=a[:, :n], in0=u[:, 0:n], scalar1=cw[:, j, 0:1], scalar2=None,
                                                                 op0=ALU.mult), reads=[r_u, rcw], writes=[r_a])
                    em.op("dve", lambda: nc.vector.scalar_tensor_tensor(out=a[:, :n], in0=u[:, 1:n + 1], scalar=cw[:, j, 1:2],
                                                                        in1=a[:, :n], op0=ALU.mult, op1=ALU.add),
                          reads=[r_u, rcw, r_a], writes=[r_a])
                    em.op("dve", lambda: nc.vector.scalar_tensor_tensor(out=a[:, :n], in0=u[:, 2:n + 2], scalar=cw[:, j, 2:3],
                                                                        in1=a[:, :n], op0=ALU.mult, op1=ALU.add),
                          reads=[r_u, rcw, r_a], writes=[r_a])
                    ys, r_ys = yst.next()
                    em.op("pool", lambda: nc.gpsimd.tensor_tensor(out=ys[:, :n], in0=a[:, :n], in1=bg[:, :n], op=ALU.mult),
                          reads=[r_a, r_bg], writes=[r_ys])
                    em.dma("act", self.yT[1024 + j * 128:1024 + (j + 1) * 128, t0:t0 + n], ys[:, :n],
                           reads=[r_ys], writes=[self.r_yT])
            em.barrier()
            em.stack = old

    def phase_rwkv(self, l):
        em, nc = self.em, self.nc
        T = self.T
        NCH = T // 128
        I64 = self.ident[0:64, 0:64]
        O64 = self.ones[0:64, 0:64]
        with contextlib.ExitStack() as st:
            em.stack, old = st, em.stack
            rs_ = Res("rwkv_setup")
            msk = [em.sb([128, 512], F32), em.sb([128, 512], F32)]
            em.dma("sp", msk[0][:], self.inp["cst_mskf"][:, :], writes=[rs_])
            em.dma("sp", msk[1][:], self.inp["cst_mskb"][:, :], writes=[rs_])
            rst = em.sb([64, 512], F32)
            em.dma("sp", rst[:], self.inp["cst_rst"][:, :], writes=[rs_])
            wup = em.sb([64, 2, 512], F32); aup = em.sb([64, 2, 512], F32); gup = em.sb([128, 512], F32)
            for d in range(2):
                em.dma("sp", wup[:, d, :], self.inp["rwkv_w_up"][l, d], writes=[rs_])
                em.dma("sp", aup[:, d, :], self.inp["rwkv_a_up"][l, d], writes=[rs_])
            em.dma("sp", gup[:], self.inp["rwkv_g_up"][l], writes=[rs_])
            pc = em.sb([64, 16], F32); rpc = Res()
            ysum = em.sb([64, T], F32); rys = Res()
            Qb = Rot(em, 2, [64, 512], F32); Yb = Rot(em, 2, [64, 512], F32)
            Gb = Rot(em, 2, [64, 4, 64], F32); Hb = Rot(em, 2, [64, 4, 64], F32)
            pdc = em.sb([64, 2], F32); r_pdc = Res()
            bk = [em.ps([128, 512], F32) for _ in range(8)]
            RB = [Res("psum_bank%d" % i, excl=True) for i in range(8)]
            r_pBK = RB[0]; r_pNT = RB[1]; r_pX1 = RB[1]
            r_pz = [RB[2], RB[3]]; r_pp = [RB[2], RB[3]]; r_ppt = [RB[2], RB[3]]
            r_ptr = RB[4]; r_pmm = RB[5]; r_pQ = RB[5]; r_pYi = RB[5]
            r_py = [RB[6], RB[6]]; r_pst = [RB[6], RB[6]]; r_pG = RB[6]; r_pH = RB[6]; r_prd = RB[7]
            pBK = bk[0]
            pNT = bk[1][:, 0:128]; pX1 = bk[1][:, 128:192]
            pz = [bk[2][:, 0:128], bk[3][:, 0:128]]
            pp = [bk[2][:, 128:256], bk[3][:, 128:256]]; ppt = [bk[2][:, 256:384], bk[3][:, 256:384]]
            ptr = bk[4][:, 0:256]
            pmm = bk[5][0:64, :]
            pQ = bk[4][0:64, 256:384]; pYi = bk[4][0:64, 384:512]
            r_pQ = RB[4]; r_pYi = RB[4]
            py = [bk[6][0:64, 0:128], bk[6][0:64, 128:256]]; pst = [bk[6][0:64, 256:320], bk[6][0:64, 320:384]]
            pG = bk[6][0:64, 384:448]; pH = bk[6][0:64, 448:512]
            prd = bk[7][0:64, :]
            xh = [Rot(em, 1, [64, 514], F32) for _ in range(3)]
            cv = [Rot(em, 2, [64, 512], F32) for _ in range(3)]
            wlr = Rot(em, 2, [64, 512], F32); alr = Rot(em, 2, [64, 512], F32)
            kkr = Rot(em, 2, [64, 512], F32)
            tmps = {nm: (em.sb([64, 512], F32), Res()) for nm in ('sq', 'nr', 'ld', 'a', 'kd', 'bn', 'L', 'Lb', 'Ei', 'dl')}
            ARr = Rot(em, 2, [64, 4, 2, 128], F32)
            Bfr = Rot(em, 2, [64, 512], F32); Kfr = Rot(em, 2, [64, 512], F32)
            Bgr = Rot(em, 2, [64, 512], F32); Kgr = Rot(em, 2, [64, 512], F32)
            Er = Rot(em, 2, [64, 512], F32)
            dgr = Rot(em, 2, [64, 64], F32)
            tokr = Rot(em, 2, [128, 256], F32)
            NBr = Rot(em, 2, [128, 512], F32)
            NTr = Rot(em, 3, [128, 128], F32)
            Pr_ = Rot(em, 3, [128, 128], F32)
            Zr = Rot(em, 3, [128, 128], F32)
            Str = Rot(em, 2, [64, 64], F32)
            glr = Rot(em, 2, [128, 512], F32)
            ybr = Rot(em, 2, [64, 512], BF16)

            def V(e):
                return nc.vector if e == "dve" else nc.gpsimd

            for h in range(8):
                hs = slice(h * 64, (h + 1) * 64)
                cwv = self.inp["rwkv_conv_w"][l]
                for q in range(3):
                    for k in range(3):
                        em.dma("sp", pc[:, q * 3 + k:q * 3 + k + 1],
                               cwv[k, q * 512 + h * 64:q * 512 + (h + 1) * 64].rearrange("(p o) -> p o", o=1), writes=[rpc])
                for i, nm in ((9, "rwkv_k_k"), (10, "rwkv_k_a"), (12, "rwkv_r_k"), (13, "rwkv_ln_g"), (14, "rwkv_ln_b")):
                    em.dma("sp", pc[:, i:i + 1], self.inp[nm][l, hs].rearrange("(p o) -> p o", o=1), writes=[rpc])
                em.op("dve", lambda: nc.vector.tensor_scalar(out=pc[:, 11:12], in0=pc[:, 10:11], scalar1=-1.0, scalar2=1.0,
                                                             op0=ALU.mult, op1=ALU.add), reads=[rpc], writes=[rpc])
                for d in range(2):
                    em.dma("sp", pdc[:, 0:1], self.inp["rwkv_w0"][l, d, hs].rearrange("(p o) -> p o", o=1), writes=[r_pdc])
                    em.dma("sp", pdc[:, 1:2], self.inp["rwkv_a0"][l, d, hs].rearrange("(p o) -> p o", o=1), writes=[r_pdc])
                    msk_d = msk[d]
                    mskT = msk[1 - d][:, 0:128]
                    chs = self.chunks()
                    blocks = chs if d == 0 else [chs[0]] + chs[:0:-1]
                    S, r_S = Str.next()
                    em.op("pool", lambda: nc.gpsimd.memset(S[:], 0.0), writes=[r_S])
                    for (t0, n, cond) in blocks:
                        nch = n // 128
                        Qs, rQs = Qb.next(); Ys, rYs = Yb.next(); Gs, rGs = Gb.next(); Hs, rHs = Hb.next()
                        lo, hi = self.seq_bounds(cond)
                        a0_ = max(lo, t0 - 1); a1_ = min(hi, t0 + n + 1)
                        off = a0_ - (t0 - 1); ln_ = a1_ - a0_
                        cvt = []
                        for q in range(3):
                            x_, r_x = xh[q].next()
                            if off > 0 or off + ln_ < n + 2:
                                em.op("pool", lambda: nc.gpsimd.memset(x_[:], 0.0), writes=[r_x])
                            row = 1536 + q * 512 + h * 64
                            em.dma("sp", x_[:, off:off + ln_], self.projT[row:row + 64, a0_:a1_], reads=[self.r_projT], writes=[r_x])
                            c_, r_c = cv[q].next()
                            e = "dve" if q != 1 else "pool"
                            em.op(e, lambda: V(e).tensor_scalar(out=c_[:, :n], in0=x_[:, 0:n], scalar1=pc[:, q * 3:q * 3 + 1], scalar2=None,
                                                                op0=ALU.mult), reads=[r_x, rpc], writes=[r_c])
                            for k in (1, 2):
                                em.op("dve", lambda: nc.vector.scalar_tensor_tensor(out=c_[:, :n], in0=x_[:, k:n + k],
                                                                                    scalar=pc[:, q * 3 + k:q * 3 + k + 1], in1=c_[:, :n],
                                                                                    op0=ALU.mult, op1=ALU.add),
                                      reads=[r_x, rpc, r_c], writes=[r_c])
                            cvt.append((c_, r_c))
                        (r_, r_r), (k_, r_k), (v_, r_v) = cvt
                        wl_, r_wl = wlr.next(); al_, r_al = alr.next()
                        em.dma("sp", wl_[:, :n], self.projT[3072:3136, t0:t0 + n], reads=[self.r_projT], writes=[r_wl])
                        em.dma("sp", al_[:, :n], self.projT[3136:3200, t0:t0 + n], reads=[self.r_projT], writes=[r_al])
                        em.op("act", lambda: nc.scalar.activation(out=wl_[:, :n], in_=wl_[:, :n], func=AF.Tanh), reads=[r_wl], writes=[r_wl])
                        kk, r_kk = kkr.next()
                        em.op("pool", lambda: nc.gpsimd.tensor_scalar(out=kk[:, :n], in0=k_[:, :n], scalar1=pc[:, 9:10], scalar2=None, op0=ALU.mult),
                              reads=[r_k, rpc], writes=[r_kk])
                        sq, r_sq = tmps['sq']
                        em.op("act", lambda: nc.scalar.activation(out=sq[:, :n], in_=kk[:, :n], func=AF.Square), reads=[r_kk], writes=[r_sq])
                        em.op("pe", lambda: nc.tensor.matmul(pmm[:, :n], lhsT=O64, rhs=sq[:, :n], start=True, stop=True),
                              reads=[r_sq, self.rc], writes=[r_pmm])
                        nr, r_nr = tmps['nr']
                        em.op("act", lambda: nc.scalar.activation(out=nr[:, :n], in_=pmm[:, :n], func=AF.Sqrt), reads=[r_pmm], writes=[r_nr])
                        em.op("dve", lambda: nc.vector.tensor_scalar(out=nr[:, :n], in0=nr[:, :n], scalar1=1e-12, scalar2=None, op0=ALU.max),
                              reads=[r_nr], writes=[r_nr])
                        em.op("dve", lambda: nc.vector.reciprocal(out=nr[:, :n], in_=nr[:, :n]), reads=[r_nr], writes=[r_nr])
                        em.op("dve", lambda: nc.vector.tensor_tensor(out=kk[:, :n], in0=kk[:, :n], in1=nr[:, :n], op=ALU.mult),
                              reads=[r_kk, r_nr], writes=[r_kk])
                        em.op("pe", lambda: nc.tensor.matmul(pmm[:, :n], lhsT=wup[:, d, hs], rhs=wl_[:, :n], start=True, stop=True),
                              reads=[rs_, r_wl], writes=[r_pmm])
                        ld, r_ld = tmps['ld']
                        em.op("act", lambda: nc.scalar.activation(out=ld[:, :n], in_=pmm[:, :n], func=AF.Sigmoid, bias=pdc[:, 0:1], scale=1.0),
                              reads=[r_pmm, r_pdc], writes=[r_ld])
                        em.op("dve", lambda: nc.vector.tensor_scalar(out=ld[:, :n], in0=ld[:, :n], scalar1=-0.6065306597126334, scalar2=None,
                                                                     op0=ALU.mult), reads=[r_ld], writes=[r_ld])
                        em.op("pe", lambda: nc.tensor.matmul(pmm[:, :n], lhsT=aup[:, d, hs], rhs=al_[:, :n], start=True, stop=True),
                              reads=[rs_, r_al], writes=[r_pmm])
                        a_, r_a = tmps['a']
                        em.op("act", lambda: nc.scalar.activation(out=a_[:, :n], in_=pmm[:, :n], func=AF.Sigmoid, bias=pdc[:, 1:2], scale=1.0),
                              reads=[r_pmm, r_pdc], writes=[r_a])
                        kd, r_kd = tmps['kd']
                        em.op("dve", lambda: nc.vector.tensor_scalar(out=kd[:, :n], in0=a_[:, :n], scalar1=pc[:, 10:11], scalar2=pc[:, 11:12],
                                                                     op0=ALU.mult, op1=ALU.add), reads=[r_a, rpc], writes=[r_kd])
                        em.op("dve", lambda: nc.vector.tensor_tensor(out=kd[:, :n], in0=kd[:, :n], in1=k_[:, :n], op=ALU.mult),
                              reads=[r_kd, r_k], writes=[r_kd])
                        em.op("pool", lambda: nc.gpsimd.tensor_tensor(out=a_[:, :n], in0=a_[:, :n], in1=kk[:, :n], op=ALU.mult),
                              reads=[r_a, r_kk], writes=[r_a])
                        b_, r_b = a_, r_a
                        bn, r_bn = tmps['bn']
                        em.op("dve", lambda: nc.vector.scalar_tensor_tensor(out=bn[:, :n], in0=r_[:, :n], scalar=pc[:, 12:13], in1=kd[:, :n],
                                                                            op0=ALU.mult, op1=ALU.mult), reads=[r_r, rpc, r_kd], writes=[r_bn])
                        em.op("pe", lambda: nc.tensor.matmul(pmm[:, :n], lhsT=O64, rhs=bn[:, :n], start=True, stop=True),
                              reads=[r_bn, self.rc], writes=[r_pmm])
                        if d == 0:
                            em.op("dve", lambda: nc.vector.tensor_tensor(out=ysum[:, t0:t0 + n], in0=pmm[:, :n], in1=v_[:, :n], op=ALU.mult),
                                  reads=[r_pmm, r_v], writes=[rys])
                        else:
                            em.op("dve", lambda: nc.vector.tensor_tensor(out=bn[:, :n], in0=pmm[:, :n], in1=v_[:, :n], op=ALU.mult),
                                  reads=[r_pmm, r_v], writes=[r_bn])
                            em.op("pool", lambda: nc.gpsimd.tensor_tensor(out=ysum[:, t0:t0 + n], in0=ysum[:, t0:t0 + n], in1=bn[:, :n], op=ALU.add),
                                  reads=[r_bn, rys], writes=[rys])
                        L, r_L = tmps['L']
                        em.op("dve", lambda: nc.vector.tensor_tensor_scan(out=L[:, :n], data0=rst[:, :n], data1=ld[:, :n], initial=0.0,
                                                                          op0=ALU.mult, op1=ALU.add), reads=[rs_, r_ld], writes=[r_L])
                        if d == 1:
                            L3 = L[:, :n].rearrange("p (c t) -> p c t", t=128)
                            tot = L3[:, :, 127:128].to_broadcast([64, nch, 128])
                            Lb, r_Lb = tmps['Lb']
                            Lb3 = Lb[:, :n].rearrange("p (c t) -> p c t", t=128)
                            em.op("dve", lambda: nc.vector.tensor_tensor(out=Lb3, in0=tot, in1=L3, op=ALU.subtract), reads=[r_L], writes=[r_Lb])
                            em.op("dve", lambda: nc.vector.tensor_tensor(out=Lb[:, :n], in0=Lb[:, :n], in1=ld[:, :n], op=ALU.add),
                                  reads=[r_Lb, r_ld], writes=[r_Lb])
                            L, r_L = Lb, r_Lb
                        E, r_E = Er.next()
                        em.op("act", lambda: nc.scalar.activation(out=E[:, :n], in_=L[:, :n], func=AF.Exp), reads=[r_L], writes=[r_E])
                        Ei, r_Ei = tmps['Ei']
                        em.op("act", lambda: nc.scalar.activation(out=Ei[:, :n], in_=L[:, :n], func=AF.Exp, scale=-1.0), reads=[r_L], writes=[r_Ei])
                        em.op("dve", lambda: nc.vector.tensor_tensor(out=ld[:, :n], in0=L[:, :n], in1=ld[:, :n], op=ALU.subtract),
                              reads=[r_L, r_ld], writes=[r_ld])
                        em.op("act", lambda: nc.scalar.activation(out=ld[:, :n], in_=ld[:, :n], func=AF.Exp), reads=[r_ld], writes=[r_ld])
                        AR, r_AR = ARr.next()
                        kk3 = kk[:, :n].rearrange("p (c t) -> p c t", t=128)
                        ep3 = ld[:, :n].rearrange("p (c t) -> p c t", t=128)
                        em.op("dve", lambda: nc.vector.scalar_tensor_tensor(out=AR[:, :nch, 0, :], in0=kk3, scalar=-1.0, in1=ep3,
                                                                            op0=ALU.mult, op1=ALU.mult), reads=[r_kk, r_ld], writes=[r_AR])
                        em.op("pool", lambda: nc.gpsimd.tensor_tensor(out=AR[:, :nch, 1, :], in0=r_[:, :n].rearrange("p (c t) -> p c t", t=128),
                                                                      in1=E[:, :n].rearrange("p (c t) -> p c t", t=128), op=ALU.mult),
                              reads=[r_r, r_E], writes=[r_AR])
                        Bf, r_Bf = Bfr.next(); Kf, r_Kf = Kfr.next()
                        em.op("dve", lambda: nc.vector.tensor_tensor(out=Bf[:, :n], in0=b_[:, :n], in1=Ei[:, :n], op=ALU.mult),
                              reads=[r_b, r_Ei], writes=[r_Bf])
                        em.op("pool", lambda: nc.gpsimd.tensor_tensor(out=Kf[:, :n], in0=kd[:, :n], in1=Ei[:, :n], op=ALU.mult),
                              reads=[r_kd, r_Ei], writes=[r_Kf])
                        gidx = 127 if d == 0 else 0
                        gC = E[:, :n].rearrange("p (c t) -> p c t", t=128)[:, :, gidx:gidx + 1]
                        Bg, r_Bg = Bgr.next(); Kg, r_Kg = Kgr.next()
                        em.op("dve", lambda: nc.vector.tensor_tensor(out=Bg[:, :n].rearrange("p (c t) -> p c t", t=128),
                                                                     in0=Bf[:, :n].rearrange("p (c t) -> p c t", t=128),
                                                                     in1=gC.to_broadcast([64, nch, 128]), op=ALU.mult),
                              reads=[r_Bf, r_E], writes=[r_Bg])
                        em.op("dve", lambda: nc.vector.tensor_tensor(out=Kg[:, :n].rearrange("p (c t) -> p c t", t=128),
                                                                     in0=Kf[:, :n].rearrange("p (c t) -> p c t", t=128),
                                                                     in1=gC.to_broadcast([64, nch, 128]), op=ALU.mult),
                              reads=[r_Kf, r_E], writes=[r_Kg])
                        for ci in range(nch if RWS >= 2 else 0):
                            g = t0 // 128 + ci
                            cs_ = slice(ci * 128, (ci + 1) * 128)
                            Af = AR[:, ci, 0, :]; Rf = AR[:, ci, 1, :]
                            for qi, (src_, rsrc) in enumerate(((Af, r_AR), (Bg[:, cs_], r_Bg), (Kg[:, cs_], r_Kg), (v_[:, cs_], r_v))):
                                em.op("pe", lambda: nc.tensor.transpose(out=ptr[:, qi * 64:(qi + 1) * 64], in_=src_, identity=I64),
                                      reads=[rsrc, self.rc], writes=[r_ptr])
                            tok, r_tok = tokr.next()
                            em.op("act", lambda: nc.scalar.copy(out=tok[:], in_=ptr), reads=[r_ptr], writes=[r_tok])
                            At = tok[:, 0:64]; Bgt = tok[:, 64:128]; Kgt = tok[:, 128:192]; Vt = tok[:, 192:256]
                            ARc = AR[:, ci].rearrange("p a t -> p (a t)")
                            em.op("pe", lambda: nc.tensor.matmul(pBK[:, 0:256], lhsT=Bf[:, cs_], rhs=ARc, start=True, stop=True),
                                  reads=[r_Bf, r_AR], writes=[r_pBK])
                            em.op("pe", lambda: nc.tensor.matmul(pBK[:, 256:512], lhsT=Kf[:, cs_], rhs=ARc, start=True, stop=True),
                                  reads=[r_Kf, r_AR], writes=[r_pBK])
                            NB, r_NB = NBr.next()
                            em.op("dve", lambda: nc.vector.tensor_tensor(out=NB[:], in0=pBK[:], in1=msk_d[:], op=ALU.mult),
                                  reads=[r_pBK, rs_], writes=[r_NB])
                            N_ = NB[:, 0:128]; Mb = NB[:, 128:256]; Mk = NB[:, 256:384]; Mr = NB[:, 384:512]
                            em.op("pe", lambda: nc.tensor.matmul(pNT, lhsT=Af, rhs=Bf[:, cs_], start=True, stop=True),
                                  reads=[r_AR, r_Bf], writes=[r_pNT])
                            NT, r_NT = NTr.next()
                            em.op("dve", lambda: nc.vector.tensor_tensor(out=NT[:], in0=pNT, in1=mskT, op=ALU.mult),
                                  reads=[r_pNT, rs_], writes=[r_NT])
                            em.op("pe", lambda: nc.tensor.matmul(pX1, lhsT=Mk, rhs=Vt, start=True, stop=True),
                                  reads=[r_NB, r_tok], writes=[r_pX1])
                            Z, r_Z = Zr.next()
                            em.op("act", lambda: nc.scalar.copy(out=Z[:, 0:64], in_=pX1), reads=[r_pX1], writes=[r_Z])
                            em.op("pool", lambda: nc.gpsimd.tensor_copy(out=Z[:, 64:128], in_=At), reads=[r_tok], writes=[r_Z])
                            P, r_P = N_, r_NB
                            PT, r_PT = NT[:], r_NT
                            for it in range(7):
                                i2 = it % 2
                                em.op("pe", lambda: nc.tensor.matmul(pz[i2], lhsT=P, rhs=Z[:], start=True, stop=True),
                                      reads=[r_P, r_Z], writes=[r_pz[i2]])
                                if it < 6:
                                    em.op("pe", lambda: nc.tensor.matmul(pp[i2], lhsT=PT, rhs=P, start=True, stop=True),
                                          reads=[r_P, r_PT], writes=[r_pp[i2]])
                                    em.op("pe", lambda: nc.tensor.matmul(ppt[i2], lhsT=P, rhs=PT, start=True, stop=True),
                                          reads=[r_P, r_PT], writes=[r_ppt[i2]])
                                Zn, r_Zn = Zr.next()
                                em.op("dve", lambda: nc.vector.tensor_tensor(out=Zn[:], in0=pz[i2], in1=Z[:], op=ALU.add),
                                      reads=[r_pz[i2], r_Z], writes=[r_Zn])
                                Z, r_Z = Zn, r_Zn
                                if it < 6:
                                    Pn, r_Pn = Pr_.next()
                                    em.op("act", lambda: nc.scalar.copy(out=Pn[:], in_=pp[i2]), reads=[r_pp[i2]], writes=[r_Pn])
                                    PTn, r_PTn = NTr.next()
                                    em.op("dve", lambda: nc.vector.tensor_copy(out=PTn[:], in_=ppt[i2]), reads=[r_ppt[i2]], writes=[r_PTn])
                                    P, r_P = Pn[:], r_Pn
                                    PT, r_PT = PTn[:], r_PTn
                            Wt = Z[:, 0:64]; Apt = Z[:, 64:128]
                            em.op("pe", lambda: nc.tensor.matmul(pQ, lhsT=Apt, rhs=Mb, start=True, stop=False),
                                  reads=[r_Z, r_NB], writes=[r_pQ])
                            em.op("pe", lambda: nc.tensor.matmul(pQ, lhsT=I64, rhs=Rf, start=False, stop=True),
                                  reads=[r_AR, self.rc], writes=[r_pQ])
                            em.op("act", lambda: nc.scalar.copy(out=Qs[:, cs_], in_=pQ), reads=[r_pQ], writes=[rQs])
                            em.op("pe", lambda: nc.tensor.matmul(pYi, lhsT=Wt, rhs=Mb, start=True, stop=False),
                                  reads=[r_Z, r_NB], writes=[r_pYi])
                            em.op("pe", lambda: nc.tensor.matmul(pYi, lhsT=Vt, rhs=Mr, start=False, stop=True),
                                  reads=[r_tok, r_NB], writes=[r_pYi])
                            em.op("dve", lambda: nc.vector.tensor_copy(out=Ys[:, cs_], in_=pYi), reads=[r_pYi], writes=[rYs])
                            dg, r_dg = dgr.next()
                            em.op("pool", lambda: nc.gpsimd.tensor_scalar(out=dg[:], in0=I64, scalar1=gC[:, ci, :], scalar2=None, op0=ALU.mult),
                                  reads=[self.rc, r_E], writes=[r_dg])
                            em.op("pe", lambda: nc.tensor.matmul(pG, lhsT=Apt, rhs=Bgt, start=True, stop=False),
                                  reads=[r_Z, r_tok], writes=[r_pG])
                            em.op("pe", lambda: nc.tensor.matmul(pG, lhsT=I64, rhs=dg[:], start=False, stop=True),
                                  reads=[r_dg, self.rc], writes=[r_pG])
                            em.op("act", lambda: nc.scalar.copy(out=Gs[:, ci, :], in_=pG), reads=[r_pG], writes=[rGs])
                            em.op("pe", lambda: nc.tensor.matmul(pH, lhsT=Kgt, rhs=Vt, start=True, stop=False),
                                  reads=[r_tok], writes=[r_pH])
                            em.op("pe", lambda: nc.tensor.matmul(pH, lhsT=Bgt, rhs=Wt, start=False, stop=True),
                                  reads=[r_tok, r_Z], writes=[r_pH])
                            em.op("dve", lambda: nc.vector.tensor_copy(out=Hs[:, ci, :], in_=pH), reads=[r_pH], writes=[rHs])
                        order = list(range(nch)) if d == 0 else list(range(nch - 1, -1, -1))
                        if RWS < 3:
                            continue
                        for ci in order:
                            i2 = ci % 2
                            gs = slice(ci * 128, (ci + 1) * 128)
                            em.op("pe", lambda: nc.tensor.matmul(py[i2], lhsT=S[:], rhs=Qs[:, gs], start=True, stop=True),
                                  reads=[r_S, rQs], writes=[r_py[i2]])
                            em.op("pe", lambda: nc.tensor.matmul(pst[i2], lhsT=Gs[:, ci, :], rhs=S[:], start=True, stop=True),
                                  reads=[r_S, rGs], writes=[r_pst[i2]])
                            Sn, r_Sn = Str.next()
                            em.op("dve", lambda: nc.vector.tensor_tensor(out=Sn[:], in0=pst[i2], in1=Hs[:, ci, :], op=ALU.add),
                                  reads=[r_pst[i2], rHs], writes=[r_Sn])
                            em.op("dve", lambda: nc.vector.tensor_tensor(out=Ys[:, gs], in0=py[i2], in1=Ys[:, gs], op=ALU.add),
                                  reads=[r_py[i2], rYs], writes=[rYs])
                            S, r_S = Sn, r_Sn
                        o_ = Ys[:, :n]
                        em.op("pe", lambda: nc.tensor.matmul(prd[:, :n], lhsT=O64, rhs=o_, start=True, stop=True),
                              reads=[rYs, self.rc], writes=[r_prd])
                        dl, r_dl = tmps['dl']
                        em.op("dve", lambda: nc.vector.scalar_tensor_tensor(out=dl[:, :n], in0=prd[:, :n], scalar=-1.0 / 64, in1=o_,
                                                                            op0=ALU.mult, op1=ALU.add), reads=[r_prd, rYs], writes=[r_dl])
                        sq, r_sq = tmps['sq']
                        em.op("act", lambda: nc.scalar.activation(out=sq[:, :n], in_=dl[:, :n], func=AF.Square), reads=[r_dl], writes=[r_sq])
                        em.op("pe", lambda: nc.tensor.matmul(prd[:, :n], lhsT=O64, rhs=sq[:, :n], start=True, stop=True),
                              reads=[r_sq, self.rc], writes=[r_prd])
                        em.op("act", lambda: nc.scalar.activation(out=sq[:, :n], in_=prd[:, :n], func=AF.Sqrt, scale=1.0 / 64,
                                                                  bias=self.eps_col(GN_EPS)[0:64, :]), reads=[r_prd, self.rc], writes=[r_sq])
                        em.op("dve", lambda: nc.vector.reciprocal(out=sq[:, :n], in_=sq[:, :n]), reads=[r_sq], writes=[r_sq])
                        em.op("dve", lambda: nc.vector.tensor_tensor(out=dl[:, :n], in0=dl[:, :n], in1=sq[:, :n], op=ALU.mult),
                              reads=[r_dl, r_sq], writes=[r_dl])
                        em.op("dve", lambda: nc.vector.tensor_scalar(out=dl[:, :n], in0=dl[:, :n], scalar1=pc[:, 13:14], scalar2=pc[:, 14:15],
                                                                     op0=ALU.mult, op1=ALU.add), reads=[r_dl, rpc], writes=[r_dl])
                        em.op("pool", lambda: nc.gpsimd.tensor_tensor(out=ysum[:, t0:t0 + n], in0=ysum[:, t0:t0 + n], in1=dl[:, :n], op=ALU.add),
                              reads=[r_dl, rys], writes=[rys])
                for (t0, n, cond) in self.chunks():
                    gl, r_gl = glr.next()
                    em.dma("sp", gl[:, :n], self.projT[3200:3328, t0:t0 + n], reads=[self.r_projT], writes=[r_gl])
                    em.op("act", lambda: nc.scalar.activation(out=gl[:, :n], in_=gl[:, :n], func=AF.Sigmoid), reads=[r_gl], writes=[r_gl])
                    em.op("pe", lambda: nc.tensor.matmul(prd[:, :n], lhsT=gup[:, hs], rhs=gl[:, :n], start=True, stop=True),
                          reads=[rs_, r_gl], writes=[r_prd])
                    yb, r_yb = ybr.next()
                    em.op("dve", lambda: nc.vector.tensor_tensor(out=yb[:, :n], in0=prd[:, :n], in1=ysum[:, t0:t0 + n], op=ALU.mult),
                          reads=[r_prd, rys], writes=[r_yb])
                    em.dma("act", self.yT[512 + h * 64:512 + (h + 1) * 64, t0:t0 + n], yb[:, :n], reads=[r_yb], writes=[self.r_yT])
            em.barrier()
            em.stack = old

    def phase_merge(self, l):
        em, nc = self.em, self.nc
        with contextlib.ExitStack() as st:
            em.stack, old = st, em.stack
            wbf = em.sb([128, 12, D], BF16); rwb = Res()
            wof = em.sb([128, 8, D], BF16); rwo = Res()
            wst = Rot(em, 2, [128, 4, D], F32)
            wbv = self.inp["w_branch"][l].rearrange("b (kt p) f -> p b kt f", p=128)
            for br in range(3):
                w, r_w = wst.next()
                em.dma("sp", w[:], wbv[:, br], writes=[r_w])
                em.op("pool", lambda: nc.gpsimd.tensor_copy(out=wbf[:, br * 4:(br + 1) * 4, :], in_=w[:]), reads=[r_w], writes=[rwb])
            wov = self.inp["w_out"][l].rearrange("(kt p) f -> p kt f", p=128)
            for hf in range(2):
                w, r_w = wst.next()
                em.dma("sp", w[:], wov[:, hf * 4:(hf + 1) * 4, :], writes=[r_w])
                em.op("pool", lambda: nc.gpsimd.tensor_copy(out=wof[:, hf * 4:(hf + 1) * 4, :], in_=w[:]), reads=[r_w], writes=[rwo])
            yin = Rot(em, 2, [128, 12, 512], BF16)
            gin = Rot(em, 3, [128, 512], F32)
            sgr = Rot(em, 3, [128, 512], F32)
            tmr = Rot(em, 3, [128, 512], F32)
            macc = Rot(em, 2, [128, 8, 512], F32)
            mbf = Rot(em, 2, [128, 8, 512], BF16)
            xin = Rot(em, 2, [128, 8, 512], F32)
            pm = Rot(em, 4, [128, 512], F32, psum=True)
            yTv = self.yT.rearrange("(kt p) t -> p kt t", p=128)
            xTv = self.xT.rearrange("(kt p) t -> p kt t", p=128)
            for (t0, n, cond) in self.chunks():
                y, r_y = yin.next()
                em.dma("sp", y[:, :, :n], yTv[:, :, t0:t0 + n], reads=[self.r_yT], writes=[r_y])
                xt, r_x = xin.next()
                em.dma("sp", xt[:, :, :n], xTv[:, :, t0:t0 + n], reads=[self.r_xT], writes=[r_x])
                ma, r_ma = macc.next()
                for d in range(8):
                    for br in range(3):
                        g, r_g = gin.next()
                        row = 4864 + br * 1024 + d * 128
                        em.dma("sp", g[:, :n], self.projT[row:row + 128, t0:t0 + n], reads=[self.r_projT], writes=[r_g])
                        sg, r_sg = sgr.next()
                        em.op("act", lambda: nc.scalar.activation(out=sg[:, :n], in_=g[:, :n], func=AF.Sigmoid),
                              reads=[r_g], writes=[r_sg])
                        pp, r_pp = pm.next()
                        for kt in range(4):
                            em.op("pe", lambda: nc.tensor.matmul(pp[:, :n], lhsT=wbf[:, br * 4 + kt, d * 128:(d + 1) * 128],
                                                                 rhs=y[:, br * 4 + kt, :n], start=(kt == 0), stop=(kt == 3)),
                                  reads=[rwb, r_y], writes=[r_pp])
                        if br == 0:
                            em.op("dve", lambda: nc.vector.tensor_tensor(out=ma[:, d, :n], in0=pp[:, :n], in1=sg[:, :n], op=ALU.mult),
                                  reads=[r_pp, r_sg], writes=[r_ma])
                        else:
                            tm, r_tm = tmr.next()
                            em.op("dve", lambda: nc.vector.tensor_tensor(out=tm[:, :n], in0=pp[:, :n], in1=sg[:, :n], op=ALU.mult),
                                  reads=[r_pp, r_sg], writes=[r_tm])
                            em.op("pool", lambda: nc.gpsimd.tensor_tensor(out=ma[:, d, :n], in0=ma[:, d, :n], in1=tm[:, :n], op=ALU.add),
                                  reads=[r_tm, r_ma], writes=[r_ma])
                mb, r_mb = mbf.next()
                em.op("act", lambda: nc.scalar.copy(out=mb[:, :, :n], in_=ma[:, :, :n]), reads=[r_ma], writes=[r_mb])
                for d in range(8):
                    pp, r_pp = pm.next()
                    for kt in range(8):
                        em.op("pe", lambda: nc.tensor.matmul(pp[:, :n], lhsT=wof[:, kt, d * 128:(d + 1) * 128], rhs=mb[:, kt, :n],
                                                             start=(kt == 0), stop=(kt == 7)), reads=[rwo, r_mb], writes=[r_pp])
                    em.op("dve", lambda: nc.vector.scalar_tensor_tensor(out=xt[:, d, :n], in0=pp[:, :n],
                                                                        scalar=self.modc[:, 2, d, cond:cond + 1], in1=xt[:, d, :n],
                                                                        op0=ALU.mult, op1=ALU.add),
                          reads=[r_pp, self.r_mod, r_x], writes=[r_x])
                em.dma("act", xTv[:, :, t0:t0 + n], xt[:, :, :n], reads=[r_x], writes=[self.r_xT])
            em.barrier()
            em.stack = old

    def phase_ada(self, l):
        em, nc = self.em, self.nc
        if l == 0:
            self.modc = em.sb([128, 6, 8, 2], F32)
            self.r_mod = Res("mod")
            self.cs = em.sb([128, 2, 8], F32)
            self.r_cs = Res("cs")
            ctmp = em.sb([128, 2, 8], F32)
            rct = Res()
            em.dma("sp", ctmp[:, 0, :], self.inp["c"].rearrange("(kt p) -> p kt", p=128), writes=[rct],
                   allow_slow_non_contiguous=True)
            em.dma("sp", ctmp[:, 1, :], self.inp["c_ctx"].rearrange("(kt p) -> p kt", p=128), writes=[rct],
                   allow_slow_non_contiguous=True)
            em.op("act", lambda: nc.scalar.activation(out=self.cs[:], in_=ctmp[:], func=AF.Silu),
                  reads=[rct], writes=[self.r_cs])
        with contextlib.ExitStack() as st:
            em.stack, old = st, em.stack
            wbuf = Rot(em, 2, [128, 8, 768], F32)
            pm = em.ps([128, 48, 2], F32); rpm = Res()
            adab = em.sb([128, 48], F32); rab = Res()
            g12 = em.sb([128, 2, 8], F32); rg = Res()
            em.dma("sp", adab[:], self.inp["ada_b"][l].rearrange("(j p) -> p j", p=128), writes=[rab],
                   allow_slow_non_contiguous=True)
            em.dma("sp", g12[:, 0, :], self.inp["norm1_g"][l].rearrange("(j p) -> p j", p=128), writes=[rg],
                   allow_slow_non_contiguous=True)
            em.dma("sp", g12[:, 1, :], self.inp["norm2_g"][l].rearrange("(j p) -> p j", p=128), writes=[rg],
                   allow_slow_non_contiguous=True)
            awv = self.inp["ada_w"][l].rearrange("(kt p) f -> p kt f", p=128)
            for ch in range(8):
                wt, rw = wbuf.next()
                em.dma("sp", wt[:], awv[:, :, ch * 768:(ch + 1) * 768], writes=[rw])
                for jj in range(6):
                    j = ch * 6 + jj
                    for kt in range(8):
                        em.op("pe", lambda: nc.tensor.matmul(pm[:, j, :], lhsT=wt[:, kt, jj * 128:(jj + 1) * 128],
                                                             rhs=self.cs[:, :, kt], start=(kt == 0), stop=(kt == 7)),
                              reads=[rw, self.r_cs], writes=[rpm])
            modraw = em.sb([128, 48, 2], F32); rmr = Res()
            em.op("dve", lambda: nc.vector.tensor_tensor(out=modraw[:], in0=pm[:],
                                                         in1=adab[:].unsqueeze(2).to_broadcast([128, 48, 2]),
                                                         op=ALU.add),
                  reads=[rpm, rab], writes=[rmr])
            mc = self.modc
            mr = modraw[:].rearrange("p (k j) c -> p k j c", k=6)
            for half in range(2):
                sh, sc, gt = mr[:, 3 * half + 0], mr[:, 3 * half + 1], mr[:, 3 * half + 2]
                gn = g12[:, half, :].unsqueeze(2).to_broadcast([128, 8, 2])
                em.op("dve", lambda: nc.vector.scalar_tensor_tensor(out=mc[:, 3 * half + 0], in0=sc, scalar=1.0, in1=gn,
                                                                    op0=ALU.add, op1=ALU.mult),
                      reads=[rmr, rg], writes=[self.r_mod])
                em.op("dve", lambda: nc.vector.tensor_copy(out=mc[:, 3 * half + 1], in_=sh),
                      reads=[rmr], writes=[self.r_mod])
                em.op("dve", lambda: nc.vector.tensor_copy(out=mc[:, 3 * half + 2], in_=gt),
                      reads=[rmr], writes=[self.r_mod])
            em.barrier()
            em.stack = old

    def phase_norm(self, l, which):
        em, nc = self.em, self.nc
        ka, kb = (0, 1) if which == 1 else (3, 4)
        with contextlib.ExitStack() as st:
            em.stack, old = st, em.stack
            xin = Rot(em, 2, [128, 8, 512], F32)
            sq = Rot(em, 2, [128, 8, 512], F32)
            pss = Rot(em, 2, [128, 512], F32, psum=True)
            rsd = Rot(em, 2, [128, 512], F32)
            tmp = Rot(em, 3, [128, 512], F32)
            hbf = Rot(em, 2, [128, 8, 512], BF16)
            if which == 2:
                tmp = Rot(em, 8, [128, 512], F32)
                h32r = Rot(em, 1, [128, 8, 512], F32)
                exr = Rot(em, 4, [16, 512], F32)
                rwt = em.sb([128, 8, NE], F32); rrw = Res()
                em.dma("sp", rwt[:], self.inp["router_w"][l].rearrange("(kt p) e -> p kt e", p=128), writes=[rrw])
            xTv = self.xT.rearrange("(kt p) t -> p kt t", p=128)
            hTv = self.hT.rearrange("(kt p) t -> p kt t", p=128)
            for (t0, n, cond) in self.chunks():
                tmpk = []
                xt, rx = xin.next()
                em.dma("sp", xt[:, :, :n], xTv[:, :, t0:t0 + n], reads=[self.r_xT], writes=[rx])
                s, rs = sq.next()
                em.op("act", lambda: nc.scalar.activation(out=s[:, :, :n], in_=xt[:, :, :n], func=AF.Square),
                      reads=[rx], writes=[rs])
                pp, rp = pss.next()
                for kt in range(8):
                    em.op("pe", lambda: nc.tensor.matmul(pp[:, :n], lhsT=self.ones[:], rhs=s[:, kt, :n],
                                                         start=(kt == 0), stop=(kt == 7)),
                          reads=[rs, self.rc], writes=[rp])
                rd, rr = rsd.next()
                em.op("act", lambda: nc.scalar.activation(out=rd[:, :n], in_=pp[:, :n], func=AF.Sqrt,
                                                          scale=1.0 / D, bias=self.eps_col(EPS)),
                      reads=[rp, self.rc], writes=[rr])
                em.op("dve", lambda: nc.vector.reciprocal(out=rd[:, :n], in_=rd[:, :n]), reads=[rr], writes=[rr])
                hb, rh = hbf.next()
                for kt in range(8):
                    tm, rt = tmp.next()
                    tmpk.append((tm, rt))
                    em.op("dve", lambda: nc.vector.scalar_tensor_tensor(
                        out=tm[:, :n], in0=xt[:, kt, :n], scalar=self.modc[:, ka, kt, cond:cond + 1],
                        in1=rd[:, :n], op0=ALU.mult, op1=ALU.mult), reads=[rx, rr, self.r_mod], writes=[rt])
                    em.op("act", lambda: nc.scalar.activation(out=hb[:, kt, :n], in_=tm[:, :n], func=AF.Identity,
                                                              bias=self.modc[:, kb, kt, cond:cond + 1], scale=1.0),
                          reads=[rt, self.r_mod], writes=[rh])
                em.dma("act", hTv[:, :, t0:t0 + n], hb[:, :, :n], reads=[rh], writes=[self.r_hT])
                if which == 2:
                    h32, r32 = h32r.next()
                    for kt in range(8):
                        em.op("pool", lambda: nc.gpsimd.tensor_scalar(out=h32[:, kt, :n], in0=tmpk[kt][0][:, :n],
                                                                      scalar1=self.modc[:, kb, kt, cond:cond + 1], scalar2=None,
                                                                      op0=ALU.add), reads=[tmpk[kt][1], self.r_mod], writes=[r32])
                    pl, rpl = pss.next()
                    for kt in range(8):
                        em.op("pe", lambda: nc.tensor.matmul(pl[0:16, :n], lhsT=rwt[:, kt, :], rhs=h32[:, kt, :n],
                                                             start=(kt == 0), stop=(kt == 7)), reads=[rrw, r32], writes=[rpl])
                    ex, rex = exr.next()
                    em.op("act", lambda: nc.scalar.activation(out=ex[:, :n], in_=pl[0:16, :n], func=AF.Exp), reads=[rpl], writes=[rex])
                    pl2, rpl2 = pss.next()
                    em.op("pe", lambda: nc.tensor.matmul(pl2[0:16, :n], lhsT=self.ones[0:16, 0:16], rhs=ex[:, :n], start=True, stop=True),
                          reads=[rex, self.rc], writes=[rpl2])
                    rc_, rrc = exr.next()
                    em.op("dve", lambda: nc.vector.reciprocal(out=rc_[:, :n], in_=pl2[0:16, :n]), reads=[rpl2], writes=[rrc])
                    em.op("dve", lambda: nc.vector.tensor_tensor(out=ex[:, :n], in0=ex[:, :n], in1=rc_[:, :n], op=ALU.mult),
                          reads=[rex, rrc], writes=[rex])
                    em.dma("act", self.affT[:, t0:t0 + n], ex[:, :n], reads=[rex], writes=[self.r_aff])
            em.barrier()
            em.stack = old

    def eps_col(self, v):
        return self.cc[:, self.ccv.index(v):self.ccv.index(v) + 1]

    def phase_inproj(self, l):
        em, nc = self.em, self.nc
        T = self.T
        with contextlib.ExitStack() as st:
            em.stack, old = st, em.stack
            wf = Rot(em, 2, [128, 8, 512], F32)
            wb = Rot(em, 2, [128, 8, 512], BF16)
            hin = Rot(em, 3, [128, 8, 512], BF16)
            pso = Rot(em, 4, [128, 512], F32, psum=True)
            stg = Rot(em, 4, [128, 512], F32)
            stgb = Rot(em, 3, [128, 512], BF16)
            hTv = self.hT.rearrange("(kt p) t -> p kt t", p=128)
            wv = self.inp["w_in"][l].rearrange("(kt p) f -> p kt f", p=128)
            ev = 0
            for c0 in range(0, N_IN, 512):
                nc_ = min(512, N_IN - c0)
                wt, rw = wf.next()
                em.dma("sp", wt[:, :, :nc_], wv[:, :, c0:c0 + nc_], writes=[rw])
                wbt, rwb = wb.next()
                for kt in range(8):
                    e = "pool" if kt % 2 == 0 else "dve"
                    eng = nc.gpsimd if e == "pool" else nc.vector
                    em.op(e, lambda: eng.tensor_copy(out=wbt[:, kt, :nc_], in_=wt[:, kt, :nc_]),
                          reads=[rw], writes=[rwb])
                token_major = (c0 == 1024)
                for (t0, n, cond) in self.chunks():
                    ht, rh = hin.next()
                    em.dma("sp", ht[:, :, :n], hTv[:, :, t0:t0 + n], reads=[self.r_hT], writes=[rh])
                    if not token_major:
                        for m in range(nc_ // 128):
                            pp, rp = pso.next()
                            for kt in range(8):
                                em.op("pe", lambda: nc.tensor.matmul(pp[:, :n], lhsT=wbt[:, kt, m * 128:(m + 1) * 128],
                                                                     rhs=ht[:, kt, :n], start=(kt == 0), stop=(kt == 7)),
                                      reads=[rwb, rh], writes=[rp])
                            sg, rs = stg.next()
                            ev += 1
                            if ev % 2 == 0:
                                em.op("dve", lambda: nc.vector.tensor_copy(out=sg[:, :n], in_=pp[:, :n]),
                                      reads=[rp], writes=[rs])
                            else:
                                em.op("act", lambda: nc.scalar.copy(out=sg[:, :n], in_=pp[:, :n]),
                                      reads=[rp], writes=[rs])
                            em.dma("act", self.projT[c0 + m * 128:c0 + (m + 1) * 128, t0:t0 + n], sg[:, :n],
                                   reads=[rs], writes=[self.r_projT])
                    else:
                        for tt in range(n // 128):
                            pp, rp = pso.next()
                            for kt in range(8):
                                em.op("pe", lambda: nc.tensor.matmul(pp[:, :], lhsT=ht[:, kt, tt * 128:(tt + 1) * 128],
                                                                     rhs=wbt[:, kt, :], start=(kt == 0), stop=(kt == 7)),
                                      reads=[rwb, rh], writes=[rp])
                            sg, rs = stgb.next()
                            em.op("dve", lambda: nc.vector.tensor_copy(out=sg[:], in_=pp[:]), reads=[rp], writes=[rs])
                            em.dma("act", self.vaTM[t0 + tt * 128:t0 + (tt + 1) * 128, :], sg[:],
                                   reads=[rs], writes=[self.r_vaTM])
            em.barrier()
            em.stack = old


    def phase_moe(self, l):
        em, nc = self.em, self.nc
        T, TL = self.T, self.TL
        with contextlib.ExitStack() as st:
            em.stack, old = st, em.stack
            aff = em.sb([16, T], F32); raf = Res()
            wk = em.sb([16, T], F32); rwk = Res()
            m8 = em.sb([16, 8], F32); rm8 = Res()
            th = em.sb([16, 2], F32); rth = Res()
            em.dma("sp", aff[:], self.affT[:, :], reads=[self.r_aff], writes=[raf])
            em.op("dve", lambda: nc.vector.tensor_copy(out=wk[:], in_=aff[:]), reads=[raf], writes=[rwk])
            for (lo, hi, col) in ((0, CTX, 1), (CTX, T, 0)):
                cap = 2 * (hi - lo) // NE
                nit = cap // 8
                for it in range(nit):
                    em.op("dve", lambda: nc.vector.max(out=m8[:], in_=wk[:, lo:hi]), reads=[rwk], writes=[rm8])
                    if it < nit - 1:
                        em.op("dve", lambda: nc.vector.match_replace(out=wk[:, lo:hi], in_to_replace=m8[:], in_values=wk[:, lo:hi],
                                                                     imm_value=-1.0), reads=[rm8, rwk], writes=[rwk])
                em.op("dve", lambda: nc.vector.tensor_copy(out=th[:, col:col + 1], in_=m8[:, 7:8]), reads=[rm8], writes=[rth])
                em.op("dve", lambda: nc.vector.scalar_tensor_tensor(out=wk[:, lo:hi], in0=aff[:, lo:hi], scalar=th[:, col:col + 1],
                                                                    in1=aff[:, lo:hi], op0=ALU.is_ge, op1=ALU.mult),
                      reads=[raf, rth, rwk], writes=[rwk])
            em.dma("act", self.coefT[:, :], wk[:], reads=[rwk], writes=[self.r_coef])
            em.barrier()
            em.stack = old
        with contextlib.ExitStack() as st:
            em.stack, old = st, em.stack
            chs = self.chunks()
            groups = [chs[i:i + 2] for i in range(0, len(chs), 2)]
            sel = em.sb([16, 16, 128], F32); rsel = Res()
            em.dma("sp", sel[:], self.inp["cst_sel"].rearrange("k (e m) -> k e m", e=16), writes=[rsel])
            acc = em.sb([128, 8, 1024], F32); racc = Res()
            hg = em.sb([128, 8, 1024], BF16); rhg = Res()
            cfg = em.sb([16, 1024], F32); rcfg = Res()
            W = [em.sb([128, 8, D], BF16) for _ in range(3)]
            rW = [Res() for _ in range(3)]
            wst = Rot(em, 2, [128, 4, D], F32)
            p1r = Rot(em, 2, [128, 512], F32, psum=True)
            p3r = Rot(em, 2, [128, 512], F32, psum=True)
            por = Rot(em, 2, [128, 512], F32, psum=True)
            pcb = em.ps([128, 512], F32); rpcb = Res()
            cbr = Rot(em, 2, [128, 512], F32)
            sr = Rot(em, 2, [128, 512], F32)
            tr = Rot(em, 2, [128, 512], F32)
            hid = Rot(em, 2, [128, 8, 512], BF16)
            xin = Rot(em, 1, [128, 8, 512], F32)
            hTv = self.hT.rearrange("(kt p) t -> p kt t", p=128)
            xTv = self.xT.rearrange("(kt p) t -> p kt t", p=128)
            wsrc = [self.inp["exp_w1"], self.inp["exp_w3"], self.inp["exp_w2"]]
            cc = 0
            for grp in groups:
                g0 = grp[0][0]
                gn = sum(c[1] for c in grp)
                em.dma("sp", hg[:, :, :gn], hTv[:, :, g0:g0 + gn], reads=[self.r_hT], writes=[rhg])
                em.dma("sp", cfg[:, :gn], self.coefT[:, g0:g0 + gn], reads=[self.r_coef], writes=[rcfg])
                for e in range(NE):
                    for wi in range(3):
                        wv = wsrc[wi][l, e].rearrange("(kt p) f -> p kt f", p=128)
                        for hf in range(2):
                            w, r_w = wst.next()
                            em.dma("sp", w[:], wv[:, hf * 4:(hf + 1) * 4, :], writes=[r_w])
                            cc += 1
                            if cc % 2 == 0:
                                em.op("pool", lambda: nc.gpsimd.tensor_copy(out=W[wi][:, hf * 4:(hf + 1) * 4, :], in_=w[:]),
                                      reads=[r_w], writes=[rW[wi]])
                            else:
                                em.op("dve", lambda: nc.vector.tensor_copy(out=W[wi][:, hf * 4:(hf + 1) * 4, :], in_=w[:]),
                                      reads=[r_w], writes=[rW[wi]])
                    for (t0, n, cond) in grp:
                        o0 = t0 - g0
                        em.op("pe", lambda: nc.tensor.matmul(pcb[:, :n], lhsT=sel[:, e, :], rhs=cfg[:, o0:o0 + n], start=True, stop=True),
                              reads=[rsel, rcfg], writes=[rpcb])
                        cb, rcb = cbr.next()
                        em.op("act", lambda: nc.scalar.copy(out=cb[:, :n], in_=pcb[:, :n]), reads=[rpcb], writes=[rcb])
                        hd, rhd = hid.next()
                        for f in range(8):
                            p1, rp1 = p1r.next(); p3, rp3 = p3r.next()
                            for kt in range(8):
                                em.op("pe", lambda: nc.tensor.matmul(p1[:, :n], lhsT=W[0][:, kt, f * 128:(f + 1) * 128],
                                                                     rhs=hg[:, kt, o0:o0 + n], start=(kt == 0), stop=(kt == 7)),
                                      reads=[rW[0], rhg], writes=[rp1])
                            for kt in range(8):
                                em.op("pe", lambda: nc.tensor.matmul(p3[:, :n], lhsT=W[1][:, kt, f * 128:(f + 1) * 128],
                                                                     rhs=hg[:, kt, o0:o0 + n], start=(kt == 0), stop=(kt == 7)),
                                      reads=[rW[1], rhg], writes=[rp3])
                            s_, rs_ = sr.next()
                            em.op("act", lambda: nc.scalar.activation(out=s_[:, :n], in_=p1[:, :n], func=AF.Silu), reads=[rp1], writes=[rs_])
                            t_, rt_ = tr.next()
                            em.op("dve", lambda: nc.vector.tensor_tensor(out=t_[:, :n], in0=p3[:, :n], in1=s_[:, :n], op=ALU.mult),
                                  reads=[rp3, rs_], writes=[rt_])
                            em.op("pool", lambda: nc.gpsimd.tensor_tensor(out=hd[:, f, :n], in0=t_[:, :n], in1=cb[:, :n], op=ALU.mult),
                                  reads=[rt_, rcb], writes=[rhd])
                        for d in range(8):
                            po, rpo = por.next()
                            for f in range(8):
                                em.op("pe", lambda: nc.tensor.matmul(po[:, :n], lhsT=W[2][:, f, d * 128:(d + 1) * 128], rhs=hd[:, f, :n],
                                                                     start=(f == 0), stop=(f == 7)), reads=[rW[2], rhd], writes=[rpo])
                            if e == 0:
                                em.op("dve", lambda: nc.vector.tensor_copy(out=acc[:, d, o0:o0 + n], in_=po[:, :n]), reads=[rpo], writes=[racc])
                            else:
                                em.op("dve", lambda: nc.vector.tensor_tensor(out=acc[:, d, o0:o0 + n], in0=po[:, :n],
                                                                             in1=acc[:, d, o0:o0 + n], op=ALU.add),
                                      reads=[rpo, racc], writes=[racc])
                for (t0, n, cond) in grp:
                    o0 = t0 - g0
                    xt, r_x = xin.next()
                    em.dma("sp", xt[:, :, :n], xTv[:, :, t0:t0 + n], reads=[self.r_xT], writes=[r_x])
                    for d in range(8):
                        em.op("dve", lambda: nc.vector.scalar_tensor_tensor(out=xt[:, d, :n], in0=acc[:, d, o0:o0 + n],
                                                                            scalar=self.modc[:, 5, d, cond:cond + 1], in1=xt[:, d, :n],
                                                                            op0=ALU.mult, op1=ALU.add),
                              reads=[racc, self.r_mod, r_x], writes=[r_x])
                    em.dma("act", xTv[:, :, t0:t0 + n], xt[:, :, :n], reads=[r_x], writes=[self.r_xT])
            em.barrier()
            em.stack = old

_CACHE = {}


def _get_prog(t_lat, depth, dbg=()):
    key = (t_lat, depth, tuple(dbg))
    if key not in _CACHE:
        lam = [0.8 - 0.6 * math.exp(-0.3 * i) for i in range(depth)]
        _CACHE[key] = K(t_lat, depth, lam, list(dbg))
    return _CACHE[key]


def run(inputs, dbg=()):
    x = np.asarray(inputs["x"], np.float32)
    B, t_lat, _ = x.shape
    depth = np.asarray(inputs["norm1_g"]).shape[0]
    prog = _get_prog(t_lat, depth, dbg)
    consts = host_consts(t_lat)
    in_maps = []
    for core in range(8):
        b = core % B
        m = {}
        for k in prog.inp:
            if k.startswith("cst_"):
                m[k] = consts[k[4:]]
            elif k == "x":
                m[k] = np.ascontiguousarray(x[b])
            elif k == "c":
                m[k] = np.ascontiguousarray(np.asarray(inputs["c"], np.float32)[b])
            elif k == "ctx":
                m[k] = np.ascontiguousarray(np.asarray(inputs["ctx"], np.float32)[b])
            elif k == "rwkv_r_k":
                m[k] = np.ascontiguousarray(np.asarray(inputs[k], np.float32).reshape(depth, 512))
            else:
                m[k] = np.ascontiguousarray(np.asarray(inputs[k], np.float32))
        in_maps.append(m)
    res = run_bass_kernel_spmd(prog.nc, in_maps, core_ids=list(range(8)))
    out = np.stack([np.asarray(res.results[b]["out"], np.float32) for b in range(B)], axis=0)
    dbg_res = {name: [np.asarray(res.results[b][name]) for b in range(B)] for name in dbg}
    return out, dbg_res


def kernel(**inputs):
    out, _ = run(inputs)
    return out
```

```python
import contextlib
import math
import numpy as np
import ml_dtypes
import concourse.bass as bass
import concourse.mybir as mybir
from concourse.bass_utils import run_bass_kernel_spmd

F32 = mybir.dt.float32
BF16 = mybir.dt.bfloat16
AF = mybir.ActivationFunctionType
ALU = mybir.AluOpType

D = 1024
CTX = 256
N_IN = 7936
NE = 16
EPS = 1e-6
GN_EPS = 64e-5
NDQ = 10
PHASES = ["attn", "conv", "rwkv", "merge", "moe"]
RWS = 3


class Res:
    __slots__ = ("w", "r", "name", "excl")

    def __init__(self, name="", excl=False):
        self.w = {}
        self.r = {}
        self.name = name
        self.excl = excl


class Em:
    ENG = ("pe", "act", "dve", "pool", "sp")

    def __init__(self, nc, stack):
        self.nc = nc
        self.stack = stack
        self.eng = {"pe": nc.tensor, "act": nc.scalar, "dve": nc.vector,
                    "pool": nc.gpsimd, "sp": nc.sync}
        self.sem = {}
        self.cnt = {}
        for k in ("pe", "act", "dve", "pool"):
            self.sem[k] = stack.enter_context(nc.semaphore("s_" + k))
            self.cnt[k] = 0
        self.dslot = {}
        for q in ("sp", "act", "pool"):
            self.dslot[q] = 0
            for i in range(NDQ):
                key = "d_%s_%d" % (q, i)
                self.sem[key] = stack.enter_context(nc.semaphore("sd_%s_%d" % (q, i)))
                self.cnt[key] = 0
        self.seen = {e: {} for e in self.ENG}
        self.n_inst = 0
        self.n_wait = 0
        self.uid = 0

    def sb(self, shape, dt, name=None):
        self.uid += 1
        return self.stack.enter_context(
            self.nc.sbuf_tensor(name or ("t%d" % self.uid), list(shape), dt))

    def ps(self, shape, dt=F32, name=None):
        self.uid += 1
        return self.stack.enter_context(
            self.nc.psum_tensor(name or ("p%d" % self.uid), list(shape), dt))

    def _wait(self, e, key, c):
        if c <= 0:
            return
        s = self.seen[e]
        if s.get(key, 0) >= c:
            return
        self.eng[e].wait_ge(self.sem[key], c)
        s[key] = c
        self.n_wait += 1

    def _deps(self, e, reads, writes, own_key):
        skip_raw = own_key if own_key == "pe" else None
        for r in reads:
            for k, c in r.w.items():
                if k == skip_raw:
                    continue
                self._wait(e, k, c)
        for w in writes:
            for k, c in w.w.items():
                if k == skip_raw:
                    continue
                self._wait(e, k, c)
            for k, c in w.r.items():
                if k == own_key:
                    continue
                self._wait(e, k, c)

    def _commit(self, key, c, reads, writes):
        for r in reads:
            if r.r.get(key, 0) < c:
                r.r[key] = c
        for w in writes:
            w.w = {key: c}
            w.r = {}

    def op(self, e, fn, reads=(), writes=()):
        if any(r.excl for r in reads):
            writes = list(writes) + [r for r in reads if r.excl]
            reads = [r for r in reads if not r.excl]
        self._deps(e, reads, writes, e)
        ins = fn()
        self.cnt[e] += 1
        c = self.cnt[e]
        ins.then_inc(self.sem[e], 1)
        self._commit(e, c, reads, writes)
        self.n_inst += 1
        return ins

    def dma(self, q, out, in_, reads=(), writes=(), **kw):
        slot = self.dslot[q]
        self.dslot[q] = (slot + 1) % NDQ
        key = "d_%s_%d" % (q, slot)
        self._wait(q, key, self.cnt[key])
        self._deps(q, reads, writes, None)
        ins = self.eng[q].dma_start(out=out, in_=in_, **kw)
        self.cnt[key] += 16
        c = self.cnt[key]
        ins.then_inc(self.sem[key], 16)
        self._commit(key, c, reads, writes)
        self.n_inst += 1
        return ins

    def barrier(self):
        for e in self.ENG:
            for k, c in self.cnt.items():
                if k == e:
                    continue
                self._wait(e, k, c)

    def finish(self):
        for k, c in self.cnt.items():
            self._wait("sp", k, c)


class Rot:
    def __init__(self, em, n, shape, dt, psum=False):
        self.items = []
        for _ in range(n):
            t = em.ps(shape, dt) if psum else em.sb(shape, dt)
            self.items.append((t, Res()))
        self.i = 0

    def next(self):
        it = self.items[self.i]
        self.i = (self.i + 1) % len(self.items)
        return it


def host_consts(t_lat):
    c = {}
    c["ident"] = np.eye(128, dtype=np.float32)
    c["ones"] = np.ones((128, 128), np.float32)
    bd = np.zeros((128, 128), np.float32)
    bd[:64, :64] = 1.0
    bd[64:, 64:] = 1.0
    c["bd64"] = bd
    rm = np.zeros((128, 128), np.float32)
    for p in range(128):
        d = p % 64
        if d < 32:
            rm[p + 32, p] = -1.0
        else:
            rm[p - 32, p] = 1.0
    c["rotm"] = rm
    sel = np.zeros((16, 16, 128), np.float32)
    for e in range(16):
        sel[e, e, :] = 1.0
    c["sel"] = sel.reshape(16, 2048)
    ii = np.arange(128)
    ms_f = (ii[:, None] < ii[None, :]).astype(np.float32)
    mi_f = (ii[:, None] <= ii[None, :]).astype(np.float32)
    c["mskf"] = np.concatenate([ms_f, mi_f, ms_f, mi_f], axis=1)
    c["mskb"] = np.concatenate([ms_f.T, mi_f.T, ms_f.T, mi_f.T], axis=1)
    rst = np.ones((64, 512), np.float32)
    rst[:, ::128] = 0.0
    c["rst"] = rst
    half = 32
    inv = np.power(10000.0, -np.arange(0, half, 2, dtype=np.float32) / half).astype(np.float32)
    rows = t_lat // 64
    r = np.repeat(np.arange(rows, dtype=np.float32), 64)
    col = np.tile(np.arange(64, dtype=np.float32), rows)
    ang = np.concatenate([r[:, None] * inv, col[:, None] * inv], axis=-1).astype(np.float32)
    cos = np.cos(ang).astype(np.float32).T
    sin = np.sin(ang).astype(np.float32).T
    c["cosT"] = np.ascontiguousarray(np.tile(cos, (4, 1)))
    c["sinT"] = np.ascontiguousarray(np.tile(sin, (4, 1)))
    return c


class K:
    def __init__(self, t_lat, depth, lam_inits, dbg=None):
        self.TL = t_lat
        self.T = CTX + t_lat
        self.depth = depth
        self.lam_inits = lam_inits
        self.dbg = dbg or []
        T = self.T
        nc = self.nc = bass.Bass("TRN2", target_bir_lowering=False)
        self.inp = {}

        def din(name, shape, dt=F32):
            self.inp[name] = nc.dram_tensor(name, list(shape), dt, kind="ExternalInput").ap()
            return self.inp[name]

        L = depth
        din("x", [t_lat, D]); din("c", [D]); din("ctx", [CTX, D]); din("c_ctx", [D])
        din("norm1_g", [L, D]); din("norm2_g", [L, D]); din("ada_w", [L, D, 6 * D]); din("ada_b", [L, 6 * D])
        din("w_in", [L, D, N_IN]); din("q_norm_g", [L, 64]); din("k_norm_g", [L, 64])
        din("diff_lambda", [L, 4, 64]); din("diff_subln_g", [L, 128])
        din("rwkv_conv_w", [L, 3, 1536]); din("rwkv_w0", [L, 2, 512]); din("rwkv_w_up", [L, 2, 64, 512])
        din("rwkv_a0", [L, 2, 512]); din("rwkv_a_up", [L, 2, 64, 512]); din("rwkv_g_up", [L, 128, 512])
        din("rwkv_k_k", [L, 512]); din("rwkv_k_a", [L, 512]); din("rwkv_r_k", [L, 512])
        din("rwkv_ln_g", [L, 512]); din("rwkv_ln_b", [L, 512]); din("conv_w", [L, 3, 512])
        din("w_branch", [L, 3, 512, D]); din("w_out", [L, D, D]); din("router_w", [L, D, NE])
        if "moe" in PHASES:
            din("exp_w1", [L, NE, D, D]); din("exp_w3", [L, NE, D, D]); din("exp_w2", [L, NE, D, D])
        for k, v in host_consts(t_lat).items():
            din("cst_" + k, v.shape)
        self.out = nc.dram_tensor("out", [t_lat, D], F32, kind="ExternalOutput").ap()
        self.dbg_out = {}

        def scratch(name, shape, dt=F32):
            kind = "ExternalOutput" if name in self.dbg else "Internal"
            ap = nc.dram_tensor(name, list(shape), dt, kind=kind).ap()
            if name in self.dbg:
                self.dbg_out[name] = ap
            return ap

        self.xT = scratch("xT", [D, T]); self.r_xT = Res("xT")
        self.hT = scratch("hT", [D, T], BF16); self.r_hT = Res("hT")
        self.projT = scratch("projT", [N_IN, T]); self.r_projT = Res("projT")
        self.vaTM = scratch("vaTM", [T, 512], BF16); self.r_vaTM = Res("vaTM")
        self.yT = scratch("yT", [3 * 512, T], BF16); self.r_yT = Res("yT")
        self.affT = scratch("affT", [NE, T]); self.r_aff = Res("aff")
        self.coefT = scratch("coefT", [NE, T]); self.r_coef = Res("coef")

        with contextlib.ExitStack() as st:
            em = self.em = Em(nc, st)
            st.enter_context(nc.Block())
            self.consts(st)
            self.phase_transpose_in()
            for l in range(depth):
                self.layer(l)
            self.phase_transpose_out()
            em.barrier()
            em.finish()

    def chunks(self):
        res = [(0, CTX, 1)]
        t = CTX
        while t < self.T:
            n = min(512, self.T - t)
            res.append((t, n, 0))
            t += n
        return res

    def consts(self, st):
        em, nc = self.em, self.nc
        self.rc = Res("consts")
        self.ident = em.sb([128, 128], F32)
        self.ones = em.sb([128, 128], F32)
        self.bd64 = em.sb([128, 128], F32)
        self.rotm = em.sb([128, 128], F32)
        self.ones_bf = em.sb([128, 128], BF16)
        for t, nm in ((self.ident, "ident"), (self.ones, "ones"), (self.bd64, "bd64"), (self.rotm, "rotm")):
            em.dma("sp", t[:], self.inp["cst_" + nm][:, :], writes=[self.rc])
        em.op("dve", lambda: nc.vector.tensor_copy(out=self.ones_bf[:], in_=self.ones[:]),
              reads=[self.rc], writes=[self.rc])
        self.ccv = [EPS, GN_EPS, 1e-24, 0.0, 1.0]
        self.cc = em.sb([128, len(self.ccv)], F32)
        for i, v in enumerate(self.ccv):
            em.op("pool", lambda: nc.gpsimd.memset(self.cc[:, i:i + 1], float(v)), writes=[self.rc])

    def phase_transpose_in(self):
        em, nc = self.em, self.nc
        with contextlib.ExitStack() as st:
            em.stack, old = st, em.stack
            xin = Rot(em, 2, [128, D], F32)
            pst = Rot(em, 2, [128, 512], F32, psum=True)
            stg = Rot(em, 2, [128, 8, 128], F32)
            xTv = self.xT.rearrange("(kt p) t -> p kt t", p=128)
            for j in range(self.T // 128):
                t0 = j * 128
                src = self.inp["ctx"][t0:t0 + 128, :] if t0 < CTX else self.inp["x"][t0 - CTX:t0 - CTX + 128, :]
                xt, rx = xin.next()
                em.dma("sp", xt[:], src, writes=[rx])
                sg, rs = stg.next()
                for half in range(2):
                    pt, rp = pst.next()
                    for q in range(4):
                        kt = half * 4 + q
                        em.op("pe", lambda: nc.tensor.transpose(out=pt[:, q * 128:(q + 1) * 128],
                                                                in_=xt[:, kt * 128:(kt + 1) * 128],
                                                                identity=self.ident[:]),
                              reads=[rx, self.rc], writes=[rp])
                    e = "dve" if half == 0 else "act"
                    dst = sg[:, half * 4:(half + 1) * 4, :]
                    srcp = pt[:].rearrange("p (q t) -> p q t", q=4)
                    if e == "dve":
                        em.op("dve", lambda: nc.vector.tensor_copy(out=dst, in_=srcp), reads=[rp], writes=[rs])
                    else:
                        em.op("act", lambda: nc.scalar.copy(out=dst, in_=srcp), reads=[rp], writes=[rs])
                em.dma("act", xTv[:, :, t0:t0 + 128], sg[:], reads=[rs], writes=[self.r_xT])
            em.barrier()
            em.stack = old

    def phase_transpose_out(self):
        em, nc = self.em, self.nc
        with contextlib.ExitStack() as st:
            em.stack, old = st, em.stack
            xin = Rot(em, 2, [128, 8, 128], F32)
            pst = Rot(em, 2, [128, 512], F32, psum=True)
            stg = Rot(em, 2, [128, D], F32)
            xTv = self.xT.rearrange("(kt p) t -> p kt t", p=128)
            self.r_out = Res("out")
            for j in range(CTX // 128, self.T // 128):
                t0 = j * 128
                xt, rx = xin.next()
                em.dma("sp", xt[:], xTv[:, :, t0:t0 + 128], reads=[self.r_xT], writes=[rx])
                sg, rs = stg.next()
                for half in range(2):
                    pt, rp = pst.next()
                    for q in range(4):
                        kt = half * 4 + q
                        em.op("pe", lambda: nc.tensor.transpose(out=pt[:, q * 128:(q + 1) * 128],
                                                                in_=xt[:, kt, :], identity=self.ident[:]),
                              reads=[rx, self.rc], writes=[rp])
                    dst = sg[:, half * 512:(half + 1) * 512]
                    if half == 0:
                        em.op("dve", lambda: nc.vector.tensor_copy(out=dst, in_=pt[:]), reads=[rp], writes=[rs])
                    else:
                        em.op("act", lambda: nc.scalar.copy(out=dst, in_=pt[:]), reads=[rp], writes=[rs])
                em.dma("act", self.out[t0 - CTX:t0 - CTX + 128, :], sg[:], reads=[rs], writes=[self.r_out])
            em.barrier()
            em.stack = old

    def layer(self, l):
        self.phase_ada(l)
        self.phase_norm(l, which=1)
        self.phase_inproj(l)
        if "attn" in PHASES:
            self.phase_attn(l)
        if "conv" in PHASES:
            self.phase_conv(l)
        if "rwkv" in PHASES:
            self.phase_rwkv(l)
        else:
            em, nc = self.em, self.nc
            with contextlib.ExitStack() as st:
                em.stack, old = st, em.stack
                z = em.sb([128, 512], BF16); rz = Res()
                em.op("pool", lambda: nc.gpsimd.memset(z[:], 0.0), writes=[rz])
                for j in range(4):
                    for (t0, n, cond) in self.chunks():
                        em.dma("act", self.yT[512 + j * 128:512 + (j + 1) * 128, t0:t0 + n], z[:, :n], reads=[rz], writes=[self.r_yT])
                em.barrier()
                em.stack = old
        if "merge" in PHASES:
            self.phase_merge(l)
        if "moe" in PHASES:
            self.phase_norm(l, which=2)
            self.phase_moe(l)

    def phase_attn(self, l):
        em, nc = self.em, self.nc
        T = self.T
        NT = T // 128
        with contextlib.ExitStack() as st:
            em.stack, old = st, em.stack
            rs_ = Res("attn_setup")
            qg = em.sb([128, 1], F32); kg = em.sb([128, 1], F32); sgc = em.sb([128, 1], F32)
            for hh in range(2):
                em.dma("sp", qg[hh * 64:(hh + 1) * 64, :], self.inp["q_norm_g"][l].rearrange("(p o) -> p o", o=1), writes=[rs_])
                em.dma("sp", kg[hh * 64:(hh + 1) * 64, :], self.inp["k_norm_g"][l].rearrange("(p o) -> p o", o=1), writes=[rs_])
            em.dma("sp", sgc[:], self.inp["diff_subln_g"][l].rearrange("(p o) -> p o", o=1), writes=[rs_])
            lam_init = self.lam_inits[l]
            em.op("dve", lambda: nc.vector.tensor_scalar(out=sgc[:], in0=sgc[:], scalar1=float(1.0 - lam_init),
                                                         scalar2=None, op0=ALU.mult), reads=[rs_], writes=[rs_])
            lv = em.sb([64, 4], F32)
            em.dma("sp", lv[:], self.inp["diff_lambda"][l].rearrange("f d -> d f"), writes=[rs_],
                   allow_slow_non_contiguous=True)
            pr = em.sb([64, 2], F32)
            lvv = lv[:].rearrange("p (a b) -> p a b", b=2)
            em.op("dve", lambda: nc.vector.tensor_tensor(out=pr[:], in0=lvv[:, :, 0], in1=lvv[:, :, 1], op=ALU.mult),
                  reads=[rs_], writes=[rs_])
            pmisc = em.ps([128, 512], F32); rpm = Res()
            em.op("pe", lambda: nc.tensor.matmul(pmisc[:, 0:2], lhsT=self.ones[0:64, :], rhs=pr[:], start=True, stop=True),
                  reads=[rs_, self.rc], writes=[rpm])
            el = em.sb([128, 2], F32)
            em.op("act", lambda: nc.scalar.activation(out=el[:], in_=pmisc[:, 0:2], func=AF.Exp), reads=[rpm], writes=[rs_])
            neglam = em.sb([128, 1], F32)
            em.op("dve", lambda: nc.vector.scalar_tensor_tensor(out=neglam[:], in0=el[:, 1:2], scalar=float(-lam_init),
                                                                in1=el[:, 0:1], op0=ALU.add, op1=ALU.subtract),
                  reads=[rs_], writes=[rs_])

            KT = em.sb([128, T], BF16); rK = Res()
            QT = em.sb([128, T], BF16); rQ = Res()
            Vh = em.sb([128, NT, 128], BF16); rV = Res()
            src = Rot(em, 2, [128, 512], F32)
            sqr = Rot(em, 2, [128, 512], F32)
            rsd = Rot(em, 2, [128, 512], F32)
            knr = Rot(em, 2, [128, 512], F32)
            cosr = Rot(em, 2, [128, 512], F32)
            sinr = Rot(em, 2, [128, 512], F32)
            t1r = Rot(em, 2, [128, 512], F32)
            t2r = Rot(em, 2, [128, 512], F32)
            pS = Rot(em, 3, [128, 512], F32, psum=True)
            pO = Rot(em, 2, [128, 512], F32, psum=True)
            pD = Rot(em, 2, [128, 512], F32, psum=True)
            Pr = Rot(em, 3, [128, 512], BF16)
            omr = Rot(em, 2, [128, 512], F32)
            accr = Rot(em, 2, [128, 512], F32)
            yst = Rot(em, 2, [128, 512], BF16)

            def qk_prep(row0, gcol, dst, rdst):
                for (t0, n, cond) in self.chunks():
                    s, r_s = src.next()
                    em.dma("sp", s[:, :n], self.projT[row0:row0 + 128, t0:t0 + n], reads=[self.r_projT], writes=[r_s])
                    q2, r_q2 = sqr.next()
                    em.op("act", lambda: nc.scalar.activation(out=q2[:, :n], in_=s[:, :n], func=AF.Square),
                          reads=[r_s], writes=[r_q2])
                    em.op("pe", lambda: nc.tensor.matmul(pmisc[:, :n], lhsT=self.bd64[:], rhs=q2[:, :n], start=True, stop=True),
                          reads=[r_q2, self.rc], writes=[rpm])
                    rd, r_rd = rsd.next()
                    em.op("act", lambda: nc.scalar.activation(out=rd[:, :n], in_=pmisc[:, :n], func=AF.Sqrt,
                                                              scale=1.0 / 64, bias=self.eps_col(EPS)),
                          reads=[rpm, self.rc], writes=[r_rd])
                    em.op("dve", lambda: nc.vector.reciprocal(out=rd[:, :n], in_=rd[:, :n]), reads=[r_rd], writes=[r_rd])
                    kn, r_kn = knr.next()
                    em.op("dve", lambda: nc.vector.scalar_tensor_tensor(out=kn[:, :n], in0=s[:, :n], scalar=gcol[:],
                                                                        in1=rd[:, :n], op0=ALU.mult, op1=ALU.mult),
                          reads=[r_s, r_rd, rs_], writes=[r_kn])
                    if cond == 1:
                        em.op("act", lambda: nc.scalar.copy(out=dst[:, t0:t0 + n], in_=kn[:, :n]), reads=[r_kn], writes=[rdst])
                    else:
                        em.op("pe", lambda: nc.tensor.matmul(pmisc[:, :n], lhsT=self.rotm[:], rhs=kn[:, :n], start=True, stop=True),
                              reads=[r_kn, self.rc], writes=[rpm])
                        cs_, r_c = cosr.next(); sn_, r_sn = sinr.next()
                        em.dma("sp", cs_[:, :n], self.inp["cst_cosT"][:, t0 - CTX:t0 - CTX + n], writes=[r_c])
                        em.dma("sp", sn_[:, :n], self.inp["cst_sinT"][:, t0 - CTX:t0 - CTX + n], writes=[r_sn])
                        t1, r_t1 = t1r.next(); t2, r_t2 = t2r.next()
                        em.op("pool", lambda: nc.gpsimd.tensor_tensor(out=t1[:, :n], in0=kn[:, :n], in1=cs_[:, :n], op=ALU.mult),
                              reads=[r_kn, r_c], writes=[r_t1])
                        em.op("dve", lambda: nc.vector.tensor_tensor(out=t2[:, :n], in0=pmisc[:, :n], in1=sn_[:, :n], op=ALU.mult),
                              reads=[rpm, r_sn], writes=[r_t2])
                        em.op("pool", lambda: nc.gpsimd.tensor_tensor(out=dst[:, t0:t0 + n], in0=t1[:, :n], in1=t2[:, :n], op=ALU.add),
                              reads=[r_t1, r_t2], writes=[rdst])

            for h in range(4):
                qk_prep(512 + h * 128, kg, KT, rK)
                qk_prep(h * 128, qg, QT, rQ)
                em.dma("sp", Vh[:], self.vaTM[:, h * 128:(h + 1) * 128].rearrange("(j p) e -> p j e", p=128),
                       reads=[self.r_vaTM], writes=[rV])
                for (t0, n, cond) in self.chunks():
                    kts = range(0, CTX // 128) if cond == 1 else range(0, NT)
                    nk = len(kts)
                    oms = []
                    for m in range(2):
                        po, r_po = pO.next(); pd, r_pd = pD.next()
                        acc, r_acc = accr.next()
                        for i, kt in enumerate(kts):
                            ps_, r_ps = pS.next()
                            em.op("pe", lambda: nc.tensor.matmul(ps_[:, :n], lhsT=KT[m * 64:(m + 1) * 64, kt * 128:(kt + 1) * 128],
                                                                 rhs=QT[m * 64:(m + 1) * 64, t0:t0 + n], start=True, stop=True),
                                  reads=[rK, rQ], writes=[r_ps])
                            P, r_P = Pr.next()
                            em.op("act", lambda: nc.scalar.activation(out=P[:, :n], in_=ps_[:, :n], func=AF.Exp, scale=0.125),
                                  reads=[r_ps], writes=[r_P])
                            em.op("pe", lambda: nc.tensor.matmul(po[:, :n], lhsT=Vh[:, kt, :], rhs=P[:, :n],
                                                                 start=(i == 0), stop=(i == nk - 1)),
                                  reads=[rV, r_P], writes=[r_po])
                            if i == 0:
                                em.op("dve", lambda: nc.vector.tensor_copy(out=acc[:, :n], in_=P[:, :n]), reads=[r_P], writes=[r_acc])
                            else:
                                em.op("dve", lambda: nc.vector.tensor_tensor(out=acc[:, :n], in0=acc[:, :n], in1=P[:, :n], op=ALU.add),
                                      reads=[r_P, r_acc], writes=[r_acc])
                        em.op("pe", lambda: nc.tensor.matmul(pd[:, :n], lhsT=self.ones[:], rhs=acc[:, :n], start=True, stop=True),
                              reads=[self.rc, r_acc], writes=[r_pd])
                        rd, r_rd = rsd.next()
                        em.op("dve", lambda: nc.vector.reciprocal(out=rd[:, :n], in_=pd[:, :n]), reads=[r_pd], writes=[r_rd])
                        om, r_om = omr.next()
                        em.op("dve", lambda: nc.vector.tensor_tensor(out=om[:, :n], in0=po[:, :n], in1=rd[:, :n], op=ALU.mult),
                              reads=[r_po, r_rd], writes=[r_om])
                        oms.append((om, r_om))
                    (o0, r0), (o1, r1) = oms
                    em.op("dve", lambda: nc.vector.scalar_tensor_tensor(out=o0[:, :n], in0=o1[:, :n], scalar=neglam[:],
                                                                        in1=o0[:, :n], op0=ALU.mult, op1=ALU.add),
                          reads=[r1, r0, rs_], writes=[r0])
                    q2, r_q2 = sqr.next()
                    em.op("act", lambda: nc.scalar.activation(out=q2[:, :n], in_=o0[:, :n], func=AF.Square),
                          reads=[r0], writes=[r_q2])
                    em.op("pe", lambda: nc.tensor.matmul(pmisc[:, :n], lhsT=self.ones[:], rhs=q2[:, :n], start=True, stop=True),
                          reads=[r_q2, self.rc], writes=[rpm])
                    rd, r_rd = rsd.next()
                    em.op("act", lambda: nc.scalar.activation(out=rd[:, :n], in_=pmisc[:, :n], func=AF.Sqrt,
                                                              scale=1.0 / 128, bias=self.eps_col(EPS)),
                          reads=[rpm, self.rc], writes=[r_rd])
                    em.op("dve", lambda: nc.vector.reciprocal(out=rd[:, :n], in_=rd[:, :n]), reads=[r_rd], writes=[r_rd])
                    ys, r_ys = yst.next()
                    em.op("dve", lambda: nc.vector.scalar_tensor_tensor(out=ys[:, :n], in0=o0[:, :n], scalar=sgc[:],
                                                                        in1=rd[:, :n], op0=ALU.mult, op1=ALU.mult),
                          reads=[r0, r_rd, rs_], writes=[r_ys])
                    em.dma("act", self.yT[h * 128:(h + 1) * 128, t0:t0 + n], ys[:, :n], reads=[r_ys], writes=[self.r_yT])
            em.barrier()
            em.stack = old

    def seq_bounds(self, cond):
        return (0, CTX) if cond == 1 else (CTX, self.T)

    def phase_conv(self, l):
        em, nc = self.em, self.nc
        with contextlib.ExitStack() as st:
            em.stack, old = st, em.stack
            cw = em.sb([128, 4, 3], F32); rcw = Res()
            for k in range(3):
                em.dma("sp", cw[:, :, k], self.inp["conv_w"][l, k].rearrange("(j p) -> p j", p=128), writes=[rcw],
                       allow_slow_non_contiguous=True)
            cgr = Rot(em, 2, [128, 514], F32); xvr = Rot(em, 2, [128, 514], F32); bgr = Rot(em, 2, [128, 512], F32)
            ur = Rot(em, 2, [128, 514], F32); ar = Rot(em, 2, [128, 512], F32); yst = Rot(em, 2, [128, 512], BF16)
            for j in range(4):
                for (t0, n, cond) in self.chunks():
                    lo, hi = self.seq_bounds(cond)
                    a0 = max(lo, t0 - 1); a1 = min(hi, t0 + n + 1)
                    off = a0 - (t0 - 1)
                    cg, r_cg = cgr.next(); xv, r_xv = xvr.next(); bg, r_bg = bgr.next()
                    em.op("pool", lambda: nc.gpsimd.memset(cg[:], 0.0), writes=[r_cg])
                    em.dma("sp", cg[:, off:off + (a1 - a0)], self.projT[3840 + j * 128:3840 + (j + 1) * 128, a0:a1],
                           reads=[self.r_projT], writes=[r_cg])
                    em.dma("sp", xv[:, off:off + (a1 - a0)], self.projT[4352 + j * 128:4352 + (j + 1) * 128, a0:a1],
                           reads=[self.r_projT], writes=[r_xv])
                    em.dma("sp", bg[:, :n], self.projT[3328 + j * 128:3328 + (j + 1) * 128, t0:t0 + n],
                           reads=[self.r_projT], writes=[r_bg])
                    u, r_u = ur.next()
                    em.op("pool", lambda: nc.gpsimd.tensor_tensor(out=u[:, off:off + (a1 - a0)], in0=cg[:, off:off + (a1 - a0)],
                                                                  in1=xv[:, off:off + (a1 - a0)], op=ALU.mult),
                          reads=[r_cg, r_xv], writes=[r_u])
                    if off > 0:
                        em.op("pool", lambda: nc.gpsimd.memset(u[:, 0:1], 0.0), writes=[r_u])
                    if off + (a1 - a0) < n + 2:
                        em.op("pool", lambda: nc.gpsimd.memset(u[:, n + 1:n + 2], 0.0), writes=[r_u])
                    a, r_a = ar.next()
                    em.op("dve", lambda: nc.vector.tensor_scalar(out=a[:, :n], in0=u[:, 0:n], scalar1=cw[:, j, 0:1], scalar2=None,
                                                                 op0=ALU.mult), reads=[r_u, rcw], writes=[r_a])
                    em.op("dve", lambda: nc.vector.scalar_tensor_tensor(out=a[:, :n], in0=u[:, 1:n + 1], scalar=cw[:, j, 1:2],
                                                                        in1=a[:, :n], op0=ALU.mult, op1=ALU.add),
                          reads=[r_u, rcw, r_a], writes=[r_a])
                    em.op("dve", lambda: nc.vector.scalar_tensor_tensor(out=a[:, :n], in0=u[:, 2:n + 2], scalar=cw[:, j, 2:3],
                                                                        in1=a[:, :n], op0=ALU.mult, op1=ALU.add),
                          reads=[r_u, rcw, r_a], writes=[r_a])
                    ys, r_ys = yst.next()
                    em.op("pool", lambda: nc.gpsimd.tensor_tensor(out=ys[:, :n], in0=a[:, :n], in1=bg[:, :n], op=ALU.mult),
                          reads=[r_a, r_bg], writes=[r_ys])
                    em.dma("act", self.yT[1024 + j * 128:1024 + (j + 1) * 128, t0:t0 + n], ys[:, :n],
                           reads=[r_ys], writes=[self.r_yT])
            em.barrier()
            em.stack = old

    def phase_rwkv(self, l):
        em, nc = self.em, self.nc
        T = self.T
        NCH = T // 128
        I64 = self.ident[0:64, 0:64]
        O64 = self.ones[0:64, 0:64]
        with contextlib.ExitStack() as st:
            em.stack, old = st, em.stack
            rs_ = Res("rwkv_setup")
            msk = [em.sb([128, 512], F32), em.sb([128, 512], F32)]
            em.dma("sp", msk[0][:], self.inp["cst_mskf"][:, :], writes=[rs_])
            em.dma("sp", msk[1][:], self.inp["cst_mskb"][:, :], writes=[rs_])
            rst = em.sb([64, 512], F32)
            em.dma("sp", rst[:], self.inp["cst_rst"][:, :], writes=[rs_])
            wup = em.sb([64, 2, 512], F32); aup = em.sb([64, 2, 512], F32); gup = em.sb([128, 512], F32)
            for d in range(2):
                em.dma("sp", wup[:, d, :], self.inp["rwkv_w_up"][l, d], writes=[rs_])
                em.dma("sp", aup[:, d, :], self.inp["rwkv_a_up"][l, d], writes=[rs_])
            em.dma("sp", gup[:], self.inp["rwkv_g_up"][l], writes=[rs_])
            pc = em.sb([64, 16], F32); rpc = Res()
            ysum = em.sb([64, T], F32); rys = Res()
            Qb = Rot(em, 2, [64, 512], F32); Yb = Rot(em, 2, [64, 512], F32)
            Gb = Rot(em, 2, [64, 4, 64], F32); Hb = Rot(em, 2, [64, 4, 64], F32)
            pdc = em.sb([64, 2], F32); r_pdc = Res()
            bk = [em.ps([128, 512], F32) for _ in range(8)]
            RB = [Res("psum_bank%d" % i, excl=True) for i in range(8)]
            bA = [bk[0], bk[1]]; RA = [RB[0], RB[1]]
            bB = [bk[2], bk[3]]; RBl = [RB[2], RB[3]]
            bC = [bk[4], bk[5]]; RC = [RB[4], RB[5]]
            pmm = bk[6][0:64, :]; r_pmm = RB[6]
            prd = bk[6][0:64, :]; r_prd = RB[6]
            py = [bk[7][0:64, 0:128], bk[7][0:64, 128:256]]; pst = [bk[7][0:64, 256:320], bk[7][0:64, 320:384]]
            r_py = [RB[7], RB[7]]; r_pst = [RB[7], RB[7]]
            xh = [Rot(em, 1, [64, 514], F32) for _ in range(3)]
            cv = [Rot(em, 2, [64, 512], F32) for _ in range(3)]
            wlr = Rot(em, 2, [64, 512], F32); alr = Rot(em, 2, [64, 512], F32)
            kkr = Rot(em, 2, [64, 512], F32)
            tmps = {nm: (em.sb([64, 512], F32), Res()) for nm in ('sq', 'nr', 'ld', 'a', 'kd', 'bn', 'L', 'Lb', 'Ei', 'dl')}
            ARr = Rot(em, 2, [64, 4, 2, 128], F32)
            Bfr = Rot(em, 2, [64, 512], F32); Kfr = Rot(em, 2, [64, 512], F32)
            Bgr = Rot(em, 2, [64, 512], F32); Kgr = Rot(em, 2, [64, 512], F32)
            Er = Rot(em, 2, [64, 512], F32)
            dgr = [Rot(em, 1, [64, 64], F32) for _ in range(2)]
            tokr = [Rot(em, 1, [128, 256], F32) for _ in range(2)]
            NBr = [Rot(em, 1, [128, 512], F32) for _ in range(2)]
            NTr = [Rot(em, 3, [128, 128], F32) for _ in range(2)]
            Pr_ = [Rot(em, 3, [128, 128], F32) for _ in range(2)]
            Zr = [Rot(em, 3, [128, 128], F32) for _ in range(2)]
            Str = Rot(em, 2, [64, 64], F32)
            glr = Rot(em, 2, [128, 512], F32)
            ybr = Rot(em, 2, [64, 512], BF16)

            def V(e):
                return nc.vector if e == "dve" else nc.gpsimd

            for h in range(8):
                hs = slice(h * 64, (h + 1) * 64)
                cwv = self.inp["rwkv_conv_w"][l]
                for q in range(3):
                    for k in range(3):
                        em.dma("sp", pc[:, q * 3 + k:q * 3 + k + 1],
                               cwv[k, q * 512 + h * 64:q * 512 + (h + 1) * 64].rearrange("(p o) -> p o", o=1), writes=[rpc])
                for i, nm in ((9, "rwkv_k_k"), (10, "rwkv_k_a"), (12, "rwkv_r_k"), (13, "rwkv_ln_g"), (14, "rwkv_ln_b")):
                    em.dma("sp", pc[:, i:i + 1], self.inp[nm][l, hs].rearrange("(p o) -> p o", o=1), writes=[rpc])
                em.op("dve", lambda: nc.vector.tensor_scalar(out=pc[:, 11:12], in0=pc[:, 10:11], scalar1=-1.0, scalar2=1.0,
                                                             op0=ALU.mult, op1=ALU.add), reads=[rpc], writes=[rpc])
                for d in range(2):
                    em.dma("sp", pdc[:, 0:1], self.inp["rwkv_w0"][l, d, hs].rearrange("(p o) -> p o", o=1), writes=[r_pdc])
                    em.dma("sp", pdc[:, 1:2], self.inp["rwkv_a0"][l, d, hs].rearrange("(p o) -> p o", o=1), writes=[r_pdc])
                    msk_d = msk[d]
                    mskT = msk[1 - d][:, 0:128]
                    chs = self.chunks()
                    blocks = chs if d == 0 else [chs[0]] + chs[:0:-1]
                    S, r_S = Str.next()
                    em.op("pool", lambda: nc.gpsimd.memset(S[:], 0.0), writes=[r_S])
                    for (t0, n, cond) in blocks:
                        nch = n // 128
                        Qs, rQs = Qb.next(); Ys, rYs = Yb.next(); Gs, rGs = Gb.next(); Hs, rHs = Hb.next()
                        lo, hi = self.seq_bounds(cond)
                        a0_ = max(lo, t0 - 1); a1_ = min(hi, t0 + n + 1)
                        off = a0_ - (t0 - 1); ln_ = a1_ - a0_
                        cvt = []
                        for q in range(3):
                            x_, r_x = xh[q].next()
                            if off > 0 or off + ln_ < n + 2:
                                em.op("pool", lambda: nc.gpsimd.memset(x_[:], 0.0), writes=[r_x])
                            row = 1536 + q * 512 + h * 64
                            em.dma("sp", x_[:, off:off + ln_], self.projT[row:row + 64, a0_:a1_], reads=[self.r_projT], writes=[r_x])
                            c_, r_c = cv[q].next()
                            e = "dve" if q != 1 else "pool"
                            em.op(e, lambda: V(e).tensor_scalar(out=c_[:, :n], in0=x_[:, 0:n], scalar1=pc[:, q * 3:q * 3 + 1], scalar2=None,
                                                                op0=ALU.mult), reads=[r_x, rpc], writes=[r_c])
                            for k in (1, 2):
                                em.op("dve", lambda: nc.vector.scalar_tensor_tensor(out=c_[:, :n], in0=x_[:, k:n + k],
                                                                                    scalar=pc[:, q * 3 + k:q * 3 + k + 1], in1=c_[:, :n],
                                                                                    op0=ALU.mult, op1=ALU.add),
                                      reads=[r_x, rpc, r_c], writes=[r_c])
                            cvt.append((c_, r_c))
                        (r_, r_r), (k_, r_k), (v_, r_v) = cvt
                        wl_, r_wl = wlr.next(); al_, r_al = alr.next()
                        em.dma("sp", wl_[:, :n], self.projT[3072:3136, t0:t0 + n], reads=[self.r_projT], writes=[r_wl])
                        em.dma("sp", al_[:, :n], self.projT[3136:3200, t0:t0 + n], reads=[self.r_projT], writes=[r_al])
                        em.op("act", lambda: nc.scalar.activation(out=wl_[:, :n], in_=wl_[:, :n], func=AF.Tanh), reads=[r_wl], writes=[r_wl])
                        kk, r_kk = kkr.next()
                        em.op("pool", lambda: nc.gpsimd.tensor_scalar(out=kk[:, :n], in0=k_[:, :n], scalar1=pc[:, 9:10], scalar2=None, op0=ALU.mult),
                              reads=[r_k, rpc], writes=[r_kk])
                        sq, r_sq = tmps['sq']
                        em.op("act", lambda: nc.scalar.activation(out=sq[:, :n], in_=kk[:, :n], func=AF.Square), reads=[r_kk], writes=[r_sq])
                        em.op("pe", lambda: nc.tensor.matmul(pmm[:, :n], lhsT=O64, rhs=sq[:, :n], start=True, stop=True),
                              reads=[r_sq, self.rc], writes=[r_pmm])
                        nr, r_nr = tmps['nr']
                        em.op("act", lambda: nc.scalar.activation(out=nr[:, :n], in_=pmm[:, :n], func=AF.Sqrt), reads=[r_pmm], writes=[r_nr])
                        em.op("dve", lambda: nc.vector.tensor_scalar(out=nr[:, :n], in0=nr[:, :n], scalar1=1e-12, scalar2=None, op0=ALU.max),
                              reads=[r_nr], writes=[r_nr])
                        em.op("dve", lambda: nc.vector.reciprocal(out=nr[:, :n], in_=nr[:, :n]), reads=[r_nr], writes=[r_nr])
                        em.op("dve", lambda: nc.vector.tensor_tensor(out=kk[:, :n], in0=kk[:, :n], in1=nr[:, :n], op=ALU.mult),
                              reads=[r_kk, r_nr], writes=[r_kk])
                        em.op("pe", lambda: nc.tensor.matmul(pmm[:, :n], lhsT=wup[:, d, hs], rhs=wl_[:, :n], start=True, stop=True),
                              reads=[rs_, r_wl], writes=[r_pmm])
                        ld, r_ld = tmps['ld']
                        em.op("act", lambda: nc.scalar.activation(out=ld[:, :n], in_=pmm[:, :n], func=AF.Sigmoid, bias=pdc[:, 0:1], scale=1.0),
                              reads=[r_pmm, r_pdc], writes=[r_ld])
                        em.op("dve", lambda: nc.vector.tensor_scalar(out=ld[:, :n], in0=ld[:, :n], scalar1=-0.6065306597126334, scalar2=None,
                                                                     op0=ALU.mult), reads=[r_ld], writes=[r_ld])
                        em.op("pe", lambda: nc.tensor.matmul(pmm[:, :n], lhsT=aup[:, d, hs], rhs=al_[:, :n], start=True, stop=True),
                              reads=[rs_, r_al], writes=[r_pmm])
                        a_, r_a = tmps['a']
                        em.op("act", lambda: nc.scalar.activation(out=a_[:, :n], in_=pmm[:, :n], func=AF.Sigmoid, bias=pdc[:, 1:2], scale=1.0),
                              reads=[r_pmm, r_pdc], writes=[r_a])
                        kd, r_kd = tmps['kd']
                        em.op("dve", lambda: nc.vector.tensor_scalar(out=kd[:, :n], in0=a_[:, :n], scalar1=pc[:, 10:11], scalar2=pc[:, 11:12],
                                                                     op0=ALU.mult, op1=ALU.add), reads=[r_a, rpc], writes=[r_kd])
                        em.op("dve", lambda: nc.vector.tensor_tensor(out=kd[:, :n], in0=kd[:, :n], in1=k_[:, :n], op=ALU.mult),
                              reads=[r_kd, r_k], writes=[r_kd])
                        em.op("pool", lambda: nc.gpsimd.tensor_tensor(out=a_[:, :n], in0=a_[:, :n], in1=kk[:, :n], op=ALU.mult),
                              reads=[r_a, r_kk], writes=[r_a])
                        b_, r_b = a_, r_a
                        bn, r_bn = tmps['bn']
                        em.op("dve", lambda: nc.vector.scalar_tensor_tensor(out=bn[:, :n], in0=r_[:, :n], scalar=pc[:, 12:13], in1=kd[:, :n],
                                                                            op0=ALU.mult, op1=ALU.mult), reads=[r_r, rpc, r_kd], writes=[r_bn])
                        em.op("pe", lambda: nc.tensor.matmul(pmm[:, :n], lhsT=O64, rhs=bn[:, :n], start=True, stop=True),
                              reads=[r_bn, self.rc], writes=[r_pmm])
                        if d == 0:
                            em.op("dve", lambda: nc.vector.tensor_tensor(out=ysum[:, t0:t0 + n], in0=pmm[:, :n], in1=v_[:, :n], op=ALU.mult),
                                  reads=[r_pmm, r_v], writes=[rys])
                        else:
                            em.op("dve", lambda: nc.vector.tensor_tensor(out=bn[:, :n], in0=pmm[:, :n], in1=v_[:, :n], op=ALU.mult),
                                  reads=[r_pmm, r_v], writes=[r_bn])
                            em.op("pool", lambda: nc.gpsimd.tensor_tensor(out=ysum[:, t0:t0 + n], in0=ysum[:, t0:t0 + n], in1=bn[:, :n], op=ALU.add),
                                  reads=[r_bn, rys], writes=[rys])
                        L, r_L = tmps['L']
                        em.op("dve", lambda: nc.vector.tensor_tensor_scan(out=L[:, :n], data0=rst[:, :n], data1=ld[:, :n], initial=0.0,
                                                                          op0=ALU.mult, op1=ALU.add), reads=[rs_, r_ld], writes=[r_L])
                        if d == 1:
                            L3 = L[:, :n].rearrange("p (c t) -> p c t", t=128)
                            tot = L3[:, :, 127:128].to_broadcast([64, nch, 128])
                            Lb, r_Lb = tmps['Lb']
                            Lb3 = Lb[:, :n].rearrange("p (c t) -> p c t", t=128)
                            em.op("dve", lambda: nc.vector.tensor_tensor(out=Lb3, in0=tot, in1=L3, op=ALU.subtract), reads=[r_L], writes=[r_Lb])
                            em.op("dve", lambda: nc.vector.tensor_tensor(out=Lb[:, :n], in0=Lb[:, :n], in1=ld[:, :n], op=ALU.add),
                                  reads=[r_Lb, r_ld], writes=[r_Lb])
                            L, r_L = Lb, r_Lb
                        E, r_E = Er.next()
                        em.op("act", lambda: nc.scalar.activation(out=E[:, :n], in_=L[:, :n], func=AF.Exp), reads=[r_L], writes=[r_E])
                        Ei, r_Ei = tmps['Ei']
                        em.op("act", lambda: nc.scalar.activation(out=Ei[:, :n], in_=L[:, :n], func=AF.Exp, scale=-1.0), reads=[r_L], writes=[r_Ei])
                        em.op("dve", lambda: nc.vector.tensor_tensor(out=ld[:, :n], in0=L[:, :n], in1=ld[:, :n], op=ALU.subtract),
                              reads=[r_L, r_ld], writes=[r_ld])
                        em.op("act", lambda: nc.scalar.activation(out=ld[:, :n], in_=ld[:, :n], func=AF.Exp), reads=[r_ld], writes=[r_ld])
                        AR, r_AR = ARr.next()
                        kk3 = kk[:, :n].rearrange("p (c t) -> p c t", t=128)
                        ep3 = ld[:, :n].rearrange("p (c t) -> p c t", t=128)
                        em.op("dve", lambda: nc.vector.scalar_tensor_tensor(out=AR[:, :nch, 0, :], in0=kk3, scalar=-1.0, in1=ep3,
                                                                            op0=ALU.mult, op1=ALU.mult), reads=[r_kk, r_ld], writes=[r_AR])
                        em.op("pool", lambda: nc.gpsimd.tensor_tensor(out=AR[:, :nch, 1, :], in0=r_[:, :n].rearrange("p (c t) -> p c t", t=128),
                                                                      in1=E[:, :n].rearrange("p (c t) -> p c t", t=128), op=ALU.mult),
                              reads=[r_r, r_E], writes=[r_AR])
                        Bf, r_Bf = Bfr.next(); Kf, r_Kf = Kfr.next()
                        em.op("dve", lambda: nc.vector.tensor_tensor(out=Bf[:, :n], in0=b_[:, :n], in1=Ei[:, :n], op=ALU.mult),
                              reads=[r_b, r_Ei], writes=[r_Bf])
                        em.op("pool", lambda: nc.gpsimd.tensor_tensor(out=Kf[:, :n], in0=kd[:, :n], in1=Ei[:, :n], op=ALU.mult),
                              reads=[r_kd, r_Ei], writes=[r_Kf])
                        gidx = 127 if d == 0 else 0
                        gC = E[:, :n].rearrange("p (c t) -> p c t", t=128)[:, :, gidx:gidx + 1]
                        Bg, r_Bg = Bgr.next(); Kg, r_Kg = Kgr.next()
                        em.op("dve", lambda: nc.vector.tensor_tensor(out=Bg[:, :n].rearrange("p (c t) -> p c t", t=128),
                                                                     in0=Bf[:, :n].rearrange("p (c t) -> p c t", t=128),
                                                                     in1=gC.to_broadcast([64, nch, 128]), op=ALU.mult),
                              reads=[r_Bf, r_E], writes=[r_Bg])
                        em.op("dve", lambda: nc.vector.tensor_tensor(out=Kg[:, :n].rearrange("p (c t) -> p c t", t=128),
                                                                     in0=Kf[:, :n].rearrange("p (c t) -> p c t", t=128),
                                                                     in1=gC.to_broadcast([64, nch, 128]), op=ALU.mult),
                              reads=[r_Kf, r_E], writes=[r_Kg])
                        def chunk_steps(ci, lane):
                            pBK = bA[lane]; r_pBK = RA[lane]
                            pQ = bA[lane][0:64, 0:128]; pYi = bA[lane][0:64, 128:256]
                            pG = bA[lane][0:64, 256:320]; pH = bA[lane][0:64, 320:384]
                            r_pQ = r_pYi = r_pG = r_pH = RA[lane]
                            ptr = bB[lane][:, 0:256]; pNT = bB[lane][:, 256:384]; pX1 = bB[lane][:, 384:448]
                            r_ptr = r_pNT = r_pX1 = RBl[lane]
                            pzl = bC[lane][:, 0:128]; ppl = bC[lane][:, 128:256]; pptl = bC[lane][:, 256:384]
                            g = t0 // 128 + ci
                            cs_ = slice(ci * 128, (ci + 1) * 128)
                            Af = AR[:, ci, 0, :]; Rf = AR[:, ci, 1, :]
                            for qi, (src_, rsrc) in enumerate(((Af, r_AR), (Bg[:, cs_], r_Bg), (Kg[:, cs_], r_Kg), (v_[:, cs_], r_v))):
                                em.op("pe", lambda: nc.tensor.transpose(out=ptr[:, qi * 64:(qi + 1) * 64], in_=src_, identity=I64),
                                      reads=[rsrc, self.rc], writes=[r_ptr])
                            tok, r_tok = tokr[lane].next()
                            em.op("act", lambda: nc.scalar.copy(out=tok[:], in_=ptr), reads=[r_ptr], writes=[r_tok])
                            yield
                            At = tok[:, 0:64]; Bgt = tok[:, 64:128]; Kgt = tok[:, 128:192]; Vt = tok[:, 192:256]
                            ARc = AR[:, ci].rearrange("p a t -> p (a t)")
                            em.op("pe", lambda: nc.tensor.matmul(pBK[:, 0:256], lhsT=Bf[:, cs_], rhs=ARc, start=True, stop=True),
                                  reads=[r_Bf, r_AR], writes=[r_pBK])
                            em.op("pe", lambda: nc.tensor.matmul(pBK[:, 256:512], lhsT=Kf[:, cs_], rhs=ARc, start=True, stop=True),
                                  reads=[r_Kf, r_AR], writes=[r_pBK])
                            NB, r_NB = NBr[lane].next()
                            em.op("dve", lambda: nc.vector.tensor_tensor(out=NB[:], in0=pBK[:], in1=msk_d[:], op=ALU.mult),
                                  reads=[r_pBK, rs_], writes=[r_NB])
                            yield
                            N_ = NB[:, 0:128]; Mb = NB[:, 128:256]; Mk = NB[:, 256:384]; Mr = NB[:, 384:512]
                            em.op("pe", lambda: nc.tensor.matmul(pNT, lhsT=Af, rhs=Bf[:, cs_], start=True, stop=True),
                                  reads=[r_AR, r_Bf], writes=[r_pNT])
                            NT, r_NT = NTr[lane].next()
                            em.op("dve", lambda: nc.vector.tensor_tensor(out=NT[:], in0=pNT, in1=mskT, op=ALU.mult),
                                  reads=[r_pNT, rs_], writes=[r_NT])
                            yield
                            em.op("pe", lambda: nc.tensor.matmul(pX1, lhsT=Mk, rhs=Vt, start=True, stop=True),
                                  reads=[r_NB, r_tok], writes=[r_pX1])
                            Z, r_Z = Zr[lane].next()
                            em.op("act", lambda: nc.scalar.copy(out=Z[:, 0:64], in_=pX1), reads=[r_pX1], writes=[r_Z])
                            em.op("pool", lambda: nc.gpsimd.tensor_copy(out=Z[:, 64:128], in_=At), reads=[r_tok], writes=[r_Z])
                            yield
                            P, r_P = N_, r_NB
                            PT, r_PT = NT[:], r_NT
                            for it in range(7):
                                i2 = it % 2
                                em.op("pe", lambda: nc.tensor.matmul(pzl, lhsT=P, rhs=Z[:], start=True, stop=True),
                                      reads=[r_P, r_Z], writes=[RC[lane]])
                                if it < 6:
                                    em.op("pe", lambda: nc.tensor.matmul(ppl, lhsT=PT, rhs=P, start=True, stop=True),
                                          reads=[r_P, r_PT], writes=[RC[lane]])
                                    em.op("pe", lambda: nc.tensor.matmul(pptl, lhsT=P, rhs=PT, start=True, stop=True),
                                          reads=[r_P, r_PT], writes=[RC[lane]])
                                yield
                                Zn, r_Zn = Zr[lane].next()
                                em.op("dve", lambda: nc.vector.tensor_tensor(out=Zn[:], in0=pzl, in1=Z[:], op=ALU.add),
                                      reads=[RC[lane], r_Z], writes=[r_Zn])
                                Z, r_Z = Zn, r_Zn
                                if it < 6:
                                    Pn, r_Pn = Pr_[lane].next()
                                    em.op("act", lambda: nc.scalar.copy(out=Pn[:], in_=ppl), reads=[RC[lane]], writes=[r_Pn])
                                    PTn, r_PTn = NTr[lane].next()
                                    em.op("dve", lambda: nc.vector.tensor_copy(out=PTn[:], in_=pptl), reads=[RC[lane]], writes=[r_PTn])
                                    P, r_P = Pn[:], r_Pn
                                    PT, r_PT = PTn[:], r_PTn
                            yield
                            Wt = Z[:, 0:64]; Apt = Z[:, 64:128]
                            em.op("pe", lambda: nc.tensor.matmul(pQ, lhsT=Apt, rhs=Mb, start=True, stop=False),
                                  reads=[r_Z, r_NB], writes=[r_pQ])
                            em.op("pe", lambda: nc.tensor.matmul(pQ, lhsT=I64, rhs=Rf, start=False, stop=True),
                                  reads=[r_AR, self.rc], writes=[r_pQ])
                            em.op("act", lambda: nc.scalar.copy(out=Qs[:, cs_], in_=pQ), reads=[r_pQ], writes=[rQs])
                            yield
                            em.op("pe", lambda: nc.tensor.matmul(pYi, lhsT=Wt, rhs=Mb, start=True, stop=False),
                                  reads=[r_Z, r_NB], writes=[r_pYi])
                            em.op("pe", lambda: nc.tensor.matmul(pYi, lhsT=Vt, rhs=Mr, start=False, stop=True),
                                  reads=[r_tok, r_NB], writes=[r_pYi])
                            em.op("dve", lambda: nc.vector.tensor_copy(out=Ys[:, cs_], in_=pYi), reads=[r_pYi], writes=[rYs])
                            yield
                            dg, r_dg = dgr[lane].next()
                            em.op("pool", lambda: nc.gpsimd.tensor_scalar(out=dg[:], in0=I64, scalar1=gC[:, ci, :], scalar2=None, op0=ALU.mult),
                                  reads=[self.rc, r_E], writes=[r_dg])
                            em.op("pe", lambda: nc.tensor.matmul(pG, lhsT=Apt, rhs=Bgt, start=True, stop=False),
                                  reads=[r_Z, r_tok], writes=[r_pG])
                            em.op("pe", lambda: nc.tensor.matmul(pG, lhsT=I64, rhs=dg[:], start=False, stop=True),
                                  reads=[r_dg, self.rc], writes=[r_pG])
                            em.op("act", lambda: nc.scalar.copy(out=Gs[:, ci, :], in_=pG), reads=[r_pG], writes=[rGs])
                            yield
                            em.op("pe", lambda: nc.tensor.matmul(pH, lhsT=Kgt, rhs=Vt, start=True, stop=False),
                                  reads=[r_tok], writes=[r_pH])
                            em.op("pe", lambda: nc.tensor.matmul(pH, lhsT=Bgt, rhs=Wt, start=False, stop=True),
                                  reads=[r_tok, r_Z], writes=[r_pH])
                            em.op("dve", lambda: nc.vector.tensor_copy(out=Hs[:, ci, :], in_=pH), reads=[r_pH], writes=[rHs])
                            yield
                        if RWS >= 2:
                            for c0 in range(0, nch, 2):
                                gens = [chunk_steps(c0 + k, k) for k in range(min(2, nch - c0))]
                                while gens:
                                    for g_ in list(gens):
                                        try:
                                            next(g_)
                                        except StopIteration:
                                            gens.remove(g_)

                        order = list(range(nch)) if d == 0 else list(range(nch - 1, -1, -1))
                        if RWS < 3:
                            continue
                        for ci in order:
                            i2 = ci % 2
                            gs = slice(ci * 128, (ci + 1) * 128)
                            em.op("pe", lambda: nc.tensor.matmul(py[i2], lhsT=S[:], rhs=Qs[:, gs], start=True, stop=True),
                                  reads=[r_S, rQs], writes=[r_py[i2]])
                            em.op("pe", lambda: nc.tensor.matmul(pst[i2], lhsT=Gs[:, ci, :], rhs=S[:], start=True, stop=True),
                                  reads=[r_S, rGs], writes=[r_pst[i2]])
                            Sn, r_Sn = Str.next()
                            em.op("dve", lambda: nc.vector.tensor_tensor(out=Sn[:], in0=pst[i2], in1=Hs[:, ci, :], op=ALU.add),
                                  reads=[r_pst[i2], rHs], writes=[r_Sn])
                            em.op("dve", lambda: nc.vector.tensor_tensor(out=Ys[:, gs], in0=py[i2], in1=Ys[:, gs], op=ALU.add),
                                  reads=[r_py[i2], rYs], writes=[rYs])
                            S, r_S = Sn, r_Sn
                        o_ = Ys[:, :n]
                        em.op("pe", lambda: nc.tensor.matmul(prd[:, :n], lhsT=O64, rhs=o_, start=True, stop=True),
                              reads=[rYs, self.rc], writes=[r_prd])
                        dl, r_dl = tmps['dl']
                        em.op("dve", lambda: nc.vector.scalar_tensor_tensor(out=dl[:, :n], in0=prd[:, :n], scalar=-1.0 / 64, in1=o_,
                                                                            op0=ALU.mult, op1=ALU.add), reads=[r_prd, rYs], writes=[r_dl])
                        sq, r_sq = tmps['sq']
                        em.op("act", lambda: nc.scalar.activation(out=sq[:, :n], in_=dl[:, :n], func=AF.Square), reads=[r_dl], writes=[r_sq])
                        em.op("pe", lambda: nc.tensor.matmul(prd[:, :n], lhsT=O64, rhs=sq[:, :n], start=True, stop=True),
                              reads=[r_sq, self.rc], writes=[r_prd])
                        em.op("act", lambda: nc.scalar.activation(out=sq[:, :n], in_=prd[:, :n], func=AF.Sqrt, scale=1.0 / 64,
                                                                  bias=self.eps_col(GN_EPS)[0:64, :]), reads=[r_prd, self.rc], writes=[r_sq])
                        em.op("dve", lambda: nc.vector.reciprocal(out=sq[:, :n], in_=sq[:, :n]), reads=[r_sq], writes=[r_sq])
                        em.op("dve", lambda: nc.vector.tensor_tensor(out=dl[:, :n], in0=dl[:, :n], in1=sq[:, :n], op=ALU.mult),
                              reads=[r_dl, r_sq], writes=[r_dl])
                        em.op("dve", lambda: nc.vector.tensor_scalar(out=dl[:, :n], in0=dl[:, :n], scalar1=pc[:, 13:14], scalar2=pc[:, 14:15],
                                                                     op0=ALU.mult, op1=ALU.add), reads=[r_dl, rpc], writes=[r_dl])
                        em.op("pool", lambda: nc.gpsimd.tensor_tensor(out=ysum[:, t0:t0 + n], in0=ysum[:, t0:t0 + n], in1=dl[:, :n], op=ALU.add),
                              reads=[r_dl, rys], writes=[rys])
                for (t0, n, cond) in self.chunks():
                    gl, r_gl = glr.next()
                    em.dma("sp", gl[:, :n], self.projT[3200:3328, t0:t0 + n], reads=[self.r_projT], writes=[r_gl])
                    em.op("act", lambda: nc.scalar.activation(out=gl[:, :n], in_=gl[:, :n], func=AF.Sigmoid), reads=[r_gl], writes=[r_gl])
                    em.op("pe", lambda: nc.tensor.matmul(prd[:, :n], lhsT=gup[:, hs], rhs=gl[:, :n], start=True, stop=True),
                          reads=[rs_, r_gl], writes=[r_prd])
                    yb, r_yb = ybr.next()
                    em.op("dve", lambda: nc.vector.tensor_tensor(out=yb[:, :n], in0=prd[:, :n], in1=ysum[:, t0:t0 + n], op=ALU.mult),
                          reads=[r_prd, rys], writes=[r_yb])
                    em.dma("act", self.yT[512 + h * 64:512 + (h + 1) * 64, t0:t0 + n], yb[:, :n], reads=[r_yb], writes=[self.r_yT])
            em.barrier()
            em.stack = old

    def phase_merge(self, l):
        em, nc = self.em, self.nc
        with contextlib.ExitStack() as st:
            em.stack, old = st, em.stack
            wbf = em.sb([128, 12, D], BF16); rwb = Res()
            wof = em.sb([128, 8, D], BF16); rwo = Res()
            wst = Rot(em, 2, [128, 4, D], F32)
            wbv = self.inp["w_branch"][l].rearrange("b (kt p) f -> p b kt f", p=128)
            for br in range(3):
                w, r_w = wst.next()
                em.dma("sp", w[:], wbv[:, br], writes=[r_w])
                em.op("pool", lambda: nc.gpsimd.tensor_copy(out=wbf[:, br * 4:(br + 1) * 4, :], in_=w[:]), reads=[r_w], writes=[rwb])
            wov = self.inp["w_out"][l].rearrange("(kt p) f -> p kt f", p=128)
            for hf in range(2):
                w, r_w = wst.next()
                em.dma("sp", w[:], wov[:, hf * 4:(hf + 1) * 4, :], writes=[r_w])
                em.op("pool", lambda: nc.gpsimd.tensor_copy(out=wof[:, hf * 4:(hf + 1) * 4, :], in_=w[:]), reads=[r_w], writes=[rwo])
            yin = Rot(em, 2, [128, 12, 512], BF16)
            gin = Rot(em, 3, [128, 512], F32)
            sgr = Rot(em, 3, [128, 512], F32)
            tmr = Rot(em, 3, [128, 512], F32)
            macc = Rot(em, 2, [128, 8, 512], F32)
            mbf = Rot(em, 2, [128, 8, 512], BF16)
            xin = Rot(em, 2, [128, 8, 512], F32)
            pm = Rot(em, 4, [128, 512], F32, psum=True)
            yTv = self.yT.rearrange("(kt p) t -> p kt t", p=128)
            xTv = self.xT.rearrange("(kt p) t -> p kt t", p=128)
            for (t0, n, cond) in self.chunks():
                y, r_y = yin.next()
                em.dma("sp", y[:, :, :n], yTv[:, :, t0:t0 + n], reads=[self.r_yT], writes=[r_y])
                xt, r_x = xin.next()
                em.dma("sp", xt[:, :, :n], xTv[:, :, t0:t0 + n], reads=[self.r_xT], writes=[r_x])
                ma, r_ma = macc.next()
                for d in range(8):
                    for br in range(3):
                        g, r_g = gin.next()
                        row = 4864 + br * 1024 + d * 128
                        em.dma("sp", g[:, :n], self.projT[row:row + 128, t0:t0 + n], reads=[self.r_projT], writes=[r_g])
                        sg, r_sg = sgr.next()
                        em.op("act", lambda: nc.scalar.activation(out=sg[:, :n], in_=g[:, :n], func=AF.Sigmoid),
                              reads=[r_g], writes=[r_sg])
                        pp, r_pp = pm.next()
                        for kt in range(4):
                            em.op("pe", lambda: nc.tensor.matmul(pp[:, :n], lhsT=wbf[:, br * 4 + kt, d * 128:(d + 1) * 128],
                                                                 rhs=y[:, br * 4 + kt, :n], start=(kt == 0), stop=(kt == 3)),
                                  reads=[rwb, r_y], writes=[r_pp])
                        if br == 0:
                            em.op("dve", lambda: nc.vector.tensor_tensor(out=ma[:, d, :n], in0=pp[:, :n], in1=sg[:, :n], op=ALU.mult),
                                  reads=[r_pp, r_sg], writes=[r_ma])
                        else:
                            tm, r_tm = tmr.next()
                            em.op("dve", lambda: nc.vector.tensor_tensor(out=tm[:, :n], in0=pp[:, :n], in1=sg[:, :n], op=ALU.mult),
                                  reads=[r_pp, r_sg], writes=[r_tm])
                            em.op("pool", lambda: nc.gpsimd.tensor_tensor(out=ma[:, d, :n], in0=ma[:, d, :n], in1=tm[:, :n], op=ALU.add),
                                  reads=[r_tm, r_ma], writes=[r_ma])
                mb, r_mb = mbf.next()
                em.op("act", lambda: nc.scalar.copy(out=mb[:, :, :n], in_=ma[:, :, :n]), reads=[r_ma], writes=[r_mb])
                for d in range(8):
                    pp, r_pp = pm.next()
                    for kt in range(8):
                        em.op("pe", lambda: nc.tensor.matmul(pp[:, :n], lhsT=wof[:, kt, d * 128:(d + 1) * 128], rhs=mb[:, kt, :n],
                                                             start=(kt == 0), stop=(kt == 7)), reads=[rwo, r_mb], writes=[r_pp])
                    em.op("dve", lambda: nc.vector.scalar_tensor_tensor(out=xt[:, d, :n], in0=pp[:, :n],
                                                                        scalar=self.modc[:, 2, d, cond:cond + 1], in1=xt[:, d, :n],
                                                                        op0=ALU.mult, op1=ALU.add),
                          reads=[r_pp, self.r_mod, r_x], writes=[r_x])
                em.dma("act", xTv[:, :, t0:t0 + n], xt[:, :, :n], reads=[r_x], writes=[self.r_xT])
            em.barrier()
            em.stack = old

    def phase_ada(self, l):
        em, nc = self.em, self.nc
        if l == 0:
            self.modc = em.sb([128, 6, 8, 2], F32)
            self.r_mod = Res("mod")
            self.cs = em.sb([128, 2, 8], F32)
            self.r_cs = Res("cs")
            ctmp = em.sb([128, 2, 8], F32)
            rct = Res()
            em.dma("sp", ctmp[:, 0, :], self.inp["c"].rearrange("(kt p) -> p kt", p=128), writes=[rct],
                   allow_slow_non_contiguous=True)
            em.dma("sp", ctmp[:, 1, :], self.inp["c_ctx"].rearrange("(kt p) -> p kt", p=128), writes=[rct],
                   allow_slow_non_contiguous=True)
            em.op("act", lambda: nc.scalar.activation(out=self.cs[:], in_=ctmp[:], func=AF.Silu),
                  reads=[rct], writes=[self.r_cs])
        with contextlib.ExitStack() as st:
            em.stack, old = st, em.stack
            wbuf = Rot(em, 2, [128, 8, 768], F32)
            pm = em.ps([128, 48, 2], F32); rpm = Res()
            adab = em.sb([128, 48], F32); rab = Res()
            g12 = em.sb([128, 2, 8], F32); rg = Res()
            em.dma("sp", adab[:], self.inp["ada_b"][l].rearrange("(j p) -> p j", p=128), writes=[rab],
                   allow_slow_non_contiguous=True)
            em.dma("sp", g12[:, 0, :], self.inp["norm1_g"][l].rearrange("(j p) -> p j", p=128), writes=[rg],
                   allow_slow_non_contiguous=True)
            em.dma("sp", g12[:, 1, :], self.inp["norm2_g"][l].rearrange("(j p) -> p j", p=128), writes=[rg],
                   allow_slow_non_contiguous=True)
            awv = self.inp["ada_w"][l].rearrange("(kt p) f -> p kt f", p=128)
            for ch in range(8):
                wt, rw = wbuf.next()
                em.dma("sp", wt[:], awv[:, :, ch * 768:(ch + 1) * 768], writes=[rw])
                for jj in range(6):
                    j = ch * 6 + jj
                    for kt in range(8):
                        em.op("pe", lambda: nc.tensor.matmul(pm[:, j, :], lhsT=wt[:, kt, jj * 128:(jj + 1) * 128],
                                                             rhs=self.cs[:, :, kt], start=(kt == 0), stop=(kt == 7)),
                              reads=[rw, self.r_cs], writes=[rpm])
            modraw = em.sb([128, 48, 2], F32); rmr = Res()
            em.op("dve", lambda: nc.vector.tensor_tensor(out=modraw[:], in0=pm[:],
                                                         in1=adab[:].unsqueeze(2).to_broadcast([128, 48, 2]),
                                                         op=ALU.add),
                  reads=[rpm, rab], writes=[rmr])
            mc = self.modc
            mr = modraw[:].rearrange("p (k j) c -> p k j c", k=6)
            for half in range(2):
                sh, sc, gt = mr[:, 3 * half + 0], mr[:, 3 * half + 1], mr[:, 3 * half + 2]
                gn = g12[:, half, :].unsqueeze(2).to_broadcast([128, 8, 2])
                em.op("dve", lambda: nc.vector.scalar_tensor_tensor(out=mc[:, 3 * half + 0], in0=sc, scalar=1.0, in1=gn,
                                                                    op0=ALU.add, op1=ALU.mult),
                      reads=[rmr, rg], writes=[self.r_mod])
                em.op("dve", lambda: nc.vector.tensor_copy(out=mc[:, 3 * half + 1], in_=sh),
                      reads=[rmr], writes=[self.r_mod])
                em.op("dve", lambda: nc.vector.tensor_copy(out=mc[:, 3 * half + 2], in_=gt),
                      reads=[rmr], writes=[self.r_mod])
            em.barrier()
            em.stack = old

    def phase_norm(self, l, which):
        em, nc = self.em, self.nc
        ka, kb = (0, 1) if which == 1 else (3, 4)
        with contextlib.ExitStack() as st:
            em.stack, old = st, em.stack
            xin = Rot(em, 2, [128, 8, 512], F32)
            sq = Rot(em, 2, [128, 8, 512], F32)
            pss = Rot(em, 2, [128, 512], F32, psum=True)
            rsd = Rot(em, 2, [128, 512], F32)
            tmp = Rot(em, 3, [128, 512], F32)
            hbf = Rot(em, 2, [128, 8, 512], BF16)
            if which == 2:
                tmp = Rot(em, 8, [128, 512], F32)
                h32r = Rot(em, 1, [128, 8, 512], F32)
                exr = Rot(em, 4, [16, 512], F32)
                rwt = em.sb([128, 8, NE], F32); rrw = Res()
                em.dma("sp", rwt[:], self.inp["router_w"][l].rearrange("(kt p) e -> p kt e", p=128), writes=[rrw])
            xTv = self.xT.rearrange("(kt p) t -> p kt t", p=128)
            hTv = self.hT.rearrange("(kt p) t -> p kt t", p=128)
            for (t0, n, cond) in self.chunks():
                tmpk = []
                xt, rx = xin.next()
                em.dma("sp", xt[:, :, :n], xTv[:, :, t0:t0 + n], reads=[self.r_xT], writes=[rx])
                s, rs = sq.next()
                em.op("act", lambda: nc.scalar.activation(out=s[:, :, :n], in_=xt[:, :, :n], func=AF.Square),
                      reads=[rx], writes=[rs])
                pp, rp = pss.next()
                for kt in range(8):
                    em.op("pe", lambda: nc.tensor.matmul(pp[:, :n], lhsT=self.ones[:], rhs=s[:, kt, :n],
                                                         start=(kt == 0), stop=(kt == 7)),
                          reads=[rs, self.rc], writes=[rp])
                rd, rr = rsd.next()
                em.op("act", lambda: nc.scalar.activation(out=rd[:, :n], in_=pp[:, :n], func=AF.Sqrt,
                                                          scale=1.0 / D, bias=self.eps_col(EPS)),
                      reads=[rp, self.rc], writes=[rr])
                em.op("dve", lambda: nc.vector.reciprocal(out=rd[:, :n], in_=rd[:, :n]), reads=[rr], writes=[rr])
                hb, rh = hbf.next()
                for kt in range(8):
                    tm, rt = tmp.next()
                    tmpk.append((tm, rt))
                    em.op("dve", lambda: nc.vector.scalar_tensor_tensor(
                        out=tm[:, :n], in0=xt[:, kt, :n], scalar=self.modc[:, ka, kt, cond:cond + 1],
                        in1=rd[:, :n], op0=ALU.mult, op1=ALU.mult), reads=[rx, rr, self.r_mod], writes=[rt])
                    em.op("act", lambda: nc.scalar.activation(out=hb[:, kt, :n], in_=tm[:, :n], func=AF.Identity,
                                                              bias=self.modc[:, kb, kt, cond:cond + 1], scale=1.0),
                          reads=[rt, self.r_mod], writes=[rh])
                em.dma("act", hTv[:, :, t0:t0 + n], hb[:, :, :n], reads=[rh], writes=[self.r_hT])
                if which == 2:
                    h32, r32 = h32r.next()
                    for kt in range(8):
                        em.op("pool", lambda: nc.gpsimd.tensor_scalar(out=h32[:, kt, :n], in0=tmpk[kt][0][:, :n],
                                                                      scalar1=self.modc[:, kb, kt, cond:cond + 1], scalar2=None,
                                                                      op0=ALU.add), reads=[tmpk[kt][1], self.r_mod], writes=[r32])
                    pl, rpl = pss.next()
                    for kt in range(8):
                        em.op("pe", lambda: nc.tensor.matmul(pl[0:16, :n], lhsT=rwt[:, kt, :], rhs=h32[:, kt, :n],
                                                             start=(kt == 0), stop=(kt == 7)), reads=[rrw, r32], writes=[rpl])
                    ex, rex = exr.next()
                    em.op("act", lambda: nc.scalar.activation(out=ex[:, :n], in_=pl[0:16, :n], func=AF.Exp), reads=[rpl], writes=[rex])
                    pl2, rpl2 = pss.next()
                    em.op("pe", lambda: nc.tensor.matmul(pl2[0:16, :n], lhsT=self.ones[0:16, 0:16], rhs=ex[:, :n], start=True, stop=True),
                          reads=[rex, self.rc], writes=[rpl2])
                    rc_, rrc = exr.next()
                    em.op("dve", lambda: nc.vector.reciprocal(out=rc_[:, :n], in_=pl2[0:16, :n]), reads=[rpl2], writes=[rrc])
                    em.op("dve", lambda: nc.vector.tensor_tensor(out=ex[:, :n], in0=ex[:, :n], in1=rc_[:, :n], op=ALU.mult),
                          reads=[rex, rrc], writes=[rex])
                    em.dma("act", self.affT[:, t0:t0 + n], ex[:, :n], reads=[rex], writes=[self.r_aff])
            em.barrier()
            em.stack = old

    def eps_col(self, v):
        return self.cc[:, self.ccv.index(v):self.ccv.index(v) + 1]

    def phase_inproj(self, l):
        em, nc = self.em, self.nc
        T = self.T
        with contextlib.ExitStack() as st:
            em.stack, old = st, em.stack
            wf = Rot(em, 2, [128, 8, 512], F32)
            wb = Rot(em, 2, [128, 8, 512], BF16)
            hin = Rot(em, 3, [128, 8, 512], BF16)
            pso = Rot(em, 4, [128, 512], F32, psum=True)
            stg = Rot(em, 4, [128, 512], F32)
            stgb = Rot(em, 3, [128, 512], BF16)
            hTv = self.hT.rearrange("(kt p) t -> p kt t", p=128)
            wv = self.inp["w_in"][l].rearrange("(kt p) f -> p kt f", p=128)
            ev = 0
            for c0 in range(0, N_IN, 512):
                nc_ = min(512, N_IN - c0)
                wt, rw = wf.next()
                em.dma("sp", wt[:, :, :nc_], wv[:, :, c0:c0 + nc_], writes=[rw])
                wbt, rwb = wb.next()
                for kt in range(8):
                    e = "pool" if kt % 2 == 0 else "dve"
                    eng = nc.gpsimd if e == "pool" else nc.vector
                    em.op(e, lambda: eng.tensor_copy(out=wbt[:, kt, :nc_], in_=wt[:, kt, :nc_]),
                          reads=[rw], writes=[rwb])
                token_major = (c0 == 1024)
                for (t0, n, cond) in self.chunks():
                    ht, rh = hin.next()
                    em.dma("sp", ht[:, :, :n], hTv[:, :, t0:t0 + n], reads=[self.r_hT], writes=[rh])
                    if not token_major:
                        for m in range(nc_ // 128):
                            pp, rp = pso.next()
                            for kt in range(8):
                                em.op("pe", lambda: nc.tensor.matmul(pp[:, :n], lhsT=wbt[:, kt, m * 128:(m + 1) * 128],
                                                                     rhs=ht[:, kt, :n], start=(kt == 0), stop=(kt == 7)),
                                      reads=[rwb, rh], writes=[rp])
                            sg, rs = stg.next()
                            ev += 1
                            if ev % 2 == 0:
                                em.op("dve", lambda: nc.vector.tensor_copy(out=sg[:, :n], in_=pp[:, :n]),
                                      reads=[rp], writes=[rs])
                            else:
                                em.op("act", lambda: nc.scalar.copy(out=sg[:, :n], in_=pp[:, :n]),
                                      reads=[rp], writes=[rs])
                            em.dma("act", self.projT[c0 + m * 128:c0 + (m + 1) * 128, t0:t0 + n], sg[:, :n],
                                   reads=[rs], writes=[self.r_projT])
                    else:
                        for tt in range(n // 128):
                            pp, rp = pso.next()
                            for kt in range(8):
                                em.op("pe", lambda: nc.tensor.matmul(pp[:, :], lhsT=ht[:, kt, tt * 128:(tt + 1) * 128],
                                                                     rhs=wbt[:, kt, :], start=(kt == 0), stop=(kt == 7)),
                                      reads=[rwb, rh], writes=[rp])
                            sg, rs = stgb.next()
                            em.op("dve", lambda: nc.vector.tensor_copy(out=sg[:], in_=pp[:]), reads=[rp], writes=[rs])
                            em.dma("act", self.vaTM[t0 + tt * 128:t0 + (tt + 1) * 128, :], sg[:],
                                   reads=[rs], writes=[self.r_vaTM])
            em.barrier()
            em.stack = old


    def phase_moe(self, l):
        em, nc = self.em, self.nc
        T, TL = self.T, self.TL
        with contextlib.ExitStack() as st:
            em.stack, old = st, em.stack
            aff = em.sb([16, T], F32); raf = Res()
            wk = em.sb([16, T], F32); rwk = Res()
            m8 = em.sb([16, 8], F32); rm8 = Res()
            th = em.sb([16, 2], F32); rth = Res()
            em.dma("sp", aff[:], self.affT[:, :], reads=[self.r_aff], writes=[raf])
            em.op("dve", lambda: nc.vector.tensor_copy(out=wk[:], in_=aff[:]), reads=[raf], writes=[rwk])
            for (lo, hi, col) in ((0, CTX, 1), (CTX, T, 0)):
                cap = 2 * (hi - lo) // NE
                nit = cap // 8
                for it in range(nit):
                    em.op("dve", lambda: nc.vector.max(out=m8[:], in_=wk[:, lo:hi]), reads=[rwk], writes=[rm8])
                    if it < nit - 1:
                        em.op("dve", lambda: nc.vector.match_replace(out=wk[:, lo:hi], in_to_replace=m8[:], in_values=wk[:, lo:hi],
                                                                     imm_value=-1.0), reads=[rm8, rwk], writes=[rwk])
                em.op("dve", lambda: nc.vector.tensor_copy(out=th[:, col:col + 1], in_=m8[:, 7:8]), reads=[rm8], writes=[rth])
                em.op("dve", lambda: nc.vector.scalar_tensor_tensor(out=wk[:, lo:hi], in0=aff[:, lo:hi], scalar=th[:, col:col + 1],
                                                                    in1=aff[:, lo:hi], op0=ALU.is_ge, op1=ALU.mult),
                      reads=[raf, rth, rwk], writes=[rwk])
            em.dma("act", self.coefT[:, :], wk[:], reads=[rwk], writes=[self.r_coef])
            em.barrier()
            em.stack = old
        with contextlib.ExitStack() as st:
            em.stack, old = st, em.stack
            chs = self.chunks()
            groups = [chs[i:i + 2] for i in range(0, len(chs), 2)]
            sel = em.sb([16, 16, 128], F32); rsel = Res()
            em.dma("sp", sel[:], self.inp["cst_sel"].rearrange("k (e m) -> k e m", e=16), writes=[rsel])
            acc = em.sb([128, 8, 1024], F32); racc = Res()
            hg = em.sb([128, 8, 1024], BF16); rhg = Res()
            cfg = em.sb([16, 1024], F32); rcfg = Res()
            W = [em.sb([128, 8, D], BF16) for _ in range(3)]
            rW = [Res() for _ in range(3)]
            wst = Rot(em, 2, [128, 4, D], F32)
            p1r = Rot(em, 2, [128, 512], F32, psum=True)
            p3r = Rot(em, 2, [128, 512], F32, psum=True)
            por = Rot(em, 2, [128, 512], F32, psum=True)
            pcb = em.ps([128, 512], F32); rpcb = Res()
            cbr = Rot(em, 2, [128, 512], F32)
            sr = Rot(em, 2, [128, 512], F32)
            tr = Rot(em, 2, [128, 512], F32)
            hid = Rot(em, 2, [128, 8, 512], BF16)
            xin = Rot(em, 1, [128, 8, 512], F32)
            hTv = self.hT.rearrange("(kt p) t -> p kt t", p=128)
            xTv = self.xT.rearrange("(kt p) t -> p kt t", p=128)
            wsrc = [self.inp["exp_w1"], self.inp["exp_w3"], self.inp["exp_w2"]]
            cc = 0
            for grp in groups:
                g0 = grp[0][0]
                gn = sum(c[1] for c in grp)
                em.dma("sp", hg[:, :, :gn], hTv[:, :, g0:g0 + gn], reads=[self.r_hT], writes=[rhg])
                em.dma("sp", cfg[:, :gn], self.coefT[:, g0:g0 + gn], reads=[self.r_coef], writes=[rcfg])
                for e in range(NE):
                    for wi in range(3):
                        wv = wsrc[wi][l, e].rearrange("(kt p) f -> p kt f", p=128)
                        for hf in range(2):
                            w, r_w = wst.next()
                            em.dma("sp", w[:], wv[:, hf * 4:(hf + 1) * 4, :], writes=[r_w])
                            cc += 1
                            if cc % 2 == 0:
                                em.op("pool", lambda: nc.gpsimd.tensor_copy(out=W[wi][:, hf * 4:(hf + 1) * 4, :], in_=w[:]),
                                      reads=[r_w], writes=[rW[wi]])
                            else:
                                em.op("dve", lambda: nc.vector.tensor_copy(out=W[wi][:, hf * 4:(hf + 1) * 4, :], in_=w[:]),
                                      reads=[r_w], writes=[rW[wi]])
                    for (t0, n, cond) in grp:
                        o0 = t0 - g0
                        em.op("pe", lambda: nc.tensor.matmul(pcb[:, :n], lhsT=sel[:, e, :], rhs=cfg[:, o0:o0 + n], start=True, stop=True),
                              reads=[rsel, rcfg], writes=[rpcb])
                        cb, rcb = cbr.next()
                        em.op("act", lambda: nc.scalar.copy(out=cb[:, :n], in_=pcb[:, :n]), reads=[rpcb], writes=[rcb])
                        hd, rhd = hid.next()
                        for f in range(8):
                            p1, rp1 = p1r.next(); p3, rp3 = p3r.next()
                            for kt in range(8):
                                em.op("pe", lambda: nc.tensor.matmul(p1[:, :n], lhsT=W[0][:, kt, f * 128:(f + 1) * 128],
                                                                     rhs=hg[:, kt, o0:o0 + n], start=(kt == 0), stop=(kt == 7)),
                                      reads=[rW[0], rhg], writes=[rp1])
                            for kt in range(8):
                                em.op("pe", lambda: nc.tensor.matmul(p3[:, :n], lhsT=W[1][:, kt, f * 128:(f + 1) * 128],
                                                                     rhs=hg[:, kt, o0:o0 + n], start=(kt == 0), stop=(kt == 7)),
                                      reads=[rW[1], rhg], writes=[rp3])
                            s_, rs_ = sr.next()
                            em.op("act", lambda: nc.scalar.activation(out=s_[:, :n], in_=p1[:, :n], func=AF.Silu), reads=[rp1], writes=[rs_])
                            t_, rt_ = tr.next()
                            em.op("dve", lambda: nc.vector.tensor_tensor(out=t_[:, :n], in0=p3[:, :n], in1=s_[:, :n], op=ALU.mult),
                                  reads=[rp3, rs_], writes=[rt_])
                            em.op("pool", lambda: nc.gpsimd.tensor_tensor(out=hd[:, f, :n], in0=t_[:, :n], in1=cb[:, :n], op=ALU.mult),
                                  reads=[rt_, rcb], writes=[rhd])
                        for d in range(8):
                            po, rpo = por.next()
                            for f in range(8):
                                em.op("pe", lambda: nc.tensor.matmul(po[:, :n], lhsT=W[2][:, f, d * 128:(d + 1) * 128], rhs=hd[:, f, :n],
                                                                     start=(f == 0), stop=(f == 7)), reads=[rW[2], rhd], writes=[rpo])
                            if e == 0:
                                em.op("dve", lambda: nc.vector.tensor_copy(out=acc[:, d, o0:o0 + n], in_=po[:, :n]), reads=[rpo], writes=[racc])
                            else:
                                em.op("dve", lambda: nc.vector.tensor_tensor(out=acc[:, d, o0:o0 + n], in0=po[:, :n],
                                                                             in1=acc[:, d, o0:o0 + n], op=ALU.add),
                                      reads=[rpo, racc], writes=[racc])
                for (t0, n, cond) in grp:
                    o0 = t0 - g0
                    xt, r_x = xin.next()
                    em.dma("sp", xt[:, :, :n], xTv[:, :, t0:t0 + n], reads=[self.r_xT], writes=[r_x])
                    for d in range(8):
                        em.op("dve", lambda: nc.vector.scalar_tensor_tensor(out=xt[:, d, :n], in0=acc[:, d, o0:o0 + n],
                                                                            scalar=self.modc[:, 5, d, cond:cond + 1], in1=xt[:, d, :n],
                                                                            op0=ALU.mult, op1=ALU.add),
                              reads=[racc, self.r_mod, r_x], writes=[r_x])
                    em.dma("act", xTv[:, :, t0:t0 + n], xt[:, :, :n], reads=[r_x], writes=[self.r_xT])
            em.barrier()
            em.stack = old

_CACHE = {}


def _get_prog(t_lat, depth, dbg=()):
    key = (t_lat, depth, tuple(dbg))
    if key not in _CACHE:
        lam = [0.8 - 0.6 * math.exp(-0.3 * i) for i in range(depth)]
        _CACHE[key] = K(t_lat, depth, lam, list(dbg))
    return _CACHE[key]


def run(inputs, dbg=()):
    x = np.asarray(inputs["x"], np.float32)
    B, t_lat, _ = x.shape
    depth = np.asarray(inputs["norm1_g"]).shape[0]
    prog = _get_prog(t_lat, depth, dbg)
    consts = host_consts(t_lat)
    in_maps = []
    for core in range(8):
        b = core % B
        m = {}
        for k in prog.inp:
            if k.startswith("cst_"):
                m[k] = consts[k[4:]]
            elif k == "x":
                m[k] = np.ascontiguousarray(x[b])
            elif k == "c":
                m[k] = np.ascontiguousarray(np.asarray(inputs["c"], np.float32)[b])
            elif k == "ctx":
                m[k] = np.ascontiguousarray(np.asarray(inputs["ctx"], np.float32)[b])
            elif k == "rwkv_r_k":
                m[k] = np.ascontiguousarray(np.asarray(inputs[k], np.float32).reshape(depth, 512))
            else:
                m[k] = np.ascontiguousarray(np.asarray(inputs[k], np.float32))
        in_maps.append(m)
    res = run_bass_kernel_spmd(prog.nc, in_maps, core_ids=list(range(8)))
    out = np.stack([np.asarray(res.results[b]["out"], np.float32) for b in range(B)], axis=0)
    dbg_res = {name: [np.asarray(res.results[b][name]) for b in range(B)] for name in dbg}
    return out, dbg_res


def kernel(**inputs):
    out, _ = run(inputs)
    return out
```

```python
import contextlib
import math
import numpy as np
import ml_dtypes
import concourse.bass as bass
import concourse.mybir as mybir
from concourse.bass_utils import run_bass_kernel_spmd

F32 = mybir.dt.float32
BF16 = mybir.dt.bfloat16
AF = mybir.ActivationFunctionType
ALU = mybir.AluOpType

D = 1024
CTX = 256
N_IN = 7936
NE = 16
EPS = 1e-6
GN_EPS = 64e-5
NDQ = 10
PHASES = ["attn", "conv", "rwkv", "merge", "moe"]
RWS = 3


class Res:
    __slots__ = ("w", "r", "name", "excl")

    def __init__(self, name="", excl=False):
        self.w = {}
        self.r = {}
        self.name = name
        self.excl = excl


class Em:
    ENG = ("pe", "act", "dve", "pool", "sp")

    def __init__(self, nc, stack):
        self.nc = nc
        self.stack = stack
        self.eng = {"pe": nc.tensor, "act": nc.scalar, "dve": nc.vector,
                    "pool": nc.gpsimd, "sp": nc.sync}
        self.sem = {}
        self.cnt = {}
        for k in ("pe", "act", "dve", "pool"):
            self.sem[k] = stack.enter_context(nc.semaphore("s_" + k))
            self.cnt[k] = 0
        self.dslot = {}
        for q in ("sp", "act", "pool"):
            self.dslot[q] = 0
            for i in range(NDQ):
                key = "d_%s_%d" % (q, i)
                self.sem[key] = stack.enter_context(nc.semaphore("sd_%s_%d" % (q, i)))
                self.cnt[key] = 0
        self.seen = {e: {} for e in self.ENG}
        self.n_inst = 0
        self.n_wait = 0
        self.uid = 0

    def sb(self, shape, dt, name=None):
        self.uid += 1
        return self.stack.enter_context(
            self.nc.sbuf_tensor(name or ("t%d" % self.uid), list(shape), dt))

    def ps(self, shape, dt=F32, name=None):
        self.uid += 1
        return self.stack.enter_context(
            self.nc.psum_tensor(name or ("p%d" % self.uid), list(shape), dt))

    def _wait(self, e, key, c):
        if c <= 0:
            return
        s = self.seen[e]
        if s.get(key, 0) >= c:
            return
        self.eng[e].wait_ge(self.sem[key], c)
        s[key] = c
        self.n_wait += 1

    def _deps(self, e, reads, writes, own_key):
        skip_raw = own_key if own_key == "pe" else None
        for r in reads:
            for k, c in r.w.items():
                if k == skip_raw:
                    continue
                self._wait(e, k, c)
        for w in writes:
            for k, c in w.w.items():
                if k == skip_raw:
                    continue
                self._wait(e, k, c)
            for k, c in w.r.items():
                if k == own_key:
                    continue
                self._wait(e, k, c)

    def _commit(self, key, c, reads, writes):
        for r in reads:
            if r.r.get(key, 0) < c:
                r.r[key] = c
        for w in writes:
            w.w = {key: c}
            w.r = {}

    def op(self, e, fn, reads=(), writes=()):
        if any(r.excl for r in reads):
            writes = list(writes) + [r for r in reads if r.excl]
            reads = [r for r in reads if not r.excl]
        self._deps(e, reads, writes, e)
        ins = fn()
        self.cnt[e] += 1
        c = self.cnt[e]
        ins.then_inc(self.sem[e], 1)
        self._commit(e, c, reads, writes)
        self.n_inst += 1
        return ins

    def dma(self, q, out, in_, reads=(), writes=(), **kw):
        slot = self.dslot[q]
        self.dslot[q] = (slot + 1) % NDQ
        key = "d_%s_%d" % (q, slot)
        self._wait(q, key, self.cnt[key])
        self._deps(q, reads, writes, None)
        ins = self.eng[q].dma_start(out=out, in_=in_, **kw)
        self.cnt[key] += 16
        c = self.cnt[key]
        ins.then_inc(self.sem[key], 16)
        self._commit(key, c, reads, writes)
        self.n_inst += 1
        return ins

    def barrier(self):
        for e in self.ENG:
            for k, c in self.cnt.items():
                if k == e:
                    continue
                self._wait(e, k, c)

    def finish(self):
        for k, c in self.cnt.items():
            self._wait("sp", k, c)


class Rot:
    def __init__(self, em, n, shape, dt, psum=False):
        self.items = []
        for _ in range(n):
            t = em.ps(shape, dt) if psum else em.sb(shape, dt)
            self.items.append((t, Res()))
        self.i = 0

    def next(self):
        it = self.items[self.i]
        self.i = (self.i + 1) % len(self.items)
        return it


def host_consts(t_lat):
    c = {}
    c["ident"] = np.eye(128, dtype=np.float32)
    c["ones"] = np.ones((128, 128), np.float32)
    bd = np.zeros((128, 128), np.float32)
    bd[:64, :64] = 1.0
    bd[64:, 64:] = 1.0
    c["bd64"] = bd
    rm = np.zeros((128, 128), np.float32)
    for p in range(128):
        d = p % 64
        if d < 32:
            rm[p + 32, p] = -1.0
        else:
            rm[p - 32, p] = 1.0
    c["rotm"] = rm
    sel = np.zeros((16, 16, 128), np.float32)
    for e in range(16):
        sel[e, e, :] = 1.0
    c["sel"] = sel.reshape(16, 2048)
    ii = np.arange(128)
    ms_f = (ii[:, None] < ii[None, :]).astype(np.float32)
    mi_f = (ii[:, None] <= ii[None, :]).astype(np.float32)
    c["mskf"] = np.concatenate([ms_f, mi_f, ms_f, mi_f], axis=1)
    c["mskb"] = np.concatenate([ms_f.T, mi_f.T, ms_f.T, mi_f.T], axis=1)
    rst = np.ones((64, 512), np.float32)
    rst[:, ::128] = 0.0
    c["rst"] = rst
    half = 32
    inv = np.power(10000.0, -np.arange(0, half, 2, dtype=np.float32) / half).astype(np.float32)
    rows = t_lat // 64
    r = np.repeat(np.arange(rows, dtype=np.float32), 64)
    col = np.tile(np.arange(64, dtype=np.float32), rows)
    ang = np.concatenate([r[:, None] * inv, col[:, None] * inv], axis=-1).astype(np.float32)
    cos = np.cos(ang).astype(np.float32).T
    sin = np.sin(ang).astype(np.float32).T
    c["cosT"] = np.ascontiguousarray(np.tile(cos, (4, 1)))
    c["sinT"] = np.ascontiguousarray(np.tile(sin, (4, 1)))
    return c


class K:
    def __init__(self, t_lat, depth, lam_inits, dbg=None):
        self.TL = t_lat
        self.T = CTX + t_lat
        self.depth = depth
        self.lam_inits = lam_inits
        self.dbg = dbg or []
        T = self.T
        nc = self.nc = bass.Bass("TRN2", target_bir_lowering=False)
        self.inp = {}

        def din(name, shape, dt=F32):
            self.inp[name] = nc.dram_tensor(name, list(shape), dt, kind="ExternalInput").ap()
            return self.inp[name]

        L = depth
        din("x", [t_lat, D]); din("c", [D]); din("ctx", [CTX, D]); din("c_ctx", [D])
        din("norm1_g", [L, D]); din("norm2_g", [L, D]); din("ada_w", [L, D, 6 * D]); din("ada_b", [L, 6 * D])
        din("w_in", [L, D, N_IN]); din("q_norm_g", [L, 64]); din("k_norm_g", [L, 64])
        din("diff_lambda", [L, 4, 64]); din("diff_subln_g", [L, 128])
        din("rwkv_conv_w", [L, 3, 1536]); din("rwkv_w0", [L, 2, 512]); din("rwkv_w_up", [L, 2, 64, 512])
        din("rwkv_a0", [L, 2, 512]); din("rwkv_a_up", [L, 2, 64, 512]); din("rwkv_g_up", [L, 128, 512])
        din("rwkv_k_k", [L, 512]); din("rwkv_k_a", [L, 512]); din("rwkv_r_k", [L, 512])
        din("rwkv_ln_g", [L, 512]); din("rwkv_ln_b", [L, 512]); din("conv_w", [L, 3, 512])
        din("w_branch", [L, 3, 512, D]); din("w_out", [L, D, D]); din("router_w", [L, D, NE])
        if "moe" in PHASES:
            din("exp_w1", [L, NE, D, D]); din("exp_w3", [L, NE, D, D]); din("exp_w2", [L, NE, D, D])
        for k, v in host_consts(t_lat).items():
            din("cst_" + k, v.shape)
        self.out = nc.dram_tensor("out", [t_lat, D], F32, kind="ExternalOutput").ap()
        self.dbg_out = {}

        def scratch(name, shape, dt=F32):
            kind = "ExternalOutput" if name in self.dbg else "Internal"
            ap = nc.dram_tensor(name, list(shape), dt, kind=kind).ap()
            if name in self.dbg:
                self.dbg_out[name] = ap
            return ap

        self.xT = scratch("xT", [D, T]); self.r_xT = Res("xT")
        self.hT = scratch("hT", [D, T], BF16); self.r_hT = Res("hT")
        self.projT = scratch("projT", [N_IN, T]); self.r_projT = Res("projT")
        self.vaTM = scratch("vaTM", [T, 512], BF16); self.r_vaTM = Res("vaTM")
        self.yT = scratch("yT", [3 * 512, T], BF16); self.r_yT = Res("yT")
        self.affT = scratch("affT", [NE, T]); self.r_aff = Res("aff")
        self.coefT = scratch("coefT", [NE, T]); self.r_coef = Res("coef")

        with contextlib.ExitStack() as st:
            em = self.em = Em(nc, st)
            st.enter_context(nc.Block())
            self.consts(st)
            self.phase_transpose_in()
            for l in range(depth):
                self.layer(l)
            self.phase_transpose_out()
            em.barrier()
            em.finish()

    def chunks(self):
        res = [(0, CTX, 1)]
        t = CTX
        while t < self.T:
            n = min(512, self.T - t)
            res.append((t, n, 0))
            t += n
        return res

    def consts(self, st):
        em, nc = self.em, self.nc
        self.rc = Res("consts")
        self.ident = em.sb([128, 128], F32)
        self.ones = em.sb([128, 128], F32)
        self.bd64 = em.sb([128, 128], F32)
        self.rotm = em.sb([128, 128], F32)
        self.ones_bf = em.sb([128, 128], BF16)
        for t, nm in ((self.ident, "ident"), (self.ones, "ones"), (self.bd64, "bd64"), (self.rotm, "rotm")):
            em.dma("sp", t[:], self.inp["cst_" + nm][:, :], writes=[self.rc])
        em.op("dve", lambda: nc.vector.tensor_copy(out=self.ones_bf[:], in_=self.ones[:]),
              reads=[self.rc], writes=[self.rc])
        self.ccv = [EPS, GN_EPS, 1e-24, 0.0, 1.0]
        self.cc = em.sb([128, len(self.ccv)], F32)
        for i, v in enumerate(self.ccv):
            em.op("pool", lambda: nc.gpsimd.memset(self.cc[:, i:i + 1], float(v)), writes=[self.rc])

    def phase_transpose_in(self):
        em, nc = self.em, self.nc
        with contextlib.ExitStack() as st:
            em.stack, old = st, em.stack
            xin = Rot(em, 2, [128, D], F32)
            pst = Rot(em, 2, [128, 512], F32, psum=True)
            stg = Rot(em, 2, [128, 8, 128], F32)
            xTv = self.xT.rearrange("(kt p) t -> p kt t", p=128)
            for j in range(self.T // 128):
                t0 = j * 128
                src = self.inp["ctx"][t0:t0 + 128, :] if t0 < CTX else self.inp["x"][t0 - CTX:t0 - CTX + 128, :]
                xt, rx = xin.next()
                em.dma("sp", xt[:], src, writes=[rx])
                sg, rs = stg.next()
                for half in range(2):
                    pt, rp = pst.next()
                    for q in range(4):
                        kt = half * 4 + q
                        em.op("pe", lambda: nc.tensor.transpose(out=pt[:, q * 128:(q + 1) * 128],
                                                                in_=xt[:, kt * 128:(kt + 1) * 128],
                                                                identity=self.ident[:]),
                              reads=[rx, self.rc], writes=[rp])
                    e = "dve" if half == 0 else "act"
                    dst = sg[:, half * 4:(half + 1) * 4, :]
                    srcp = pt[:].rearrange("p (q t) -> p q t", q=4)
                    if e == "dve":
                        em.op("dve", lambda: nc.vector.tensor_copy(out=dst, in_=srcp), reads=[rp], writes=[rs])
                    else:
                        em.op("act", lambda: nc.scalar.copy(out=dst, in_=srcp), reads=[rp], writes=[rs])
                em.dma("act", xTv[:, :, t0:t0 + 128], sg[:], reads=[rs], writes=[self.r_xT])
            em.barrier()
            em.stack = old

    def phase_transpose_out(self):
        em, nc = self.em, self.nc
        with contextlib.ExitStack() as st:
            em.stack, old = st, em.stack
            xin = Rot(em, 2, [128, 8, 128], F32)
            pst = Rot(em, 2, [128, 512], F32, psum=True)
            stg = Rot(em, 2, [128, D], F32)
            xTv = self.xT.rearrange("(kt p) t -> p kt t", p=128)
            self.r_out = Res("out")
            for j in range(CTX // 128, self.T // 128):
                t0 = j * 128
                xt, rx = xin.next()
                em.dma("sp", xt[:], xTv[:, :, t0:t0 + 128], reads=[self.r_xT], writes=[rx])
                sg, rs = stg.next()
                for half in range(2):
                    pt, rp = pst.next()
                    for q in range(4):
                        kt = half * 4 + q
                        em.op("pe", lambda: nc.tensor.transpose(out=pt[:, q * 128:(q + 1) * 128],
                                                                in_=xt[:, kt, :], identity=self.ident[:]),
                              reads=[rx, self.rc], writes=[rp])
                    dst = sg[:, half * 512:(half + 1) * 512]
                    if half == 0:
                        em.op("dve", lambda: nc.vector.tensor_copy(out=dst, in_=pt[:]), reads=[rp], writes=[rs])
                    else:
                        em.op("act", lambda: nc.scalar.copy(out=dst, in_=pt[:]), reads=[rp], writes=[rs])
                em.dma("act", self.out[t0 - CTX:t0 - CTX + 128, :], sg[:], reads=[rs], writes=[self.r_out])
            em.barrier()
            em.stack = old

    def layer(self, l):
        self.phase_ada(l)
        self.phase_norm(l, which=1)
        self.phase_inproj(l)
        if "attn" in PHASES:
            self.phase_attn(l)
        if "conv" in PHASES:
            self.phase_conv(l)
        if "rwkv" in PHASES:
            self.phase_rwkv(l)
        else:
            em, nc = self.em, self.nc
            with contextlib.ExitStack() as st:
                em.stack, old = st, em.stack
                z = em.sb([128, 512], BF16); rz = Res()
                em.op("pool", lambda: nc.gpsimd.memset(z[:], 0.0), writes=[rz])
                for j in range(4):
                    for (t0, n, cond) in self.chunks():
                        em.dma("act", self.yT[512 + j * 128:512 + (j + 1) * 128, t0:t0 + n], z[:, :n], reads=[rz], writes=[self.r_yT])
                em.barrier()
                em.stack = old
        if "merge" in PHASES:
            self.phase_merge(l)
        if "moe" in PHASES:
            self.phase_norm(l, which=2)
            self.phase_moe(l)

    def phase_attn(self, l):
        em, nc = self.em, self.nc
        T = self.T
        NT = T // 128
        with contextlib.ExitStack() as st:
            em.stack, old = st, em.stack
            rs_ = Res("attn_setup")
            qg = em.sb([128, 1], F32); kg = em.sb([128, 1], F32); sgc = em.sb([128, 1], F32)
            for hh in range(2):
                em.dma("sp", qg[hh * 64:(hh + 1) * 64, :], self.inp["q_norm_g"][l].rearrange("(p o) -> p o", o=1), writes=[rs_])
                em.dma("sp", kg[hh * 64:(hh + 1) * 64, :], self.inp["k_norm_g"][l].rearrange("(p o) -> p o", o=1), writes=[rs_])
            em.dma("sp", sgc[:], self.inp["diff_subln_g"][l].rearrange("(p o) -> p o", o=1), writes=[rs_])
            lam_init = self.lam_inits[l]
            em.op("dve", lambda: nc.vector.tensor_scalar(out=sgc[:], in0=sgc[:], scalar1=float(1.0 - lam_init),
                                                         scalar2=None, op0=ALU.mult), reads=[rs_], writes=[rs_])
            lv = em.sb([64, 4], F32)
            em.dma("sp", lv[:], self.inp["diff_lambda"][l].rearrange("f d -> d f"), writes=[rs_],
                   allow_slow_non_contiguous=True)
            pr = em.sb([64, 2], F32)
            lvv = lv[:].rearrange("p (a b) -> p a b", b=2)
            em.op("dve", lambda: nc.vector.tensor_tensor(out=pr[:], in0=lvv[:, :, 0], in1=lvv[:, :, 1], op=ALU.mult),
                  reads=[rs_], writes=[rs_])
            pmisc = em.ps([128, 512], F32); rpm = Res()
            em.op("pe", lambda: nc.tensor.matmul(pmisc[:, 0:2], lhsT=self.ones[0:64, :], rhs=pr[:], start=True, stop=True),
                  reads=[rs_, self.rc], writes=[rpm])
            el = em.sb([128, 2], F32)
            em.op("act", lambda: nc.scalar.activation(out=el[:], in_=pmisc[:, 0:2], func=AF.Exp), reads=[rpm], writes=[rs_])
            neglam = em.sb([128, 1], F32)
            em.op("dve", lambda: nc.vector.scalar_tensor_tensor(out=neglam[:], in0=el[:, 1:2], scalar=float(-lam_init),
                                                                in1=el[:, 0:1], op0=ALU.add, op1=ALU.subtract),
                  reads=[rs_], writes=[rs_])

            KT = em.sb([128, T], BF16); rK = Res()
            QT = em.sb([128, T], BF16); rQ = Res()
            Vh = em.sb([128, NT, 128], BF16); rV = Res()
            src = Rot(em, 2, [128, 512], F32)
            sqr = Rot(em, 2, [128, 512], F32)
            rsd = Rot(em, 2, [128, 512], F32)
            knr = Rot(em, 2, [128, 512], F32)
            cosr = Rot(em, 2, [128, 512], F32)
            sinr = Rot(em, 2, [128, 512], F32)
            t1r = Rot(em, 2, [128, 512], F32)
            t2r = Rot(em, 2, [128, 512], F32)
            pS = Rot(em, 3, [128, 512], F32, psum=True)
            pO = Rot(em, 2, [128, 512], F32, psum=True)
            pD = Rot(em, 2, [128, 512], F32, psum=True)
            Pr = Rot(em, 3, [128, 512], BF16)
            omr = Rot(em, 2, [128, 512], F32)
            accr = Rot(em, 2, [128, 512], F32)
            yst = Rot(em, 2, [128, 512], BF16)

            def qk_prep(row0, gcol, dst, rdst):
                for (t0, n, cond) in self.chunks():
                    s, r_s = src.next()
                    em.dma("sp", s[:, :n], self.projT[row0:row0 + 128, t0:t0 + n], reads=[self.r_projT], writes=[r_s])
                    q2, r_q2 = sqr.next()
                    em.op("act", lambda: nc.scalar.activation(out=q2[:, :n], in_=s[:, :n], func=AF.Square),
                          reads=[r_s], writes=[r_q2])
                    em.op("pe", lambda: nc.tensor.matmul(pmisc[:, :n], lhsT=self.bd64[:], rhs=q2[:, :n], start=True, stop=True),
                          reads=[r_q2, self.rc], writes=[rpm])
                    rd, r_rd = rsd.next()
                    em.op("act", lambda: nc.scalar.activation(out=rd[:, :n], in_=pmisc[:, :n], func=AF.Sqrt,
                                                              scale=1.0 / 64, bias=self.eps_col(EPS)),
                          reads=[rpm, self.rc], writes=[r_rd])
                    em.op("dve", lambda: nc.vector.reciprocal(out=rd[:, :n], in_=rd[:, :n]), reads=[r_rd], writes=[r_rd])
                    kn, r_kn = knr.next()
                    em.op("dve", lambda: nc.vector.scalar_tensor_tensor(out=kn[:, :n], in0=s[:, :n], scalar=gcol[:],
                                                                        in1=rd[:, :n], op0=ALU.mult, op1=ALU.mult),
                          reads=[r_s, r_rd, rs_], writes=[r_kn])
                    if cond == 1:
                        em.op("act", lambda: nc.scalar.copy(out=dst[:, t0:t0 + n], in_=kn[:, :n]), reads=[r_kn], writes=[rdst])
                    else:
                        em.op("pe", lambda: nc.tensor.matmul(pmisc[:, :n], lhsT=self.rotm[:], rhs=kn[:, :n], start=True, stop=True),
                              reads=[r_kn, self.rc], writes=[rpm])
                        cs_, r_c = cosr.next(); sn_, r_sn = sinr.next()
                        em.dma("sp", cs_[:, :n], self.inp["cst_cosT"][:, t0 - CTX:t0 - CTX + n], writes=[r_c])
                        em.dma("sp", sn_[:, :n], self.inp["cst_sinT"][:, t0 - CTX:t0 - CTX + n], writes=[r_sn])
                        t1, r_t1 = t1r.next(); t2, r_t2 = t2r.next()
                        em.op("pool", lambda: nc.gpsimd.tensor_tensor(out=t1[:, :n], in0=kn[:, :n], in1=cs_[:, :n], op=ALU.mult),
                              reads=[r_kn, r_c], writes=[r_t1])
                        em.op("dve", lambda: nc.vector.tensor_tensor(out=t2[:, :n], in0=pmisc[:, :n], in1=sn_[:, :n], op=ALU.mult),
                              reads=[rpm, r_sn], writes=[r_t2])
                        em.op("pool", lambda: nc.gpsimd.tensor_tensor(out=dst[:, t0:t0 + n], in0=t1[:, :n], in1=t2[:, :n], op=ALU.add),
                              reads=[r_t1, r_t2], writes=[rdst])

            for h in range(4):
                qk_prep(512 + h * 128, kg, KT, rK)
                qk_prep(h * 128, qg, QT, rQ)
                em.dma("sp", Vh[:], self.vaTM[:, h * 128:(h + 1) * 128].rearrange("(j p) e -> p j e", p=128),
                       reads=[self.r_vaTM], writes=[rV])
                for (t0, n, cond) in self.chunks():
                    kts = list(range(0, CTX // 128)) if cond == 1 else list(range(0, NT))
                    nk = len(kts)
                    oms = []
                    for m in range(2):
                        po, r_po = pO.next(); pd, r_pd = pD.next()
                        Ps = {}
                        for i in range(nk + 1):
                            if i < nk:
                                kt = kts[i]
                                ps_, r_ps = pS.next()
                                em.op("pe", lambda: nc.tensor.matmul(ps_[:, :n], lhsT=KT[m * 64:(m + 1) * 64, kt * 128:(kt + 1) * 128],
                                                                     rhs=QT[m * 64:(m + 1) * 64, t0:t0 + n], start=True, stop=True),
                                      reads=[rK, rQ], writes=[r_ps])
                                P, r_P = Pr.next()
                                em.op("act", lambda: nc.scalar.activation(out=P[:, :n], in_=ps_[:, :n], func=AF.Exp, scale=0.125),
                                      reads=[r_ps], writes=[r_P])
                                Ps[i] = (P, r_P)
                            if i >= 1:
                                j = i - 1
                                ktj = kts[j]
                                Pj, r_Pj = Ps.pop(j)
                                em.op("pe", lambda: nc.tensor.matmul(po[:, :n], lhsT=Vh[:, ktj, :], rhs=Pj[:, :n],
                                                                     start=(j == 0), stop=(j == nk - 1)),
                                      reads=[rV, r_Pj], writes=[r_po])
                                em.op("pe", lambda: nc.tensor.matmul(pd[:, :n], lhsT=self.ones_bf[:], rhs=Pj[:, :n],
                                                                     start=(j == 0), stop=(j == nk - 1)),
                                      reads=[self.rc, r_Pj], writes=[r_pd])
                        rd, r_rd = rsd.next()
                        em.op("dve", lambda: nc.vector.reciprocal(out=rd[:, :n], in_=pd[:, :n]), reads=[r_pd], writes=[r_rd])
                        om, r_om = omr.next()
                        em.op("dve", lambda: nc.vector.tensor_tensor(out=om[:, :n], in0=po[:, :n], in1=rd[:, :n], op=ALU.mult),
                              reads=[r_po, r_rd], writes=[r_om])
                        oms.append((om, r_om))
                    (o0, r0), (o1, r1) = oms
                    em.op("dve", lambda: nc.vector.scalar_tensor_tensor(out=o0[:, :n], in0=o1[:, :n], scalar=neglam[:],
                                                                        in1=o0[:, :n], op0=ALU.mult, op1=ALU.add),
                          reads=[r1, r0, rs_], writes=[r0])
                    q2, r_q2 = sqr.next()
                    em.op("act", lambda: nc.scalar.activation(out=q2[:, :n], in_=o0[:, :n], func=AF.Square),
                          reads=[r0], writes=[r_q2])
                    em.op("pe", lambda: nc.tensor.matmul(pmisc[:, :n], lhsT=self.ones[:], rhs=q2[:, :n], start=True, stop=True),
                          reads=[r_q2, self.rc], writes=[rpm])
                    rd, r_rd = rsd.next()
                    em.op("act", lambda: nc.scalar.activation(out=rd[:, :n], in_=pmisc[:, :n], func=AF.Sqrt,
                                                              scale=1.0 / 128, bias=self.eps_col(EPS)),
                          reads=[rpm, self.rc], writes=[r_rd])
                    em.op("dve", lambda: nc.vector.reciprocal(out=rd[:, :n], in_=rd[:, :n]), reads=[r_rd], writes=[r_rd])
                    ys, r_ys = yst.next()
                    em.op("dve", lambda: nc.vector.scalar_tensor_tensor(out=ys[:, :n], in0=o0[:, :n], scalar=sgc[:],
                                                                        in1=rd[:, :n], op0=ALU.mult, op1=ALU.mult),
                          reads=[r0, r_rd, rs_], writes=[r_ys])
                    em.dma("act", self.yT[h * 128:(h + 1) * 128, t0:t0 + n], ys[:, :n], reads=[r_ys], writes=[self.r_yT])
            em.barrier()
            em.stack = old

    def seq_bounds(self, cond):
        return (0, CTX) if cond == 1 else (CTX, self.T)

    def phase_conv(self, l):
        em, nc = self.em, self.nc
        with contextlib.ExitStack() as st:
            em.stack, old = st, em.stack
            cw = em.sb([128, 4, 3], F32); rcw = Res()
            for k in range(3):
                em.dma("sp", cw[:, :, k], self.inp["conv_w"][l, k].rearrange("(j p) -> p j", p=128), writes=[rcw],
                       allow_slow_non_contiguous=True)
            cgr = Rot(em, 2, [128, 514], F32); xvr = Rot(em, 2, [128, 514], F32); bgr = Rot(em, 2, [128, 512], F32)
            ur = Rot(em, 2, [128, 514], F32); ar = Rot(em, 2, [128, 512], F32); yst = Rot(em, 2, [128, 512], BF16)
            for j in range(4):
                for (t0, n, cond) in self.chunks():
                    lo, hi = self.seq_bounds(cond)
                    a0 = max(lo, t0 - 1); a1 = min(hi, t0 + n + 1)
                    off = a0 - (t0 - 1)
                    cg, r_cg = cgr.next(); xv, r_xv = xvr.next(); bg, r_bg = bgr.next()
                    em.op("pool", lambda: nc.gpsimd.memset(cg[:], 0.0), writes=[r_cg])
                    em.dma("sp", cg[:, off:off + (a1 - a0)], self.projT[3840 + j * 128:3840 + (j + 1) * 128, a0:a1],
                           reads=[self.r_projT], writes=[r_cg])
                    em.dma("sp", xv[:, off:off + (a1 - a0)], self.projT[4352 + j * 128:4352 + (j + 1) * 128, a0:a1],
                           reads=[self.r_projT], writes=[r_xv])
                    em.dma("sp", bg[:, :n], self.projT[3328 + j * 128:3328 + (j + 1) * 128, t0:t0 + n],
                           reads=[self.r_projT], writes=[r_bg])
                    u, r_u = ur.next()
                    em.op("pool", lambda: nc.gpsimd.tensor_tensor(out=u[:, off:off + (a1 - a0)], in0=cg[:, off:off + (a1 - a0)],
                                                                  in1=xv[:, off:off + (a1 - a0)], op=ALU.mult),
                          reads=[r_cg, r_xv], writes=[r_u])
                    if off > 0:
                        em.op("pool", lambda: nc.gpsimd.memset(u[:, 0:1], 0.0), writes=[r_u])
                    if off + (a1 - a0) < n + 2:
                        em.op("pool", lambda: nc.gpsimd.memset(u[:, n + 1:n + 2], 0.0), writes=[r_u])
                    a, r_a = ar.next()
                    em.op("dve", lambda: nc.vector.tensor_scalar(out=a[:, :n], in0=u[:, 0:n], scalar1=cw[:, j, 0:1], scalar2=None,
                                                                 op0=ALU.mult), reads=[r_u, rcw], writes=[r_a])
                    em.op("dve", lambda: nc.vector.scalar_tensor_tensor(out=a[:, :n], in0=u[:, 1:n + 1], scalar=cw[:, j, 1:2],
                                                                        in1=a[:, :n], op0=ALU.mult, op1=ALU.add),
                          reads=[r_u, rcw, r_a], writes=[r_a])
                    em.op("dve", lambda: nc.vector.scalar_tensor_tensor(out=a[:, :n], in0=u[:, 2:n + 2], scalar=cw[:, j, 2:3],
                                                                        in1=a[:, :n], op0=ALU.mult, op1=ALU.add),
                          reads=[r_u, rcw, r_a], writes=[r_a])
                    ys, r_ys = yst.next()
                    em.op("pool", lambda: nc.gpsimd.tensor_tensor(out=ys[:, :n], in0=a[:, :n], in1=bg[:, :n], op=ALU.mult),
                          reads=[r_a, r_bg], writes=[r_ys])
                    em.dma("act", self.yT[1024 + j * 128:1024 + (j + 1) * 128, t0:t0 + n], ys[:, :n],
                           reads=[r_ys], writes=[self.r_yT])
            em.barrier()
            em.stack = old

    def phase_rwkv(self, l):
        em, nc = self.em, self.nc
        T = self.T
        NCH = T // 128
        I64 = self.ident[0:64, 0:64]
        O64 = self.ones[0:64, 0:64]
        with contextlib.ExitStack() as st:
            em.stack, old = st, em.stack
            rs_ = Res("rwkv_setup")
            msk = [em.sb([128, 512], F32), em.sb([128, 512], F32)]
            em.dma("sp", msk[0][:], self.inp["cst_mskf"][:, :], writes=[rs_])
            em.dma("sp", msk[1][:], self.inp["cst_mskb"][:, :], writes=[rs_])
            rst = em.sb([64, 512], F32)
            em.dma("sp", rst[:], self.inp["cst_rst"][:, :], writes=[rs_])
            wup = em.sb([64, 2, 512], F32); aup = em.sb([64, 2, 512], F32); gup = em.sb([128, 512], F32)
            for d in range(2):
                em.dma("sp", wup[:, d, :], self.inp["rwkv_w_up"][l, d], writes=[rs_])
                em.dma("sp", aup[:, d, :], self.inp["rwkv_a_up"][l, d], writes=[rs_])
            em.dma("sp", gup[:], self.inp["rwkv_g_up"][l], writes=[rs_])
            pc = em.sb([64, 16], F32); rpc = Res()
            ysum = em.sb([64, T], F32); rys = Res()
            QYb = Rot(em, 2, [64, 2, 512], F32)
            GHb = Rot(em, 2, [64, 2, 4, 64], F32)
            pdc = em.sb([64, 2], F32); r_pdc = Res()
            bk = [em.ps([128, 512], F32) for _ in range(8)]
            RB = [Res("psum_bank%d" % i, excl=True) for i in range(8)]
            bA = [bk[0], bk[1]]; RA = [RB[0], RB[1]]
            bB = [bk[2], bk[3]]; RBl = [RB[2], RB[3]]
            bC = [bk[4], bk[5]]; RC = [RB[4], RB[5]]
            pmm = bk[6][0:64, :]; r_pmm = RB[6]
            prd = bk[6][0:64, :]; r_prd = RB[6]
            py = [bk[7][0:64, 0:128], bk[7][0:64, 128:256]]; pst = [bk[7][0:64, 256:320], bk[7][0:64, 320:384]]
            r_py = [RB[7], RB[7]]; r_pst = [RB[7], RB[7]]
            xh = [Rot(em, 1, [64, 514], F32) for _ in range(3)]
            cv = [Rot(em, 2, [64, 512], F32) for _ in range(3)]
            wlr = Rot(em, 2, [64, 512], F32); alr = Rot(em, 2, [64, 512], F32)
            kkr = Rot(em, 2, [64, 512], F32)
            tmps = {nm: (em.sb([64, 512], F32), Res()) for nm in ('sq', 'nr', 'ld', 'a', 'kd', 'bn', 'L', 'Lb', 'Ei', 'dl')}
            ARr = Rot(em, 2, [64, 4, 2, 128], F32)
            Bfr = Rot(em, 2, [64, 512], F32); Kfr = Rot(em, 2, [64, 512], F32)
            Bgr = Rot(em, 2, [64, 512], F32); Kgr = Rot(em, 2, [64, 512], F32)
            Er = Rot(em, 2, [64, 512], F32)
            dgr = [Rot(em, 1, [64, 64], F32) for _ in range(2)]
            tokr = [Rot(em, 1, [128, 256], F32) for _ in range(2)]
            NBr = [Rot(em, 1, [128, 512], F32) for _ in range(2)]
            NTr = [Rot(em, 3, [128, 128], F32) for _ in range(2)]
            Pr_ = [Rot(em, 3, [128, 256], F32) for _ in range(2)]
            Zr = [Rot(em, 3, [128, 128], F32) for _ in range(2)]
            Str = Rot(em, 2, [64, 64], F32)
            glr = Rot(em, 2, [128, 512], F32)
            ybr = Rot(em, 2, [64, 512], BF16)

            def V(e):
                return nc.vector if e == "dve" else nc.gpsimd

            for h in range(8):
                hs = slice(h * 64, (h + 1) * 64)
                cwv = self.inp["rwkv_conv_w"][l]
                for q in range(3):
                    for k in range(3):
                        em.dma("sp", pc[:, q * 3 + k:q * 3 + k + 1],
                               cwv[k, q * 512 + h * 64:q * 512 + (h + 1) * 64].rearrange("(p o) -> p o", o=1), writes=[rpc])
                for i, nm in ((9, "rwkv_k_k"), (10, "rwkv_k_a"), (12, "rwkv_r_k"), (13, "rwkv_ln_g"), (14, "rwkv_ln_b")):
                    em.dma("sp", pc[:, i:i + 1], self.inp[nm][l, hs].rearrange("(p o) -> p o", o=1), writes=[rpc])
                em.op("dve", lambda: nc.vector.tensor_scalar(out=pc[:, 11:12], in0=pc[:, 10:11], scalar1=-1.0, scalar2=1.0,
                                                             op0=ALU.mult, op1=ALU.add), reads=[rpc], writes=[rpc])
                for d in range(2):
                    em.dma("sp", pdc[:, 0:1], self.inp["rwkv_w0"][l, d, hs].rearrange("(p o) -> p o", o=1), writes=[r_pdc])
                    em.dma("sp", pdc[:, 1:2], self.inp["rwkv_a0"][l, d, hs].rearrange("(p o) -> p o", o=1), writes=[r_pdc])
                    msk_d = msk[d]
                    mskT = msk[1 - d][:, 0:128]
                    chs = self.chunks()
                    blocks = chs if d == 0 else [chs[0]] + chs[:0:-1]
                    S, r_S = Str.next()
                    em.op("pool", lambda: nc.gpsimd.memset(S[:], 0.0), writes=[r_S])
                    for (t0, n, cond) in blocks:
                        nch = n // 128
                        QY, rQY = QYb.next(); GH, rGH = GHb.next()
                        Qs = QY[:, 0, :]; Ys = QY[:, 1, :]; rQs = rYs = rQY
                        Gs = GH[:, 0]; Hs = GH[:, 1]; rGs = rHs = rGH
                        lo, hi = self.seq_bounds(cond)
                        a0_ = max(lo, t0 - 1); a1_ = min(hi, t0 + n + 1)
                        off = a0_ - (t0 - 1); ln_ = a1_ - a0_
                        cvt = []
                        for q in range(3):
                            x_, r_x = xh[q].next()
                            if off > 0 or off + ln_ < n + 2:
                                em.op("pool", lambda: nc.gpsimd.memset(x_[:], 0.0), writes=[r_x])
                            row = 1536 + q * 512 + h * 64
                            em.dma("sp", x_[:, off:off + ln_], self.projT[row:row + 64, a0_:a1_], reads=[self.r_projT], writes=[r_x])
                            c_, r_c = cv[q].next()
                            e = "dve" if q != 1 else "pool"
                            em.op(e, lambda: V(e).tensor_scalar(out=c_[:, :n], in0=x_[:, 0:n], scalar1=pc[:, q * 3:q * 3 + 1], scalar2=None,
                                                                op0=ALU.mult), reads=[r_x, rpc], writes=[r_c])
                            for k in (1, 2):
                                em.op("dve", lambda: nc.vector.scalar_tensor_tensor(out=c_[:, :n], in0=x_[:, k:n + k],
                                                                                    scalar=pc[:, q * 3 + k:q * 3 + k + 1], in1=c_[:, :n],
                                                                                    op0=ALU.mult, op1=ALU.add),
                                      reads=[r_x, rpc, r_c], writes=[r_c])
                            cvt.append((c_, r_c))
                        (r_, r_r), (k_, r_k), (v_, r_v) = cvt
                        wl_, r_wl = wlr.next(); al_, r_al = alr.next()
                        em.dma("sp", wl_[:, :n], self.projT[3072:3136, t0:t0 + n], reads=[self.r_projT], writes=[r_wl])
                        em.dma("sp", al_[:, :n], self.projT[3136:3200, t0:t0 + n], reads=[self.r_projT], writes=[r_al])
                        em.op("act", lambda: nc.scalar.activation(out=wl_[:, :n], in_=wl_[:, :n], func=AF.Tanh), reads=[r_wl], writes=[r_wl])
                        kk, r_kk = kkr.next()
                        em.op("pool", lambda: nc.gpsimd.tensor_scalar(out=kk[:, :n], in0=k_[:, :n], scalar1=pc[:, 9:10], scalar2=None, op0=ALU.mult),
                              reads=[r_k, rpc], writes=[r_kk])
                        sq, r_sq = tmps['sq']
                        em.op("act", lambda: nc.scalar.activation(out=sq[:, :n], in_=kk[:, :n], func=AF.Square), reads=[r_kk], writes=[r_sq])
                        em.op("pe", lambda: nc.tensor.matmul(pmm[:, :n], lhsT=O64, rhs=sq[:, :n], start=True, stop=True),
                              reads=[r_sq, self.rc], writes=[r_pmm])
                        nr, r_nr = tmps['nr']
                        em.op("act", lambda: nc.scalar.activation(out=nr[:, :n], in_=pmm[:, :n], func=AF.Sqrt), reads=[r_pmm], writes=[r_nr])
                        em.op("dve", lambda: nc.vector.tensor_scalar(out=nr[:, :n], in0=nr[:, :n], scalar1=1e-12, scalar2=None, op0=ALU.max),
                              reads=[r_nr], writes=[r_nr])
                        em.op("dve", lambda: nc.vector.reciprocal(out=nr[:, :n], in_=nr[:, :n]), reads=[r_nr], writes=[r_nr])
                        em.op("dve", lambda: nc.vector.tensor_tensor(out=kk[:, :n], in0=kk[:, :n], in1=nr[:, :n], op=ALU.mult),
                              reads=[r_kk, r_nr], writes=[r_kk])
                        em.op("pe", lambda: nc.tensor.matmul(pmm[:, :n], lhsT=wup[:, d, hs], rhs=wl_[:, :n], start=True, stop=True),
                              reads=[rs_, r_wl], writes=[r_pmm])
                        ld, r_ld = tmps['ld']
                        em.op("act", lambda: nc.scalar.activation(out=ld[:, :n], in_=pmm[:, :n], func=AF.Sigmoid, bias=pdc[:, 0:1], scale=1.0),
                              reads=[r_pmm, r_pdc], writes=[r_ld])
                        em.op("dve", lambda: nc.vector.tensor_scalar(out=ld[:, :n], in0=ld[:, :n], scalar1=-0.6065306597126334, scalar2=None,
                                                                     op0=ALU.mult), reads=[r_ld], writes=[r_ld])
                        em.op("pe", lambda: nc.tensor.matmul(pmm[:, :n], lhsT=aup[:, d, hs], rhs=al_[:, :n], start=True, stop=True),
                              reads=[rs_, r_al], writes=[r_pmm])
                        a_, r_a = tmps['a']
                        em.op("act", lambda: nc.scalar.activation(out=a_[:, :n], in_=pmm[:, :n], func=AF.Sigmoid, bias=pdc[:, 1:2], scale=1.0),
                              reads=[r_pmm, r_pdc], writes=[r_a])
                        kd, r_kd = tmps['kd']
                        em.op("dve", lambda: nc.vector.tensor_scalar(out=kd[:, :n], in0=a_[:, :n], scalar1=pc[:, 10:11], scalar2=pc[:, 11:12],
                                                                     op0=ALU.mult, op1=ALU.add), reads=[r_a, rpc], writes=[r_kd])
                        em.op("dve", lambda: nc.vector.tensor_tensor(out=kd[:, :n], in0=kd[:, :n], in1=k_[:, :n], op=ALU.mult),
                              reads=[r_kd, r_k], writes=[r_kd])
                        em.op("pool", lambda: nc.gpsimd.tensor_tensor(out=a_[:, :n], in0=a_[:, :n], in1=kk[:, :n], op=ALU.mult),
                              reads=[r_a, r_kk], writes=[r_a])
                        b_, r_b = a_, r_a
                        bn, r_bn = tmps['bn']
                        em.op("dve", lambda: nc.vector.scalar_tensor_tensor(out=bn[:, :n], in0=r_[:, :n], scalar=pc[:, 12:13], in1=kd[:, :n],
                                                                            op0=ALU.mult, op1=ALU.mult), reads=[r_r, rpc, r_kd], writes=[r_bn])
                        em.op("pe", lambda: nc.tensor.matmul(pmm[:, :n], lhsT=O64, rhs=bn[:, :n], start=True, stop=True),
                              reads=[r_bn, self.rc], writes=[r_pmm])
                        if d == 0:
                            em.op("dve", lambda: nc.vector.tensor_tensor(out=ysum[:, t0:t0 + n], in0=pmm[:, :n], in1=v_[:, :n], op=ALU.mult),
                                  reads=[r_pmm, r_v], writes=[rys])
                        else:
                            em.op("dve", lambda: nc.vector.tensor_tensor(out=bn[:, :n], in0=pmm[:, :n], in1=v_[:, :n], op=ALU.mult),
                                  reads=[r_pmm, r_v], writes=[r_bn])
                            em.op("pool", lambda: nc.gpsimd.tensor_tensor(out=ysum[:, t0:t0 + n], in0=ysum[:, t0:t0 + n], in1=bn[:, :n], op=ALU.add),
                                  reads=[r_bn, rys], writes=[rys])
                        L, r_L = tmps['L']
                        em.op("dve", lambda: nc.vector.tensor_tensor_scan(out=L[:, :n], data0=rst[:, :n], data1=ld[:, :n], initial=0.0,
                                                                          op0=ALU.mult, op1=ALU.add), reads=[rs_, r_ld], writes=[r_L])
                        if d == 1:
                            L3 = L[:, :n].rearrange("p (c t) -> p c t", t=128)
                            tot = L3[:, :, 127:128].to_broadcast([64, nch, 128])
                            Lb, r_Lb = tmps['Lb']
                            Lb3 = Lb[:, :n].rearrange("p (c t) -> p c t", t=128)
                            em.op("dve", lambda: nc.vector.tensor_tensor(out=Lb3, in0=tot, in1=L3, op=ALU.subtract), reads=[r_L], writes=[r_Lb])
                            em.op("dve", lambda: nc.vector.tensor_tensor(out=Lb[:, :n], in0=Lb[:, :n], in1=ld[:, :n], op=ALU.add),
                                  reads=[r_Lb, r_ld], writes=[r_Lb])
                            L, r_L = Lb, r_Lb
                        E, r_E = Er.next()
                        em.op("act", lambda: nc.scalar.activation(out=E[:, :n], in_=L[:, :n], func=AF.Exp), reads=[r_L], writes=[r_E])
                        Ei, r_Ei = tmps['Ei']
                        em.op("act", lambda: nc.scalar.activation(out=Ei[:, :n], in_=L[:, :n], func=AF.Exp, scale=-1.0), reads=[r_L], writes=[r_Ei])
                        em.op("dve", lambda: nc.vector.tensor_tensor(out=ld[:, :n], in0=L[:, :n], in1=ld[:, :n], op=ALU.subtract),
                              reads=[r_L, r_ld], writes=[r_ld])
                        em.op("act", lambda: nc.scalar.activation(out=ld[:, :n], in_=ld[:, :n], func=AF.Exp), reads=[r_ld], writes=[r_ld])
                        AR, r_AR = ARr.next()
                        kk3 = kk[:, :n].rearrange("p (c t) -> p c t", t=128)
                        ep3 = ld[:, :n].rearrange("p (c t) -> p c t", t=128)
                        em.op("dve", lambda: nc.vector.scalar_tensor_tensor(out=AR[:, :nch, 0, :], in0=kk3, scalar=-1.0, in1=ep3,
                                                                            op0=ALU.mult, op1=ALU.mult), reads=[r_kk, r_ld], writes=[r_AR])
                        em.op("pool", lambda: nc.gpsimd.tensor_tensor(out=AR[:, :nch, 1, :], in0=r_[:, :n].rearrange("p (c t) -> p c t", t=128),
                                                                      in1=E[:, :n].rearrange("p (c t) -> p c t", t=128), op=ALU.mult),
                              reads=[r_r, r_E], writes=[r_AR])
                        Bf, r_Bf = Bfr.next(); Kf, r_Kf = Kfr.next()
                        em.op("dve", lambda: nc.vector.tensor_tensor(out=Bf[:, :n], in0=b_[:, :n], in1=Ei[:, :n], op=ALU.mult),
                              reads=[r_b, r_Ei], writes=[r_Bf])
                        em.op("pool", lambda: nc.gpsimd.tensor_tensor(out=Kf[:, :n], in0=kd[:, :n], in1=Ei[:, :n], op=ALU.mult),
                              reads=[r_kd, r_Ei], writes=[r_Kf])
                        gidx = 127 if d == 0 else 0
                        gC = E[:, :n].rearrange("p (c t) -> p c t", t=128)[:, :, gidx:gidx + 1]
                        Bg, r_Bg = Bgr.next(); Kg, r_Kg = Kgr.next()
                        em.op("dve", lambda: nc.vector.tensor_tensor(out=Bg[:, :n].rearrange("p (c t) -> p c t", t=128),
                                                                     in0=Bf[:, :n].rearrange("p (c t) -> p c t", t=128),
                                                                     in1=gC.to_broadcast([64, nch, 128]), op=ALU.mult),
                              reads=[r_Bf, r_E], writes=[r_Bg])
                        em.op("dve", lambda: nc.vector.tensor_tensor(out=Kg[:, :n].rearrange("p (c t) -> p c t", t=128),
                                                                     in0=Kf[:, :n].rearrange("p (c t) -> p c t", t=128),
                                                                     in1=gC.to_broadcast([64, nch, 128]), op=ALU.mult),
                              reads=[r_Kf, r_E], writes=[r_Kg])
                        def chunk_steps(ci, lane):
                            pBK = bA[lane]; r_pBK = RA[lane]
                            pQ = bA[lane][0:64, 0:128]; pYi = bA[lane][0:64, 128:256]
                            pG = bA[lane][0:64, 256:320]; pH = bA[lane][0:64, 320:384]
                            r_pQ = r_pYi = r_pG = r_pH = RA[lane]
                            ptr = bB[lane][:, 0:256]; pNT = bB[lane][:, 256:384]; pX1 = bB[lane][:, 384:448]
                            r_ptr = r_pNT = r_pX1 = RBl[lane]
                            pzl = bC[lane][:, 0:128]; ppl = bC[lane][:, 128:256]; pptl = bC[lane][:, 256:384]
                            g = t0 // 128 + ci
                            cs_ = slice(ci * 128, (ci + 1) * 128)
                            Af = AR[:, ci, 0, :]; Rf = AR[:, ci, 1, :]
                            for qi, (src_, rsrc) in enumerate(((Af, r_AR), (Bg[:, cs_], r_Bg), (Kg[:, cs_], r_Kg), (v_[:, cs_], r_v))):
                                em.op("pe", lambda: nc.tensor.transpose(out=ptr[:, qi * 64:(qi + 1) * 64], in_=src_, identity=I64),
                                      reads=[rsrc, self.rc], writes=[r_ptr])
                            tok, r_tok = tokr[lane].next()
                            em.op("act", lambda: nc.scalar.copy(out=tok[:], in_=ptr), reads=[r_ptr], writes=[r_tok])
                            yield
                            At = tok[:, 0:64]; Bgt = tok[:, 64:128]; Kgt = tok[:, 128:192]; Vt = tok[:, 192:256]
                            ARc = AR[:, ci].rearrange("p a t -> p (a t)")
                            em.op("pe", lambda: nc.tensor.matmul(pBK[:, 0:256], lhsT=Bf[:, cs_], rhs=ARc, start=True, stop=True),
                                  reads=[r_Bf, r_AR], writes=[r_pBK])
                            em.op("pe", lambda: nc.tensor.matmul(pBK[:, 256:512], lhsT=Kf[:, cs_], rhs=ARc, start=True, stop=True),
                                  reads=[r_Kf, r_AR], writes=[r_pBK])
                            NB, r_NB = NBr[lane].next()
                            em.op("dve", lambda: nc.vector.tensor_tensor(out=NB[:], in0=pBK[:], in1=msk_d[:], op=ALU.mult),
                                  reads=[r_pBK, rs_], writes=[r_NB])
                            yield
                            N_ = NB[:, 0:128]; Mb = NB[:, 128:256]; Mk = NB[:, 256:384]; Mr = NB[:, 384:512]
                            em.op("pe", lambda: nc.tensor.matmul(pNT, lhsT=Af, rhs=Bf[:, cs_], start=True, stop=True),
                                  reads=[r_AR, r_Bf], writes=[r_pNT])
                            NT, r_NT = NTr[lane].next()
                            em.op("dve", lambda: nc.vector.tensor_tensor(out=NT[:], in0=pNT, in1=mskT, op=ALU.mult),
                                  reads=[r_pNT, rs_], writes=[r_NT])
                            yield
                            em.op("pe", lambda: nc.tensor.matmul(pX1, lhsT=Mk, rhs=Vt, start=True, stop=True),
                                  reads=[r_NB, r_tok], writes=[r_pX1])
                            Z, r_Z = Zr[lane].next()
                            em.op("act", lambda: nc.scalar.copy(out=Z[:, 0:64], in_=pX1), reads=[r_pX1], writes=[r_Z])
                            em.op("pool", lambda: nc.gpsimd.tensor_copy(out=Z[:, 64:128], in_=At), reads=[r_tok], writes=[r_Z])
                            yield
                            P, r_P = N_, r_NB
                            PT, r_PT = NT[:], r_NT
                            for it in range(7):
                                i2 = it % 2
                                em.op("pe", lambda: nc.tensor.matmul(pzl, lhsT=P, rhs=Z[:], start=True, stop=True),
                                      reads=[r_P, r_Z], writes=[RC[lane]])
                                if it < 6:
                                    em.op("pe", lambda: nc.tensor.matmul(ppl, lhsT=PT, rhs=P, start=True, stop=True),
                                          reads=[r_P, r_PT], writes=[RC[lane]])
                                    em.op("pe", lambda: nc.tensor.matmul(pptl, lhsT=P, rhs=PT, start=True, stop=True),
                                          reads=[r_P, r_PT], writes=[RC[lane]])
                                yield
                                Zn, r_Zn = Zr[lane].next()
                                em.op("dve", lambda: nc.vector.tensor_tensor(out=Zn[:], in0=pzl, in1=Z[:], op=ALU.add),
                                      reads=[RC[lane], r_Z], writes=[r_Zn])
                                Z, r_Z = Zn, r_Zn
                                if it < 6:
                                    PPn, r_PPn = Pr_[lane].next()
                                    if it % 2 == 0:
                                        em.op("act", lambda: nc.scalar.copy(out=PPn[:], in_=bC[lane][:, 128:384]), reads=[RC[lane]], writes=[r_PPn])
                                    else:
                                        em.op("dve", lambda: nc.vector.tensor_copy(out=PPn[:], in_=bC[lane][:, 128:384]), reads=[RC[lane]], writes=[r_PPn])
                                    P, r_P = PPn[:, 0:128], r_PPn
                                    PT, r_PT = PPn[:, 128:256], r_PPn
                            yield
                            Wt = Z[:, 0:64]; Apt = Z[:, 64:128]
                            em.op("pe", lambda: nc.tensor.matmul(pQ, lhsT=Apt, rhs=Mb, start=True, stop=False),
                                  reads=[r_Z, r_NB], writes=[r_pQ])
                            em.op("pe", lambda: nc.tensor.matmul(pQ, lhsT=I64, rhs=Rf, start=False, stop=True),
                                  reads=[r_AR, self.rc], writes=[r_pQ])
                            yield
                            em.op("pe", lambda: nc.tensor.matmul(pYi, lhsT=Wt, rhs=Mb, start=True, stop=False),
                                  reads=[r_Z, r_NB], writes=[r_pYi])
                            em.op("pe", lambda: nc.tensor.matmul(pYi, lhsT=Vt, rhs=Mr, start=False, stop=True),
                                  reads=[r_tok, r_NB], writes=[r_pYi])
                            em.op("dve", lambda: nc.vector.tensor_copy(out=QY[:, :, cs_], in_=bA[lane][0:64, 0:256].rearrange("p (a t) -> p a t", a=2)),
                                  reads=[r_pYi], writes=[rQY])
                            yield
                            dg, r_dg = dgr[lane].next()
                            em.op("pool", lambda: nc.gpsimd.tensor_scalar(out=dg[:], in0=I64, scalar1=gC[:, ci, :], scalar2=None, op0=ALU.mult),
                                  reads=[self.rc, r_E], writes=[r_dg])
                            em.op("pe", lambda: nc.tensor.matmul(pG, lhsT=Apt, rhs=Bgt, start=True, stop=False),
                                  reads=[r_Z, r_tok], writes=[r_pG])
                            em.op("pe", lambda: nc.tensor.matmul(pG, lhsT=I64, rhs=dg[:], start=False, stop=True),
                                  reads=[r_dg, self.rc], writes=[r_pG])
                            yield
                            em.op("pe", lambda: nc.tensor.matmul(pH, lhsT=Kgt, rhs=Vt, start=True, stop=False),
                                  reads=[r_tok], writes=[r_pH])
                            em.op("pe", lambda: nc.tensor.matmul(pH, lhsT=Bgt, rhs=Wt, start=False, stop=True),
                                  reads=[r_tok, r_Z], writes=[r_pH])
                            em.op("act", lambda: nc.scalar.copy(out=GH[:, :, ci, :], in_=bA[lane][0:64, 256:384].rearrange("p (a t) -> p a t", a=2)),
                                  reads=[r_pH], writes=[rGH])
                            yield
                        if RWS >= 2:
                            for c0 in range(0, nch, 2):
                                gens = [chunk_steps(c0 + k, k) for k in range(min(2, nch - c0))]
                                while gens:
                                    for g_ in list(gens):
                                        try:
                                            next(g_)
                                        except StopIteration:
                                            gens.remove(g_)

                        order = list(range(nch)) if d == 0 else list(range(nch - 1, -1, -1))
                        if RWS < 3:
                            continue
                        for ci in order:
                            i2 = ci % 2
                            gs = slice(ci * 128, (ci + 1) * 128)
                            em.op("pe", lambda: nc.tensor.matmul(py[i2], lhsT=S[:], rhs=Qs[:, gs], start=True, stop=True),
                                  reads=[r_S, rQs], writes=[r_py[i2]])
                            em.op("pe", lambda: nc.tensor.matmul(pst[i2], lhsT=Gs[:, ci, :], rhs=S[:], start=True, stop=True),
                                  reads=[r_S, rGs], writes=[r_pst[i2]])
                            Sn, r_Sn = Str.next()
                            em.op("dve", lambda: nc.vector.tensor_tensor(out=Sn[:], in0=pst[i2], in1=Hs[:, ci, :], op=ALU.add),
                                  reads=[r_pst[i2], rHs], writes=[r_Sn])
                            em.op("dve", lambda: nc.vector.tensor_tensor(out=Ys[:, gs], in0=py[i2], in1=Ys[:, gs], op=ALU.add),
                                  reads=[r_py[i2], rYs], writes=[rYs])
                            S, r_S = Sn, r_Sn
                        o_ = Ys[:, :n]
                        em.op("pe", lambda: nc.tensor.matmul(prd[:, :n], lhsT=O64, rhs=o_, start=True, stop=True),
                              reads=[rYs, self.rc], writes=[r_prd])
                        dl, r_dl = tmps['dl']
                        em.op("dve", lambda: nc.vector.scalar_tensor_tensor(out=dl[:, :n], in0=prd[:, :n], scalar=-1.0 / 64, in1=o_,
                                                                            op0=ALU.mult, op1=ALU.add), reads=[r_prd, rYs], writes=[r_dl])
                        sq, r_sq = tmps['sq']
                        em.op("act", lambda: nc.scalar.activation(out=sq[:, :n], in_=dl[:, :n], func=AF.Square), reads=[r_dl], writes=[r_sq])
                        em.op("pe", lambda: nc.tensor.matmul(prd[:, :n], lhsT=O64, rhs=sq[:, :n], start=True, stop=True),
                              reads=[r_sq, self.rc], writes=[r_prd])
                        em.op("act", lambda: nc.scalar.activation(out=sq[:, :n], in_=prd[:, :n], func=AF.Sqrt, scale=1.0 / 64,
                                                                  bias=self.eps_col(GN_EPS)[0:64, :]), reads=[r_prd, self.rc], writes=[r_sq])
                        em.op("dve", lambda: nc.vector.reciprocal(out=sq[:, :n], in_=sq[:, :n]), reads=[r_sq], writes=[r_sq])
                        em.op("dve", lambda: nc.vector.tensor_tensor(out=dl[:, :n], in0=dl[:, :n], in1=sq[:, :n], op=ALU.mult),
                              reads=[r_dl, r_sq], writes=[r_dl])
                        em.op("dve", lambda: nc.vector.tensor_scalar(out=dl[:, :n], in0=dl[:, :n], scalar1=pc[:, 13:14], scalar2=pc[:, 14:15],
                                                                     op0=ALU.mult, op1=ALU.add), reads=[r_dl, rpc], writes=[r_dl])
                        em.op("pool", lambda: nc.gpsimd.tensor_tensor(out=ysum[:, t0:t0 + n], in0=ysum[:, t0:t0 + n], in1=dl[:, :n], op=ALU.add),
                              reads=[r_dl, rys], writes=[rys])
                for (t0, n, cond) in self.chunks():
                    gl, r_gl = glr.next()
                    em.dma("sp", gl[:, :n], self.projT[3200:3328, t0:t0 + n], reads=[self.r_projT], writes=[r_gl])
                    em.op("act", lambda: nc.scalar.activation(out=gl[:, :n], in_=gl[:, :n], func=AF.Sigmoid), reads=[r_gl], writes=[r_gl])
                    em.op("pe", lambda: nc.tensor.matmul(prd[:, :n], lhsT=gup[:, hs], rhs=gl[:, :n], start=True, stop=True),
                          reads=[rs_, r_gl], writes=[r_prd])
                    yb, r_yb = ybr.next()
                    em.op("dve", lambda: nc.vector.tensor_tensor(out=yb[:, :n], in0=prd[:, :n], in1=ysum[:, t0:t0 + n], op=ALU.mult),
                          reads=[r_prd, rys], writes=[r_yb])
                    em.dma("act", self.yT[512 + h * 64:512 + (h + 1) * 64, t0:t0 + n], yb[:, :n], reads=[r_yb], writes=[self.r_yT])
            em.barrier()
            em.stack = old

    def phase_merge(self, l):
        em, nc = self.em, self.nc
        with contextlib.ExitStack() as st:
            em.stack, old = st, em.stack
            wbf = em.sb([128, 12, D], BF16); rwb = Res()
            wof = em.sb([128, 8, D], BF16); rwo = Res()
            wst = Rot(em, 2, [128, 4, D], F32)
            wbv = self.inp["w_branch"][l].rearrange("b (kt p) f -> p b kt f", p=128)
            for br in range(3):
                w, r_w = wst.next()
                em.dma("sp", w[:], wbv[:, br], writes=[r_w])
                em.op("pool", lambda: nc.gpsimd.tensor_copy(out=wbf[:, br * 4:(br + 1) * 4, :], in_=w[:]), reads=[r_w], writes=[rwb])
            wov = self.inp["w_out"][l].rearrange("(kt p) f -> p kt f", p=128)
            for hf in range(2):
                w, r_w = wst.next()
                em.dma("sp", w[:], wov[:, hf * 4:(hf + 1) * 4, :], writes=[r_w])
                em.op("pool", lambda: nc.gpsimd.tensor_copy(out=wof[:, hf * 4:(hf + 1) * 4, :], in_=w[:]), reads=[r_w], writes=[rwo])
            yin = Rot(em, 2, [128, 12, 512], BF16)
            gin = Rot(em, 3, [128, 512], F32)
            sgr = Rot(em, 3, [128, 512], F32)
            tmr = Rot(em, 3, [128, 512], F32)
            macc = Rot(em, 2, [128, 8, 512], F32)
            mbf = Rot(em, 2, [128, 8, 512], BF16)
            xin = Rot(em, 2, [128, 8, 512], F32)
            pm = Rot(em, 4, [128, 512], F32, psum=True)
            yTv = self.yT.rearrange("(kt p) t -> p kt t", p=128)
            xTv = self.xT.rearrange("(kt p) t -> p kt t", p=128)
            for (t0, n, cond) in self.chunks():
                y, r_y = yin.next()
                em.dma("sp", y[:, :, :n], yTv[:, :, t0:t0 + n], reads=[self.r_yT], writes=[r_y])
                xt, r_x = xin.next()
                em.dma("sp", xt[:, :, :n], xTv[:, :, t0:t0 + n], reads=[self.r_xT], writes=[r_x])
                ma, r_ma = macc.next()
                for d in range(8):
                    for br in range(3):
                        g, r_g = gin.next()
                        row = 4864 + br * 1024 + d * 128
                        em.dma("sp", g[:, :n], self.projT[row:row + 128, t0:t0 + n], reads=[self.r_projT], writes=[r_g])
                        sg, r_sg = sgr.next()
                        em.op("act", lambda: nc.scalar.activation(out=sg[:, :n], in_=g[:, :n], func=AF.Sigmoid),
                              reads=[r_g], writes=[r_sg])
                        pp, r_pp = pm.next()
                        for kt in range(4):
                            em.op("pe", lambda: nc.tensor.matmul(pp[:, :n], lhsT=wbf[:, br * 4 + kt, d * 128:(d + 1) * 128],
                                                                 rhs=y[:, br * 4 + kt, :n], start=(kt == 0), stop=(kt == 3)),
                                  reads=[rwb, r_y], writes=[r_pp])
                        if br == 0:
                            em.op("dve", lambda: nc.vector.tensor_tensor(out=ma[:, d, :n], in0=pp[:, :n], in1=sg[:, :n], op=ALU.mult),
                                  reads=[r_pp, r_sg], writes=[r_ma])
                        else:
                            tm, r_tm = tmr.next()
                            em.op("dve", lambda: nc.vector.tensor_tensor(out=tm[:, :n], in0=pp[:, :n], in1=sg[:, :n], op=ALU.mult),
                                  reads=[r_pp, r_sg], writes=[r_tm])
                            em.op("pool", lambda: nc.gpsimd.tensor_tensor(out=ma[:, d, :n], in0=ma[:, d, :n], in1=tm[:, :n], op=ALU.add),
                                  reads=[r_tm, r_ma], writes=[r_ma])
                mb, r_mb = mbf.next()
                em.op("act", lambda: nc.scalar.copy(out=mb[:, :, :n], in_=ma[:, :, :n]), reads=[r_ma], writes=[r_mb])
                for d in range(8):
                    pp, r_pp = pm.next()
                    for kt in range(8):
                        em.op("pe", lambda: nc.tensor.matmul(pp[:, :n], lhsT=wof[:, kt, d * 128:(d + 1) * 128], rhs=mb[:, kt, :n],
                                                             start=(kt == 0), stop=(kt == 7)), reads=[rwo, r_mb], writes=[r_pp])
                    em.op("dve", lambda: nc.vector.scalar_tensor_tensor(out=xt[:, d, :n], in0=pp[:, :n],
                                                                        scalar=self.modc[:, 2, d, cond:cond + 1], in1=xt[:, d, :n],
                                                                        op0=ALU.mult, op1=ALU.add),
                          reads=[r_pp, self.r_mod, r_x], writes=[r_x])
                em.dma("act", xTv[:, :, t0:t0 + n], xt[:, :, :n], reads=[r_x], writes=[self.r_xT])
            em.barrier()
            em.stack = old

    def phase_ada(self, l):
        em, nc = self.em, self.nc
        if l == 0:
            self.modc = em.sb([128, 6, 8, 2], F32)
            self.r_mod = Res("mod")
            self.cs = em.sb([128, 2, 8], F32)
            self.r_cs = Res("cs")
            ctmp = em.sb([128, 2, 8], F32)
            rct = Res()
            em.dma("sp", ctmp[:, 0, :], self.inp["c"].rearrange("(kt p) -> p kt", p=128), writes=[rct],
                   allow_slow_non_contiguous=True)
            em.dma("sp", ctmp[:, 1, :], self.inp["c_ctx"].rearrange("(kt p) -> p kt", p=128), writes=[rct],
                   allow_slow_non_contiguous=True)
            em.op("act", lambda: nc.scalar.activation(out=self.cs[:], in_=ctmp[:], func=AF.Silu),
                  reads=[rct], writes=[self.r_cs])
        with contextlib.ExitStack() as st:
            em.stack, old = st, em.stack
            wbuf = Rot(em, 2, [128, 8, 768], F32)
            pm = em.ps([128, 48, 2], F32); rpm = Res()
            adab = em.sb([128, 48], F32); rab = Res()
            g12 = em.sb([128, 2, 8], F32); rg = Res()
            em.dma("sp", adab[:], self.inp["ada_b"][l].rearrange("(j p) -> p j", p=128), writes=[rab],
                   allow_slow_non_contiguous=True)
            em.dma("sp", g12[:, 0, :], self.inp["norm1_g"][l].rearrange("(j p) -> p j", p=128), writes=[rg],
                   allow_slow_non_contiguous=True)
            em.dma("sp", g12[:, 1, :], self.inp["norm2_g"][l].rearrange("(j p) -> p j", p=128), writes=[rg],
                   allow_slow_non_contiguous=True)
            awv = self.inp["ada_w"][l].rearrange("(kt p) f -> p kt f", p=128)
            for ch in range(8):
                wt, rw = wbuf.next()
                em.dma("sp", wt[:], awv[:, :, ch * 768:(ch + 1) * 768], writes=[rw])
                for jj in range(6):
                    j = ch * 6 + jj
                    for kt in range(8):
                        em.op("pe", lambda: nc.tensor.matmul(pm[:, j, :], lhsT=wt[:, kt, jj * 128:(jj + 1) * 128],
                                                             rhs=self.cs[:, :, kt], start=(kt == 0), stop=(kt == 7)),
                              reads=[rw, self.r_cs], writes=[rpm])
            modraw = em.sb([128, 48, 2], F32); rmr = Res()
            em.op("dve", lambda: nc.vector.tensor_tensor(out=modraw[:], in0=pm[:],
                                                         in1=adab[:].unsqueeze(2).to_broadcast([128, 48, 2]),
                                                         op=ALU.add),
                  reads=[rpm, rab], writes=[rmr])
            mc = self.modc
            mr = modraw[:].rearrange("p (k j) c -> p k j c", k=6)
            for half in range(2):
                sh, sc, gt = mr[:, 3 * half + 0], mr[:, 3 * half + 1], mr[:, 3 * half + 2]
                gn = g12[:, half, :].unsqueeze(2).to_broadcast([128, 8, 2])
                em.op("dve", lambda: nc.vector.scalar_tensor_tensor(out=mc[:, 3 * half + 0], in0=sc, scalar=1.0, in1=gn,
                                                                    op0=ALU.add, op1=ALU.mult),
                      reads=[rmr, rg], writes=[self.r_mod])
                em.op("dve", lambda: nc.vector.tensor_copy(out=mc[:, 3 * half + 1], in_=sh),
                      reads=[rmr], writes=[self.r_mod])
                em.op("dve", lambda: nc.vector.tensor_copy(out=mc[:, 3 * half + 2], in_=gt),
                      reads=[rmr], writes=[self.r_mod])
            em.barrier()
            em.stack = old

    def phase_norm(self, l, which):
        em, nc = self.em, self.nc
        ka, kb = (0, 1) if which == 1 else (3, 4)
        with contextlib.ExitStack() as st:
            em.stack, old = st, em.stack
            xin = Rot(em, 2, [128, 8, 512], F32)
            sq = Rot(em, 2, [128, 8, 512], F32)
            pss = Rot(em, 2, [128, 512], F32, psum=True)
            rsd = Rot(em, 2, [128, 512], F32)
            tmp = Rot(em, 3, [128, 512], F32)
            hbf = Rot(em, 2, [128, 8, 512], BF16)
            if which == 2:
                tmp = Rot(em, 8, [128, 512], F32)
                h32r = Rot(em, 1, [128, 8, 512], F32)
                exr = Rot(em, 4, [16, 512], F32)
                rwt = em.sb([128, 8, NE], F32); rrw = Res()
                em.dma("sp", rwt[:], self.inp["router_w"][l].rearrange("(kt p) e -> p kt e", p=128), writes=[rrw])
            xTv = self.xT.rearrange("(kt p) t -> p kt t", p=128)
            hTv = self.hT.rearrange("(kt p) t -> p kt t", p=128)
            for (t0, n, cond) in self.chunks():
                tmpk = []
                xt, rx = xin.next()
                em.dma("sp", xt[:, :, :n], xTv[:, :, t0:t0 + n], reads=[self.r_xT], writes=[rx])
                s, rs = sq.next()
                em.op("act", lambda: nc.scalar.activation(out=s[:, :, :n], in_=xt[:, :, :n], func=AF.Square),
                      reads=[rx], writes=[rs])
                pp, rp = pss.next()
                for kt in range(8):
                    em.op("pe", lambda: nc.tensor.matmul(pp[:, :n], lhsT=self.ones[:], rhs=s[:, kt, :n],
                                                         start=(kt == 0), stop=(kt == 7)),
                          reads=[rs, self.rc], writes=[rp])
                rd, rr = rsd.next()
                em.op("act", lambda: nc.scalar.activation(out=rd[:, :n], in_=pp[:, :n], func=AF.Sqrt,
                                                          scale=1.0 / D, bias=self.eps_col(EPS)),
                      reads=[rp, self.rc], writes=[rr])
                em.op("dve", lambda: nc.vector.reciprocal(out=rd[:, :n], in_=rd[:, :n]), reads=[rr], writes=[rr])
                hb, rh = hbf.next()
                for kt in range(8):
                    tm, rt = tmp.next()
                    tmpk.append((tm, rt))
                    em.op("dve", lambda: nc.vector.scalar_tensor_tensor(
                        out=tm[:, :n], in0=xt[:, kt, :n], scalar=self.modc[:, ka, kt, cond:cond + 1],
                        in1=rd[:, :n], op0=ALU.mult, op1=ALU.mult), reads=[rx, rr, self.r_mod], writes=[rt])
                    em.op("act", lambda: nc.scalar.activation(out=hb[:, kt, :n], in_=tm[:, :n], func=AF.Identity,
                                                              bias=self.modc[:, kb, kt, cond:cond + 1], scale=1.0),
                          reads=[rt, self.r_mod], writes=[rh])
                em.dma("act", hTv[:, :, t0:t0 + n], hb[:, :, :n], reads=[rh], writes=[self.r_hT])
                if which == 2:
                    h32, r32 = h32r.next()
                    for kt in range(8):
                        em.op("pool", lambda: nc.gpsimd.tensor_scalar(out=h32[:, kt, :n], in0=tmpk[kt][0][:, :n],
                                                                      scalar1=self.modc[:, kb, kt, cond:cond + 1], scalar2=None,
                                                                      op0=ALU.add), reads=[tmpk[kt][1], self.r_mod], writes=[r32])
                    pl, rpl = pss.next()
                    for kt in range(8):
                        em.op("pe", lambda: nc.tensor.matmul(pl[0:16, :n], lhsT=rwt[:, kt, :], rhs=h32[:, kt, :n],
                                                             start=(kt == 0), stop=(kt == 7)), reads=[rrw, r32], writes=[rpl])
                    ex, rex = exr.next()
                    em.op("act", lambda: nc.scalar.activation(out=ex[:, :n], in_=pl[0:16, :n], func=AF.Exp), reads=[rpl], writes=[rex])
                    pl2, rpl2 = pss.next()
                    em.op("pe", lambda: nc.tensor.matmul(pl2[0:16, :n], lhsT=self.ones[0:16, 0:16], rhs=ex[:, :n], start=True, stop=True),
                          reads=[rex, self.rc], writes=[rpl2])
                    rc_, rrc = exr.next()
                    em.op("dve", lambda: nc.vector.reciprocal(out=rc_[:, :n], in_=pl2[0:16, :n]), reads=[rpl2], writes=[rrc])
                    em.op("dve", lambda: nc.vector.tensor_tensor(out=ex[:, :n], in0=ex[:, :n], in1=rc_[:, :n], op=ALU.mult),
                          reads=[rex, rrc], writes=[rex])
                    em.dma("act", self.affT[:, t0:t0 + n], ex[:, :n], reads=[rex], writes=[self.r_aff])
            em.barrier()
            em.stack = old

    def eps_col(self, v):
        return self.cc[:, self.ccv.index(v):self.ccv.index(v) + 1]

    def phase_inproj(self, l):
        em, nc = self.em, self.nc
        T = self.T
        with contextlib.ExitStack() as st:
            em.stack, old = st, em.stack
            wf = Rot(em, 2, [128, 8, 512], F32)
            wb = Rot(em, 2, [128, 8, 512], BF16)
            hin = Rot(em, 3, [128, 8, 512], BF16)
            pso = Rot(em, 4, [128, 512], F32, psum=True)
            stg = Rot(em, 4, [128, 512], F32)
            stgb = Rot(em, 3, [128, 512], BF16)
            hTv = self.hT.rearrange("(kt p) t -> p kt t", p=128)
            wv = self.inp["w_in"][l].rearrange("(kt p) f -> p kt f", p=128)
            ev = 0
            for c0 in range(0, N_IN, 512):
                nc_ = min(512, N_IN - c0)
                wt, rw = wf.next()
                em.dma("sp", wt[:, :, :nc_], wv[:, :, c0:c0 + nc_], writes=[rw])
                wbt, rwb = wb.next()
                for kt in range(8):
                    e = "pool" if kt % 2 == 0 else "dve"
                    eng = nc.gpsimd if e == "pool" else nc.vector
                    em.op(e, lambda: eng.tensor_copy(out=wbt[:, kt, :nc_], in_=wt[:, kt, :nc_]),
                          reads=[rw], writes=[rwb])
                token_major = (c0 == 1024)
                for (t0, n, cond) in self.chunks():
                    ht, rh = hin.next()
                    em.dma("sp", ht[:, :, :n], hTv[:, :, t0:t0 + n], reads=[self.r_hT], writes=[rh])
                    if not token_major:
                        for m in range(nc_ // 128):
                            pp, rp = pso.next()
                            for kt in range(8):
                                em.op("pe", lambda: nc.tensor.matmul(pp[:, :n], lhsT=wbt[:, kt, m * 128:(m + 1) * 128],
                                                                     rhs=ht[:, kt, :n], start=(kt == 0), stop=(kt == 7)),
                                      reads=[rwb, rh], writes=[rp])
                            sg, rs = stg.next()
                            ev += 1
                            if ev % 2 == 0:
                                em.op("dve", lambda: nc.vector.tensor_copy(out=sg[:, :n], in_=pp[:, :n]),
                                      reads=[rp], writes=[rs])
                            else:
                                em.op("act", lambda: nc.scalar.copy(out=sg[:, :n], in_=pp[:, :n]),
                                      reads=[rp], writes=[rs])
                            em.dma("act", self.projT[c0 + m * 128:c0 + (m + 1) * 128, t0:t0 + n], sg[:, :n],
                                   reads=[rs], writes=[self.r_projT])
                    else:
                        for tt in range(n // 128):
                            pp, rp = pso.next()
                            for kt in range(8):
                                em.op("pe", lambda: nc.tensor.matmul(pp[:, :], lhsT=ht[:, kt, tt * 128:(tt + 1) * 128],
                                                                     rhs=wbt[:, kt, :], start=(kt == 0), stop=(kt == 7)),
                                      reads=[rwb, rh], writes=[rp])
                            sg, rs = stgb.next()
                            em.op("dve", lambda: nc.vector.tensor_copy(out=sg[:], in_=pp[:]), reads=[rp], writes=[rs])
                            em.dma("act", self.vaTM[t0 + tt * 128:t0 + (tt + 1) * 128, :], sg[:],
                                   reads=[rs], writes=[self.r_vaTM])
            em.barrier()
            em.stack = old


    def phase_moe(self, l):
        em, nc = self.em, self.nc
        T, TL = self.T, self.TL
        with contextlib.ExitStack() as st:
            em.stack, old = st, em.stack
            aff = em.sb([16, T], F32); raf = Res()
            wk = em.sb([16, T], F32); rwk = Res()
            m8 = em.sb([16, 8], F32); rm8 = Res()
            th = em.sb([16, 2], F32); rth = Res()
            em.dma("sp", aff[:], self.affT[:, :], reads=[self.r_aff], writes=[raf])
            em.op("dve", lambda: nc.vector.tensor_copy(out=wk[:], in_=aff[:]), reads=[raf], writes=[rwk])
            for (lo, hi, col) in ((0, CTX, 1), (CTX, T, 0)):
                cap = 2 * (hi - lo) // NE
                nit = cap // 8
                for it in range(nit):
                    em.op("dve", lambda: nc.vector.max(out=m8[:], in_=wk[:, lo:hi]), reads=[rwk], writes=[rm8])
                    if it < nit - 1:
                        em.op("dve", lambda: nc.vector.match_replace(out=wk[:, lo:hi], in_to_replace=m8[:], in_values=wk[:, lo:hi],
                                                                     imm_value=-1.0), reads=[rm8, rwk], writes=[rwk])
                em.op("dve", lambda: nc.vector.tensor_copy(out=th[:, col:col + 1], in_=m8[:, 7:8]), reads=[rm8], writes=[rth])
                em.op("dve", lambda: nc.vector.scalar_tensor_tensor(out=wk[:, lo:hi], in0=aff[:, lo:hi], scalar=th[:, col:col + 1],
                                                                    in1=aff[:, lo:hi], op0=ALU.is_ge, op1=ALU.mult),
                      reads=[raf, rth, rwk], writes=[rwk])
            em.dma("act", self.coefT[:, :], wk[:], reads=[rwk], writes=[self.r_coef])
            em.barrier()
            em.stack = old
        with contextlib.ExitStack() as st:
            em.stack, old = st, em.stack
            chs = self.chunks()
            groups = [chs[i:i + 2] for i in range(0, len(chs), 2)]
            sel = em.sb([16, 16, 128], F32); rsel = Res()
            em.dma("sp", sel[:], self.inp["cst_sel"].rearrange("k (e m) -> k e m", e=16), writes=[rsel])
            acc = em.sb([128, 8, 1024], F32); racc = Res()
            hg = em.sb([128, 8, 1024], BF16); rhg = Res()
            cfg = em.sb([16, 1024], F32); rcfg = Res()
            W = [em.sb([128, 8, D], BF16) for _ in range(3)]
            rW = [Res() for _ in range(3)]
            wst = Rot(em, 2, [128, 4, D], F32)
            p1r = Rot(em, 2, [128, 512], F32, psum=True)
            p3r = Rot(em, 2, [128, 512], F32, psum=True)
            por = Rot(em, 2, [128, 512], F32, psum=True)
            pcb = em.ps([128, 512], F32); rpcb = Res()
            cbr = Rot(em, 2, [128, 512], F32)
            sr = Rot(em, 2, [128, 512], F32)
            tr = Rot(em, 2, [128, 512], F32)
            hid = Rot(em, 2, [128, 8, 512], BF16)
            xin = Rot(em, 1, [128, 8, 512], F32)
            hTv = self.hT.rearrange("(kt p) t -> p kt t", p=128)
            xTv = self.xT.rearrange("(kt p) t -> p kt t", p=128)
            wsrc = [self.inp["exp_w1"], self.inp["exp_w3"], self.inp["exp_w2"]]
            cc = 0
            for grp in groups:
                g0 = grp[0][0]
                gn = sum(c[1] for c in grp)
                em.dma("sp", hg[:, :, :gn], hTv[:, :, g0:g0 + gn], reads=[self.r_hT], writes=[rhg])
                em.dma("sp", cfg[:, :gn], self.coefT[:, g0:g0 + gn], reads=[self.r_coef], writes=[rcfg])
                for e in range(NE):
                    for wi in range(3):
                        wv = wsrc[wi][l, e].rearrange("(kt p) f -> p kt f", p=128)
                        for hf in range(2):
                            w, r_w = wst.next()
                            em.dma("sp", w[:], wv[:, hf * 4:(hf + 1) * 4, :], writes=[r_w])
                            cc += 1
                            if cc % 2 == 0:
                                em.op("pool", lambda: nc.gpsimd.tensor_copy(out=W[wi][:, hf * 4:(hf + 1) * 4, :], in_=w[:]),
                                      reads=[r_w], writes=[rW[wi]])
                            else:
                                em.op("dve", lambda: nc.vector.tensor_copy(out=W[wi][:, hf * 4:(hf + 1) * 4, :], in_=w[:]),
                                      reads=[r_w], writes=[rW[wi]])
                    for (t0, n, cond) in grp:
                        o0 = t0 - g0
                        em.op("pe", lambda: nc.tensor.matmul(pcb[:, :n], lhsT=sel[:, e, :], rhs=cfg[:, o0:o0 + n], start=True, stop=True),
                              reads=[rsel, rcfg], writes=[rpcb])
                        cb, rcb = cbr.next()
                        em.op("act", lambda: nc.scalar.copy(out=cb[:, :n], in_=pcb[:, :n]), reads=[rpcb], writes=[rcb])
                        hd, rhd = hid.next()
                        for f in range(8):
                            p1, rp1 = p1r.next(); p3, rp3 = p3r.next()
                            for kt in range(8):
                                em.op("pe", lambda: nc.tensor.matmul(p1[:, :n], lhsT=W[0][:, kt, f * 128:(f + 1) * 128],
                                                                     rhs=hg[:, kt, o0:o0 + n], start=(kt == 0), stop=(kt == 7)),
                                      reads=[rW[0], rhg], writes=[rp1])
                            for kt in range(8):
                                em.op("pe", lambda: nc.tensor.matmul(p3[:, :n], lhsT=W[1][:, kt, f * 128:(f + 1) * 128],
                                                                     rhs=hg[:, kt, o0:o0 + n], start=(kt == 0), stop=(kt == 7)),
                                      reads=[rW[1], rhg], writes=[rp3])
                            s_, rs_ = sr.next()
                            em.op("act", lambda: nc.scalar.activation(out=s_[:, :n], in_=p1[:, :n], func=AF.Silu), reads=[rp1], writes=[rs_])
                            t_, rt_ = tr.next()
                            em.op("dve", lambda: nc.vector.tensor_tensor(out=t_[:, :n], in0=p3[:, :n], in1=s_[:, :n], op=ALU.mult),
                                  reads=[rp3, rs_], writes=[rt_])
                            em.op("pool", lambda: nc.gpsimd.tensor_tensor(out=hd[:, f, :n], in0=t_[:, :n], in1=cb[:, :n], op=ALU.mult),
                                  reads=[rt_, rcb], writes=[rhd])
                        for d in range(8):
                            po, rpo = por.next()
                            for f in range(8):
                                em.op("pe", lambda: nc.tensor.matmul(po[:, :n], lhsT=W[2][:, f, d * 128:(d + 1) * 128], rhs=hd[:, f, :n],
                                                                     start=(f == 0), stop=(f == 7)), reads=[rW[2], rhd], writes=[rpo])
                            if e == 0:
                                em.op("dve", lambda: nc.vector.tensor_copy(out=acc[:, d, o0:o0 + n], in_=po[:, :n]), reads=[rpo], writes=[racc])
                            else:
                                em.op("dve", lambda: nc.vector.tensor_tensor(out=acc[:, d, o0:o0 + n], in0=po[:, :n],
                                                                             in1=acc[:, d, o0:o0 + n], op=ALU.add),
                                      reads=[rpo, racc], writes=[racc])
                for (t0, n, cond) in grp:
                    o0 = t0 - g0
                    xt, r_x = xin.next()
                    em.dma("sp", xt[:, :, :n], xTv[:, :, t0:t0 + n], reads=[self.r_xT], writes=[r_x])
                    for d in range(8):
                        em.op("dve", lambda: nc.vector.scalar_tensor_tensor(out=xt[:, d, :n], in0=acc[:, d, o0:o0 + n],
                                                                            scalar=self.modc[:, 5, d, cond:cond + 1], in1=xt[:, d, :n],
                                                                            op0=ALU.mult, op1=ALU.add),
                              reads=[racc, self.r_mod, r_x], writes=[r_x])
                    em.dma("act", xTv[:, :, t0:t0 + n], xt[:, :, :n], reads=[r_x], writes=[self.r_xT])
            em.barrier()
            em.stack = old

_CACHE = {}


def _get_prog(t_lat, depth, dbg=()):
    key = (t_lat, depth, tuple(dbg))
    if key not in _CACHE:
        lam = [0.8 - 0.6 * math.exp(-0.3 * i) for i in range(depth)]
        _CACHE[key] = K(t_lat, depth, lam, list(dbg))
    return _CACHE[key]


def run(inputs, dbg=()):
    x = np.asarray(inputs["x"], np.float32)
    B, t_lat, _ = x.shape
    depth = np.asarray(inputs["norm1_g"]).shape[0]
    prog = _get_prog(t_lat, depth, dbg)
    consts = host_consts(t_lat)
    in_maps = []
    for core in range(8):
        b = core % B
        m = {}
        for k in prog.inp:
            if k.startswith("cst_"):
                m[k] = consts[k[4:]]
            elif k == "x":
                m[k] = np.ascontiguousarray(x[b])
            elif k == "c":
                m[k] = np.ascontiguousarray(np.asarray(inputs["c"], np.float32)[b])
            elif k == "ctx":
                m[k] = np.ascontiguousarray(np.asarray(inputs["ctx"], np.float32)[b])
            elif k == "rwkv_r_k":
                m[k] = np.ascontiguousarray(np.asarray(inputs[k], np.float32).reshape(depth, 512))
            else:
                m[k] = np.ascontiguousarray(np.asarray(inputs[k], np.float32))
        in_maps.append(m)
    res = run_bass_kernel_spmd(prog.nc, in_maps, core_ids=list(range(8)))
    out = np.stack([np.asarray(res.results[b]["out"], np.float32) for b in range(B)], axis=0)
    dbg_res = {name: [np.asarray(res.results[b][name]) for b in range(B)] for name in dbg}
    return out, dbg_res


def kernel(**inputs):
    out, _ = run(inputs)
    return out
```
